# Optimizing a Trainium2 kernel written in Bass

```python
import math
import jax, jax.numpy as jnp
from jax import lax
import numpy as np

D_MODEL = 1024
BATCH = 16
SEQ = 2048
DEPTH = 4

MIX_WIDTH = D_MODEL
M_WIDTH = MIX_WIDTH // 2
A_WIDTH = MIX_WIDTH - M_WIDTH
M_HEADS = 4
M_HEAD_DIM = M_WIDTH // M_HEADS
A_HEADS = 4
A_V_DIM = A_WIDTH // A_HEADS
A_QK_DIM = A_V_DIM // 2
CONV_K = 4
CHUNK = 64
Q_BLOCK = 128
ROPE_THETA = 10000.0
FFN_HIDDEN = -(-8 * D_MODEL // (3 * 256)) * 256
EPS = 1e-6
HEAD_EPS = 1e-5
SPLITS = [2 * M_WIDTH,
          3 * M_WIDTH,
          4 * M_WIDTH,
          4 * M_WIDTH + 2 * M_HEADS,
          4 * M_WIDTH + 2 * M_HEADS + A_WIDTH,
          4 * M_WIDTH + 2 * M_HEADS + 2 * A_WIDTH]
IN_WIDTH = 4 * M_WIDTH + 2 * M_HEADS + 3 * A_WIDTH

kernel_name = "hymba_mlstm_diffattn_trunk"


def rmsnorm(x, g, eps=EPS):
    xf = x.astype(jnp.float32)
    y = xf * lax.rsqrt(jnp.mean(xf * xf, axis=-1, keepdims=True) + eps)
    return (y * g.astype(jnp.float32)).astype(x.dtype)


def causal_conv(u, w, b):
    k_len = w.shape[0]
    s = u.shape[1]
    up = jnp.pad(u, ((0, 0), (k_len - 1, 0), (0, 0)))
    y = up[:, 0:s] * w[0]
    for j in range(1, k_len):
        y = y + up[:, j:j + s] * w[j]
    return y + b


def rope_tables(seq, dim):
    pos = jnp.arange(seq, dtype=jnp.float32)
    inv = ROPE_THETA ** (-jnp.arange(0, dim, 2, dtype=jnp.float32) / dim)
    ang = pos[:, None] * inv[None, :]
    return jnp.cos(ang), jnp.sin(ang)


def apply_rope(t, cos, sin):
    c = cos[:, None, None, :].astype(t.dtype)
    s = sin[:, None, None, :].astype(t.dtype)
    t1, t2 = jnp.split(t, 2, axis=-1)
    return jnp.concatenate([t1 * c - t2 * s, t2 * c + t1 * s], axis=-1)


def mlstm_chunkwise(q, k, v, i_pre, log_f):
    b_sz, n_h, s_len, d = q.shape
    nc = s_len // CHUNK
    to_c = lambda t: jnp.moveaxis(t.reshape(b_sz, n_h, nc, CHUNK, *t.shape[3:]), 2, 0)
    qc, kc, vc, ic, fc = to_c(q), to_c(k), to_c(v), to_c(i_pre), to_c(log_f)
    mask = jnp.tril(jnp.ones((CHUNK, CHUNK), dtype=bool))

    def step(carry, inp):
        c_st, n_st, m_st = carry
        qq, kk, vv, ii, lf = inp
        bcum = jnp.cumsum(lf, axis=-1)
        dlog = bcum[..., :, None] - bcum[..., None, :] + ii[..., None, :]
        dlog = jnp.where(mask, dlog, -jnp.inf)
        inter = bcum + m_st[..., None]
        m_t = jnp.maximum(inter, jnp.max(dlog, axis=-1))
        w_intra = jnp.exp(dlog - m_t[..., None])
        w_inter = jnp.exp(inter - m_t)
        sc = jnp.einsum('bhtd,bhsd->bhts', qq, kk) * w_intra
        num = jnp.einsum('bhts,bhse->bhte', sc, vv) + \
            w_inter[..., None] * jnp.einsum('bhtd,bhed->bhte', qq, c_st)
        den = jnp.sum(sc, axis=-1) + w_inter * jnp.einsum('bhtd,bhd->bht', qq, n_st)
        h = num / jnp.maximum(jnp.abs(den), jnp.exp(-m_t))[..., None]
        b_last = bcum[..., -1]
        g = b_last[..., None] - bcum + ii
        m_new = jnp.maximum(b_last + m_st, jnp.max(g, axis=-1))
        ws = jnp.exp(g - m_new[..., None])
        decay = jnp.exp(b_last + m_st - m_new)
        c_new = decay[..., None, None] * c_st + jnp.einsum('bhs,bhse,bhsd->bhed', ws, vv, kk)
        n_new = decay[..., None] * n_st + jnp.einsum('bhs,bhsd->bhd', ws, kk)
        return (c_new, n_new, m_new), h

    init = (jnp.zeros((b_sz, n_h, d, d), jnp.float32),
            jnp.zeros((b_sz, n_h, d), jnp.float32),
            jnp.zeros((b_sz, n_h), jnp.float32))
    _, hs = lax.scan(step, init, (qc, kc, vc, ic, fc))
    return jnp.moveaxis(hs, 0, 2).reshape(b_sz, n_h, s_len, d)


def mlstm_mixer(qk_raw, v_raw, o_raw, gate_pre, conv_w, conv_b, g_head):
    b_sz, s_len, _ = v_raw.shape
    qk = jax.nn.silu(causal_conv(qk_raw, conv_w, conv_b))
    q, k = jnp.split(qk, 2, axis=-1)
    heads = lambda t: t.reshape(b_sz, s_len, M_HEADS, M_HEAD_DIM).transpose(0, 2, 1, 3).astype(jnp.float32)
    q = heads(q)
    k = heads(k) * (M_HEAD_DIM ** -0.5)
    v = heads(v_raw)
    i_pre, f_pre = jnp.split(gate_pre.astype(jnp.float32), 2, axis=-1)
    i_pre = i_pre.transpose(0, 2, 1)
    log_f = jax.nn.log_sigmoid(f_pre).transpose(0, 2, 1)
    h = mlstm_chunkwise(q, k, v, i_pre, log_f).transpose(0, 2, 1, 3)
    h = rmsnorm(h, g_head.reshape(M_HEADS, M_HEAD_DIM), HEAD_EPS)
    return jax.nn.sigmoid(o_raw) * h.reshape(b_sz, s_len, M_WIDTH).astype(o_raw.dtype)


def diff_attention_mixer(q_raw, k_raw, v_raw, cos, sin, lam, lam_init, g_head):
    b_sz, s_len, _ = v_raw.shape
    q = apply_rope(q_raw.reshape(b_sz, s_len, A_HEADS, 2, A_QK_DIM), cos, sin)
    k = apply_rope(k_raw.reshape(b_sz, s_len, A_HEADS, 2, A_QK_DIM), cos, sin)
    v = v_raw.reshape(b_sz, s_len, A_HEADS, A_V_DIM)
    nblk = s_len // Q_BLOCK
    qb = jnp.moveaxis(q.reshape(b_sz, nblk, Q_BLOCK, A_HEADS, 2, A_QK_DIM), 1, 0)
    key_pos = jnp.arange(s_len)
    scale = A_QK_DIM ** -0.5

    def block(args):
        qblk, bi = args
        s = jnp.einsum('bqhcd,bkhcd->bhcqk', qblk, k).astype(jnp.float32) * scale
        q_pos = bi * Q_BLOCK + jnp.arange(Q_BLOCK)
        causal = key_pos[None, :] <= q_pos[:, None]
        p = jax.nn.softmax(jnp.where(causal, s, -jnp.inf), axis=-1)
        a = p[:, :, 0] - lam * p[:, :, 1]
        return jnp.einsum('bhqk,bkhe->bqhe', a.astype(v.dtype), v)

    out = lax.map(block, (qb, jnp.arange(nblk)))
    out = jnp.moveaxis(out, 0, 1).reshape(b_sz, s_len, A_HEADS, A_V_DIM)
    out = rmsnorm(out, g_head.reshape(A_HEADS, A_V_DIM), HEAD_EPS) * (1.0 - lam_init)
    return out.reshape(b_sz, s_len, A_WIDTH)


def setup_inputs(seed: int = 0) -> dict:
    key = jax.random.key(seed)
    ks = jax.random.split(key, 20)
    nrm = lambda k, shape, s: jax.random.normal(k, shape, jnp.float32) * s
    b_i = nrm(ks[5], (DEPTH, M_HEADS), 0.1)
    b_f = 3.0 + 3.0 * jax.random.uniform(ks[6], (DEPTH, M_HEADS), jnp.float32)
    return {
        'x': nrm(ks[0], (BATCH, SEQ, D_MODEL), 1.0),
        'g_mix': 1.0 + nrm(ks[1], (DEPTH, D_MODEL), 0.02),
        'w_in': nrm(ks[2], (DEPTH, D_MODEL, IN_WIDTH), D_MODEL ** -0.5),
        'conv_w': nrm(ks[3], (DEPTH, CONV_K, 2 * M_WIDTH), CONV_K ** -0.5),
        'conv_b': nrm(ks[4], (DEPTH, 2 * M_WIDTH), 0.01),
        'b_gates': jnp.concatenate([b_i, b_f], axis=-1),
        'g_mlstm_head': 1.0 + nrm(ks[7], (DEPTH, M_WIDTH), 0.02),
        'lam_q1': nrm(ks[8], (DEPTH, A_QK_DIM), 0.1),
        'lam_k1': nrm(ks[9], (DEPTH, A_QK_DIM), 0.1),
        'lam_q2': nrm(ks[10], (DEPTH, A_QK_DIM), 0.1),
        'lam_k2': nrm(ks[11], (DEPTH, A_QK_DIM), 0.1),
        'g_diff_head': 1.0 + nrm(ks[12], (DEPTH, A_WIDTH), 0.02),
        'w_out': nrm(ks[13], (DEPTH, MIX_WIDTH, D_MODEL), MIX_WIDTH ** -0.5),
        'g_ffn': 1.0 + nrm(ks[14], (DEPTH, D_MODEL), 0.02),
        'w_gate': nrm(ks[15], (DEPTH, D_MODEL, FFN_HIDDEN), D_MODEL ** -0.5),
        'w_up': nrm(ks[16], (DEPTH, D_MODEL, FFN_HIDDEN), D_MODEL ** -0.5),
        'w_down': nrm(ks[17], (DEPTH, FFN_HIDDEN, D_MODEL), FFN_HIDDEN ** -0.5),
        'g_final': 1.0 + nrm(ks[18], (D_MODEL,), 0.02),
    }


def reference(x, g_mix, w_in, conv_w, conv_b, b_gates, g_mlstm_head, lam_q1, lam_k1,
              lam_q2, lam_k2, g_diff_head, w_out, g_ffn, w_gate, w_up, w_down, g_final):
    cos, sin = rope_tables(x.shape[1], A_QK_DIM)
    for l in range(DEPTH):
        h = rmsnorm(x, g_mix[l])
        z = h @ w_in[l]
        qk_m, v_m, o_m, gates, q_a, k_a, v_a = jnp.split(z, SPLITS, axis=-1)
        m_out = mlstm_mixer(qk_m, v_m, o_m, gates + b_gates[l], conv_w[l], conv_b[l],
                            g_mlstm_head[l])
        lam_init = 0.8 - 0.6 * math.exp(-0.3 * l)
        lam = (jnp.exp(jnp.sum(lam_q1[l].astype(jnp.float32) * lam_k1[l].astype(jnp.float32)))
               - jnp.exp(jnp.sum(lam_q2[l].astype(jnp.float32) * lam_k2[l].astype(jnp.float32)))
               + lam_init)
        a_out = diff_attention_mixer(q_a, k_a, v_a, cos, sin, lam, lam_init, g_diff_head[l])
        x = x + jnp.concatenate([m_out, a_out.astype(m_out.dtype)], axis=-1) @ w_out[l]
        h2 = rmsnorm(x, g_ffn[l])
        x = x + (jax.nn.silu(h2 @ w_gate[l]) * (h2 @ w_up[l])) @ w_down[l]
    return rmsnorm(x, g_final)
```

```python
import math
import contextlib
import numpy as np
import ml_dtypes
import concourse.bass as bass
import concourse.mybir as mybir
from concourse.bass_utils import run_bass_kernel_spmd

F32 = mybir.dt.float32
BF16 = mybir.dt.bfloat16
AF = mybir.ActivationFunctionType
ALU = mybir.AluOpType
AX = mybir.AxisListType

ENGS = ("pe", "act", "dve", "pool", "sp")

D = 1024
KC = 8
S = 2048
NT = 4
TT = 512
NB = 16
DEPTH = 4
INW = 3592
QM0, KM0, VM0, OM0, G0, QA0, KA0, VA0 = 0, 512, 1024, 1536, 2048, 2056, 2568, 3080
FF = 2816
FC = 22
EPS = 1e-6
HEPS = 1e-5
SC_M = 128.0 ** -0.5
SC_A = 64.0 ** -0.5
FFN_GROUPS = [(0, 6), (6, 6), (12, 5), (17, 5)]


class Op:
    __slots__ = ("eng", "fn", "deps", "sig", "dma_sem", "dma_val", "name")

    def __init__(self, eng, fn, name=""):
        self.eng = eng
        self.fn = fn
        self.deps = set()
        self.sig = None
        self.dma_sem = None
        self.dma_val = None
        self.name = name


class Prog:
    def __init__(self, nc):
        self.nc = nc
        self.ops = {e: [] for e in ENGS}
        self.all_ops = []
        self.last_writer = {}
        self.readers = {}
        self.dma_sems = {}
        self.barrier_deps = []

    def op(self, eng, fn, reads=(), writes=(), name="", dma=None, dma_total=False):
        o = Op(eng, fn, name)
        if dma is not None:
            ent = self.dma_sems.setdefault(dma, [eng, 0, dma_total])
            assert ent[0] == eng
            ent[1] += 16
            o.dma_sem = dma
            o.dma_val = ent[1]
        reads = list(reads)
        writes = list(writes)
        ps_reads = [k for k in reads if isinstance(k, tuple) and len(k) == 2 and k[0] == "ps"]
        if ps_reads:
            reads = [k for k in reads if k not in ps_reads]
            writes = writes + [("psr", k[1]) for k in ps_reads]
            for k in ps_reads:
                w = self.last_writer.get(k)
                if w is not None:
                    o.deps.add(w)
                self.readers.setdefault(k, []).append(o)
        for k in reads:
            w = self.last_writer.get(k)
            if w is not None:
                o.deps.add(w)
            self.readers.setdefault(k, []).append(o)
        for k in writes:
            w = self.last_writer.get(k)
            if w is not None:
                o.deps.add(w)
            for r in self.readers.get(k, ()):
                o.deps.add(r)
            self.readers[k] = []
            self.last_writer[k] = o
        for b in self.barrier_deps:
            o.deps.add(b)
        o.deps.discard(o)
        self.ops[eng].append(o)
        self.all_ops.append(o)
        return o

    def barrier(self):
        deps = [self.ops[e][-1] for e in ENGS if self.ops[e]]
        last_dma = {}
        for o in self.all_ops:
            if o.dma_sem is not None:
                last_dma[o.dma_sem] = o
        self.barrier_deps = deps + list(last_dma.values())

    def emit(self):
        nc = self.nc
        referenced = set()
        for o in self.all_ops:
            for d in o.deps:
                if d.dma_sem is None and not (d.eng == o.eng == "pe"):
                    referenced.add(d)
        for e in ENGS:
            c = 0
            for o in self.ops[e]:
                if o in referenced:
                    c += 1
                    o.sig = c
        with contextlib.ExitStack() as st:
            esem = {e: st.enter_context(nc.semaphore("S_" + e)) for e in ENGS}
            dsem = {n: st.enter_context(nc.semaphore("D_" + n)) for n in self.dma_sems}
            block = st.enter_context(nc.Block())

            def run(e, eng):
                waited = {}
                for o in self.ops[e]:
                    need = {}
                    for d in o.deps:
                        if d.dma_sem is not None:
                            key = ("d", d.dma_sem)
                            val = self.dma_sems[d.dma_sem][1] if self.dma_sems[d.dma_sem][2] else d.dma_val
                        else:
                            if d.eng == e and e == "pe":
                                continue
                            if d.sig is None:
                                continue
                            key = ("e", d.eng)
                            val = d.sig
                        if waited.get(key, 0) >= val:
                            continue
                        if need.get(key, 0) < val:
                            need[key] = val
                    items = list(need.items())
                    for key, val in items:
                        waited[key] = val
                    sems = [(dsem[k[1]] if k[0] == "d" else esem[k[1]], v) for k, v in items]
                    for s_, v in sems[:-1]:
                        eng.wait_ge(s_, v)
                    r = o.fn(eng)
                    first, last = r if isinstance(r, tuple) else (r, r)
                    if sems:
                        s_, v = sems[-1]
                        first._wait_ge(s_, v)
                    if o.dma_sem is not None:
                        last.then_inc(dsem[o.dma_sem], 16)
                    elif o.sig is not None:
                        last.then_inc(esem[e], 1)
                for n, (owner, cnt, _tot) in self.dma_sems.items():
                    if owner == e:
                        eng.wait_ge(dsem[n], cnt)

            @block.tensor
            def _(eng):
                run("pe", eng)

            @block.scalar
            def _(eng):
                run("act", eng)

            @block.vector
            def _(eng):
                run("dve", eng)

            @block.gpsimd
            def _(eng):
                run("pool", eng)

            @block.sync
            def _(eng):
                run("sp", eng)


class Arena:
    def __init__(self, nc, base=16512, top=229344):
        self.nc = nc
        self.cur = base
        self.top = top
        self.n = 0

    def alloc(self, name, shape, dtype):
        sz = int(np.prod(shape[1:])) * mybir.dt.size(dtype)
        off = (self.cur + 31) // 32 * 32
        assert off + sz <= self.top, f"SBUF arena overflow at {name}: need {off + sz - self.top} more bytes"
        self.cur = off + sz
        self.n += 1
        return self.nc.alloc_sbuf_tensor_at(f"{name}_{self.n}", list(shape), dtype, offset=off)

    def mark(self):
        return self.cur

    def reset(self, m):
        self.cur = m


def AP(t, off, dims):
    return bass.AP(t, off, [list(d) for d in dims])


def host_constants():
    c = {}
    I = np.eye(128, dtype=np.float32)
    c["c_identb"] = I.astype(ml_dtypes.bfloat16)
    c["c_identf"] = I
    c["c_onesb"] = np.ones((128, 128), dtype=ml_dtypes.bfloat16)
    s_ = np.arange(128)[:, None]
    t_ = np.arange(128)[None, :]
    tri = (s_ <= t_).astype(np.float32)
    c["c_maskf"] = (tri * SC_M).astype(np.float32)
    c["c_maskb"] = tri.astype(ml_dtypes.bfloat16)
    perm = np.zeros((128, 128), dtype=np.float32)
    for r in range(128):
        i = r % 64
        if i < 32:
            perm[r + 32, r] = -1.0
        else:
            perm[r - 32, r] = 1.0
    c["c_perm"] = perm.astype(ml_dtypes.bfloat16)
    pos = np.arange(S, dtype=np.float32)
    inv = (np.float32(10000.0) ** (-np.arange(0, 64, 2, dtype=np.float32) / np.float32(64))).astype(np.float32)
    ang = (pos[:, None] * inv[None, :]).astype(np.float32)
    cosr = np.cos(ang.astype(np.float64)).T
    sinr = np.sin(ang.astype(np.float64)).T
    f_of_row = np.arange(128) % 32
    c["c_cos"] = cosr[f_of_row].astype(np.float32)
    c["c_sin"] = sinr[f_of_row].astype(np.float32)
    q = np.arange(64)
    jq, hq = q // 4, q % 4
    c["c_mcarry"] = ((hq[:, None] == hq[None, :]) & (jq[:, None] < jq[None, :])).astype(ml_dtypes.bfloat16)
    sel = (hq[:, None] == np.arange(4)[None, :]).astype(np.float32)
    c["c_sel"] = sel.astype(ml_dtypes.bfloat16)
    c["c_ohj"] = (jq[:, None] == np.arange(16)[None, :]).astype(np.float32)
    c["c_selT"] = np.ascontiguousarray(sel.T).astype(ml_dtypes.bfloat16)
    return c


CONST_SPECS = [("c_identb", [128, 128], BF16), ("c_identf", [128, 128], F32), ("c_onesb", [128, 128], BF16),
               ("c_maskf", [128, 128], F32), ("c_maskb", [128, 128], BF16), ("c_perm", [128, 128], BF16),
               ("c_cos", [128, S], F32), ("c_sin", [128, S], F32), ("c_mcarry", [64, 64], BF16),
               ("c_sel", [64, 4], BF16), ("c_ohj", [64, 16], F32), ("c_selT", [4, 64], BF16)]

PARAM_SPECS = [("g_mix", [DEPTH, D]), ("w_in", [DEPTH, D, INW]), ("conv_w", [DEPTH, 4, D]), ("conv_b", [DEPTH, D]),
               ("b_gates", [DEPTH, 8]), ("g_mlstm_head", [DEPTH, 512]), ("lam_q1", [DEPTH, 64]),
               ("lam_k1", [DEPTH, 64]), ("lam_q2", [DEPTH, 64]), ("lam_k2", [DEPTH, 64]),
               ("g_diff_head", [DEPTH, 512]), ("w_out", [DEPTH, D, D]), ("g_ffn", [DEPTH, D]),
               ("w_gate", [DEPTH, D, FF]), ("w_up", [DEPTH, D, FF]), ("w_down", [DEPTH, FF, D]), ("g_final", [D])]


class Builder:
    def __init__(self, layers, nseq=2, final_norm=True, debug=None, pdepth=DEPTH):
        self.layers = list(layers)
        self.nseq = nseq
        self.final_norm = final_norm
        self.debug = {}
        self.debug_names = set(debug or [])
        nc = bass.Bass("TRN2", target_bir_lowering=False)
        self.nc = nc
        self.P = Prog(nc)
        self.ar = Arena(nc)
        self.din = {}
        self.din["x"] = nc.dram_tensor("x", [nseq, S, D], F32, kind="ExternalInput").ap()
        self.pdepth = pdepth
        for n, shp in PARAM_SPECS:
            shp = list(shp)
            if n != "g_final":
                shp[0] = pdepth
            self.din[n] = nc.dram_tensor(n, shp, F32, kind="ExternalInput").ap()
        for n, shp, dt_ in CONST_SPECS:
            self.din[n] = nc.dram_tensor(n, list(shp), dt_, kind="ExternalInput").ap()
        self.out = nc.dram_tensor("out", [nseq, S, D], F32, kind="ExternalOutput").ap()
        self.dbg_out = {}
        for n, shp in self.debug.items():
            self.dbg_out[n] = nc.dram_tensor(n, list(shp), F32, kind="ExternalOutput").ap()
        self.ps = nc.alloc_psum_tensor("ps", [128, 8, 512], F32)
        self.wq_n = 0
        self.uid = 0
        self.att_level = 9

    def dump(self, name, t, keys):
        if name not in self.debug_names:
            return
        shp = [int(v) for v in t.shape]
        dt_ = t.dtype
        d = self.nc.dram_tensor("dbg_" + name, shp, dt_, kind="ExternalOutput").ap()
        self.P.op("sp", lambda e: e.dma_start(out=d, in_=t[:]), reads=keys, name="dump", dma="dbg_" + name)

    def bank(self, b):
        return ("ps", b)

    def mm(self, out_ap, pairs, reads, writes, name="mm", start=True, stop=True, skip=False):
        n = len(pairs)

        def fn(e):
            first = last = None
            for i, (l, r) in enumerate(pairs):
                ins = e.matmul(out_ap, l, r, start=(start and i == 0), stop=(stop and i == n - 1),
                               skip_group_check=skip)
                if first is None:
                    first = ins
                last = ins
            return first, last
        return self.P.op("pe", fn, reads=reads, writes=writes, name=name)

    def transposes(self, items, reads, writes, name="tr"):
        def fn(e):
            first = last = None
            for (o, i, idn) in items:
                ins = e.transpose(out=o, in_=i, identity=idn)
                if first is None:
                    first = ins
                last = ins
            return first, last
        return self.P.op("pe", fn, reads=reads, writes=writes, name=name)

    def wload(self, dst_ap, src_ap, keys, sem, name="wload", total=False):
        if not isinstance(keys, list):
            keys = [keys]

        def fn(e):
            return e.dma_start(out=dst_ap, in_=src_ap)
        return self.P.op("pool", fn, writes=keys, name=name, dma=sem, dma_total=total)

    def sload(self, dst_ap, src_ap, key, sem="setup", name="sload", nonc=False, total=True):
        def fn(e):
            if nonc:
                return e.dma_start(out=dst_ap, in_=src_ap, allow_slow_non_contiguous=True)
            return e.dma_start(out=dst_ap, in_=src_ap)
        return self.P.op("sp", fn, writes=[key], name=name, dma=sem, dma_total=total)

    def setup(self):
        ar, P, din = self.ar, self.P, self.din
        self.xT = ar.alloc("xT", [128, KC, S], F32)
        self.hT = ar.alloc("hT", [128, KC, S], BF16)
        self.c = {}
        for n, shp, dt_ in CONST_SPECS:
            if n in ("c_cos", "c_sin"):
                t = ar.alloc(n, shp, BF16)
                self.c[n] = t
                continue
            t = ar.alloc(n, shp, dt_)
            self.c[n] = t
            self.sload(t[:], din[n], key=n)
        self.g1 = ar.alloc("g1", [128, DEPTH, KC], F32)
        self.g2 = ar.alloc("g2", [128, DEPTH, KC], F32)
        self.gf = ar.alloc("gf", [128, KC], F32)
        self.cw = ar.alloc("cw", [128, DEPTH, 4, KC], F32)
        self.cb = ar.alloc("cb", [128, DEPTH, KC], F32)
        self.bg = ar.alloc("bg", [128, DEPTH, 8], F32)
        self.lamv = ar.alloc("lamv", [128, 4, DEPTH, 64], F32)
        for l in range(self.pdepth):
            self.sload(self.g1[:, l, :], din["g_mix"][l].rearrange("(c p) -> p c", p=128), key=("g1", l), nonc=True)
            self.sload(self.g2[:, l, :], din["g_ffn"][l].rearrange("(c p) -> p c", p=128), key=("g2", l), nonc=True)
            for j in range(4):
                self.sload(self.cw[:, l, j, :], din["conv_w"][l, j].rearrange("(c p) -> p c", p=128), key=("cw", l, j), nonc=True)
            self.sload(self.cb[:, l, :], din["conv_b"][l].rearrange("(c p) -> p c", p=128), key=("cb", l), nonc=True)
        self.sload(self.gf[:], din["g_final"].rearrange("(c p) -> p c", p=128), key="gf", nonc=True)
        bgd = din["b_gates"]
        self.sload(self.bg[:, 0:self.pdepth, :], AP(bgd.tensor, 0, [[0, 128], [8, self.pdepth], [1, 8]]), key="bg")
        for i, n in enumerate(("lam_q1", "lam_k1", "lam_q2", "lam_k2")):
            self.sload(self.lamv[:, i, 0:self.pdepth, :], AP(din[n].tensor, 0, [[0, 128], [64, self.pdepth], [1, 64]]), key=("lamv", i))
        self.wload(self.c["c_cos"][:], din["c_cos"], "c_cos", "wsetup", total=True)
        self.wload(self.c["c_sin"][:], din["c_sin"], "c_sin", "wsetup", total=True)
        self.gm_bc = ar.alloc("gm_bc", [128, 512], F32)
        self.gd_bc = ar.alloc("gd_bc", [128, 512], F32)
        self.lam_s = ar.alloc("lam_s", [128, 8], F32)
        self.lam_t = ar.alloc("lam_t", [128, 2, 64], F32)
        self.phase_base = ar.mark()

    def load_x(self, sq):
        P, ar = self.P, self.ar
        m = ar.mark()
        xin = [ar.alloc("xin", [128, D], F32) for _ in range(2)]
        xd = self.din["x"]
        for b in range(NB):
            t = xin[b % 2]
            self.sload(t[:], xd[sq, b * 128:(b + 1) * 128, :], key=("xin", b % 2), sem="xin%d" % (b % 2), total=False)
            for half in range(2):
                bk = (2 * b + half) % 8
                items = [(self.ps[:, bk, cc * 128:(cc + 1) * 128], t[:, (half * 4 + cc) * 128:(half * 4 + cc + 1) * 128],
                          self.c["c_identf"][:]) for cc in range(4)]
                self.transposes(items, reads=[("xin", b % 2), "c_identf"], writes=[self.bank(bk)], name="xtr")
                src = AP(self.ps, bk * 512, [[4096, 128], [128, 4], [1, 128]])
                dst = self.xT[:, half * 4:(half + 1) * 4, b * 128:(b + 1) * 128]
                eng = "dve" if half == 0 else "act"
                if eng == "dve":
                    P.op("dve", (lambda d_, s_: lambda e: e.tensor_copy(out=d_, in_=s_))(dst, src),
                         reads=[self.bank(bk)], writes=[("xT", b // 4)])
                else:
                    P.op("act", (lambda d_, s_: lambda e: e.activation(out=d_, in_=s_, func=AF.Copy))(dst, src),
                         reads=[self.bank(bk)], writes=[("xT", b // 4)])
        P.barrier()
        ar.reset(m)

    def norm(self, gt_ap_fn, gkey, out_f32=None, tts=None):
        P, ar = self.P, self.ar
        m = ar.mark()
        sq = [ar.alloc("sq", [128, KC, TT], BF16) for _ in range(2)]
        rs = [ar.alloc("rs", [128, TT], F32) for _ in range(2)]
        for tt in (range(NT) if tts is None else tts):
            i = tt % 2
            tsl = slice(tt * TT, (tt + 1) * TT)
            P.op("act", (lambda o_, i_: lambda e: e.activation(out=o_, in_=i_, func=AF.Square))(sq[i][:], self.xT[:, :, tsl]),
                 reads=[("xT", tt)], writes=[("sq", i)])
            bk = i
            self.mm(self.ps[:, bk, :], [(self.c["c_onesb"][:], sq[i][:, c, :]) for c in range(KC)],
                    reads=[("sq", i), "c_onesb"], writes=[self.bank(bk)], name="ssq")
            P.op("act", (lambda o_, i_: lambda e: e.activation(out=o_, in_=i_, func=AF.Ln, scale=1.0 / D, bias=EPS))(rs[i][:], self.ps[:, bk, :]),
                 reads=[self.bank(bk)], writes=[("rs", i)])
            P.op("act", (lambda o_: lambda e: e.activation(out=o_, in_=o_, func=AF.Exp, scale=-0.5))(rs[i][:]),
                 reads=[("rs", i)], writes=[("rs", i)])
            for c in range(KC):
                if out_f32 is None:
                    dst = self.hT[:, c, tsl]
                    wk = ("hT", tt)
                else:
                    dst, wk = out_f32(tt, c)
                P.op("dve", (lambda d_, x_, g_, r_: lambda e: e.scalar_tensor_tensor(out=d_, in0=x_, scalar=g_, in1=r_, op0=ALU.mult, op1=ALU.mult))(
                    dst, self.xT[:, c, tsl], gt_ap_fn(c), rs[i][:]),
                    reads=[("xT", tt), ("rs", i), gkey], writes=[wk])
        ar.reset(m)

    def dve(self, fn, reads, writes, name="dve"):
        return self.P.op("dve", fn, reads=reads, writes=writes, name=name)

    def act(self, out, in_, func, reads, writes, scale=1.0, bias=0.0, accum=None, name="act"):
        def fn(e):
            kw = {}
            if accum is not None:
                kw["accum_out"] = accum
            return e.activation(out=out, in_=in_, func=func, bias=bias, scale=scale, **kw)
        return self.P.op("act", fn, reads=reads, writes=writes, name=name)

    def evac(self, k, out, in_, reads, writes):
        if k % 2 == 0:
            return self.act(out, in_, AF.Copy, reads, writes, name="evac")
        return self.dve(lambda e: e.tensor_copy(out=out, in_=in_), reads, writes, name="evac")

    def rsqrt_small(self, t_ap, key, bias):
        self.act(t_ap, t_ap, AF.Ln, [key], [key], bias=bias)
        self.act(t_ap, t_ap, AF.Exp, [key], [key], scale=-0.5)

    def split3(self, dst, src, np_, n, skey, dkey):
        ar = self.ar
        r1 = ar.alloc("sp_r1", [np_, n], F32)
        r2 = ar.alloc("sp_r2", [np_, n], F32)
        self.uid += 1
        k1, k2 = ("sp_r1", self.uid), ("sp_r2", self.uid)
        self.dve(lambda e: e.tensor_copy(out=dst[:, 0, :], in_=src), [skey], [(dkey, 0)])
        self.dve(lambda e: e.tensor_tensor(out=r1[:], in0=src, in1=dst[:, 0, :], op=ALU.subtract), [skey, (dkey, 0)], [k1])
        self.dve(lambda e: e.tensor_copy(out=dst[:, 1, :], in_=r1[:]), [k1], [(dkey, 1)])
        self.dve(lambda e: e.tensor_tensor(out=r2[:], in0=r1[:], in1=dst[:, 1, :], op=ALU.subtract), [k1, (dkey, 1)], [k2])
        self.dve(lambda e: e.tensor_copy(out=dst[:, 2, :], in_=r2[:]), [k2], [(dkey, 2)])
        return [(dkey, i) for i in range(3)]

    def layer_params(self, l):
        P, din = self.P, self.din
        lam_init = 0.8 - 0.6 * math.exp(-0.3 * l)
        self.sload(self.gm_bc[:], AP(din["g_mlstm_head"].tensor, l * 512, [[0, 128], [1, 512]]), key="gm_bc", sem="gm_bc", total=False)
        self.sload(self.gd_bc[:], AP(din["g_diff_head"].tensor, l * 512, [[0, 128], [1, 512]]), key="gd_bc", sem="gd_bc", total=False)
        gm, gd = self.gm_bc, self.gd_bc
        self.dve(lambda e: e.tensor_scalar(out=gm[:], in0=gm[:], scalar1=0.5, scalar2=None, op0=ALU.mult), ["gm_bc"], ["gm_bc"])
        self.dve(lambda e: e.tensor_scalar(out=gd[:], in0=gd[:], scalar1=1.0 - lam_init, scalar2=None, op0=ALU.mult), ["gd_bc"], ["gd_bc"])
        lv, lt, ls = self.lamv, self.lam_t, self.lam_s
        for i in range(2):
            self.dve((lambda i: lambda e: e.tensor_tensor(out=lt[:, i, :], in0=lv[:, 2 * i, l, :], in1=lv[:, 2 * i + 1, l, :], op=ALU.mult))(i),
                     [("lamv", 2 * i), ("lamv", 2 * i + 1)], ["lam_t"])
        self.dve(lambda e: e.tensor_reduce(out=ls[:, 0:2], in_=lt[:], axis=AX.X, op=ALU.add), ["lam_t"], ["lam_s"])
        self.act(ls[:, 2:4], ls[:, 0:2], AF.Exp, ["lam_s"], ["lam_s"])
        self.dve(lambda e: e.tensor_tensor(out=ls[:, 4:5], in0=ls[:, 3:4], in1=ls[:, 2:3], op=ALU.subtract), ["lam_s"], ["lam_s"])
        self.dve(lambda e: e.tensor_scalar(out=ls[:, 5:6], in0=ls[:, 4:5], scalar1=-lam_init, scalar2=None, op0=ALU.add), ["lam_s"], ["nlam"])

    def mixer_alloc(self):
        ar = self.ar
        a = {}
        a["mixT"] = ar.alloc("mixT", [128, 4, S], BF16)
        a["vfam"] = ar.alloc("vfam", [128, NB, 4, 129], BF16)
        off0 = (ar.cur + 31) // 32 * 32
        a["slotA"] = ar.alloc("slotA", [128, KC, 512], BF16)
        off1 = (ar.cur + 31) // 32 * 32
        a["slotB"] = ar.alloc("slotB", [128, KC, 512], BF16)
        a["woA"] = self.nc.alloc_sbuf_tensor_at("woA_%d" % ar.n, [128, 4, D], BF16, offset=off0)
        a["woB"] = self.nc.alloc_sbuf_tensor_at("woB_%d" % ar.n, [128, 4, D], BF16, offset=off1)
        a["qT"] = ar.alloc("qT", [128, S], BF16)
        a["kT"] = ar.alloc("kT", [128, S], BF16)
        a["wg"] = ar.alloc("wg", [128, KC, 8], BF16)
        a["gt"] = ar.alloc("gt", [128, 5, 64], F32)
        return a

    def gates(self, l, a):
        P, ar, ps, c = self.P, self.ar, self.ps, self.c
        m = ar.mark()
        wg, gt = a["wg"], a["gt"]
        wgf = ar.alloc("wgf", [128, KC, 8], F32)
        self.sload(wgf[:], self.din["w_in"][l][:, G0:G0 + 8].rearrange("(k p) n -> p k n", p=128), key="wgf", sem="wgf", total=False)
        self.dve(lambda e: e.tensor_copy(out=wg[:], in_=wgf[:]), ["wgf"], ["wg"])
        hT = self.hT

        def fn(e):
            first = last = None
            for j in range(NB):
                for k in range(KC):
                    ins = e.matmul(ps[:, 2, j * 8:(j + 1) * 8], hT[:, k, j * 128:(j + 1) * 128], wg[:, k, :],
                                   start=(k == 0), stop=(k == KC - 1))
                    if first is None:
                        first = ins
                    last = ins
            return first, last
        P.op("pe", fn, reads=["wg"] + [("hT", t) for t in range(NT)], writes=[self.bank(2)], name="gates_mm")
        g_tm = ar.alloc("g_tm", [128, 2, 64], F32)
        bg = self.bg
        self.dve(lambda e: e.tensor_tensor(out=AP(g_tm, 0, [[128, 128], [4, NB], [64, 2], [1, 4]]),
                                           in0=AP(ps, 2 * 512, [[4096, 128], [8, NB], [4, 2], [1, 4]]),
                                           in1=AP(bg, l * 8, [[DEPTH * 8, 128], [0, NB], [4, 2], [1, 4]]), op=ALU.add),
                 [self.bank(2), "bg"], ["g_tm"])
        idf = c["c_identf"]
        self.transposes([(ps[0:64, 3, 0:128], g_tm[:, 0, :], idf[:]),
                         (ps[0:64, 3, 128:256], g_tm[:, 1, :], idf[:])],
                        reads=["g_tm", "c_identf"], writes=[self.bank(3)], name="gates_tr")
        T = lambda n, w=128: ar.alloc(n, [64, w], F32)
        ef, lsp, zer, cs, carry, Bn, A, cm, cmxJ = T("ef"), T("lsp"), T("zer"), T("cs"), T("carry", 1), T("Bn"), T("A"), T("cm"), T("cmxJ", 16)
        self.act(ef[:], ps[0:64, 3, 128:256], AF.Exp, [self.bank(3)], ["ef"], scale=-1.0)
        self.act(lsp[:], ef[:], AF.Ln, ["ef"], ["lsp"], bias=1.0)
        self.dve(lambda e: e.memset(zer[:], 0.0), [], ["zer"])
        self.dve(lambda e: e.tensor_tensor_scan(out=cs[:], data0=lsp[:], data1=zer[:], initial=0.0, op0=ALU.add, op1=ALU.add),
                 ["lsp", "zer"], ["cs"])
        tot3 = ar.alloc("tot3", [64, 3, 2], BF16)
        k3 = self.split3(tot3, cs[:, 126:128], 64, 2, "cs", "tot3")
        self.mm(ps[0:64, 4, 0:2], [(c["c_mcarry"][:], tot3[:, i, :]) for i in range(3)], reads=k3 + ["c_mcarry"], writes=[self.bank(4)], name="carry")
        self.dve(lambda e: e.tensor_copy(out=carry[:], in_=ps[0:64, 4, 1:2]), [self.bank(4)], ["carry"])
        self.dve(lambda e: e.tensor_scalar(out=Bn[:], in0=cs[:], scalar1=carry[:], scalar2=None, op0=ALU.add), ["cs", "carry"], ["Bn"])
        self.dve(lambda e: e.tensor_tensor(out=A[:], in0=ps[0:64, 3, 0:128], in1=Bn[:], op=ALU.add), [self.bank(3), "Bn"], ["A"])
        self.dve(lambda e: e.tensor_tensor_scan(out=cm[:], data0=A[:], data1=A[:], initial=0.0, op0=ALU.max, op1=ALU.max), ["A"], ["cm"])
        self.dve(lambda e: e.tensor_scalar(out=cmxJ[:], in0=c["c_ohj"][:], scalar1=cm[:, 127:128], scalar2=None, op0=ALU.mult),
                 ["cm", "c_ohj"], ["cmxJ"])
        cmx3 = ar.alloc("cmx3", [64, 3, 16], BF16)
        k3 = self.split3(cmx3, cmxJ[:], 64, 16, "cmxJ", "cmx3")
        self.mm(ps[0:4, 4, 8:24], [(c["c_sel"][:], cmx3[:, i, :]) for i in range(3)], reads=k3 + ["c_sel"], writes=[self.bank(4)], name="hj")
        hj = ar.alloc("hj", [4, 16], F32)
        Mp = ar.alloc("Mp", [4, 17], F32)
        self.dve(lambda e: e.tensor_copy(out=hj[:], in_=ps[0:4, 4, 8:24]), [self.bank(4)], ["hj"])
        self.dve(lambda e: e.memset(Mp[:], 0.0), [], ["Mp"])
        self.dve(lambda e: e.tensor_tensor_scan(out=Mp[:, 1:17], data0=hj[:], data1=hj[:], initial=0.0, op0=ALU.max, op1=ALU.max),
                 ["hj", "Mp"], ["Mp"])

        Mp3 = ar.alloc("Mp3", [4, 3, 18], BF16)
        self.dve(lambda e: e.memset(Mp3[:], 0.0), [], [("Mp3", i) for i in range(3)])
        Mp3v = AP(Mp3, 0, [[54, 4], [18, 3], [1, 17]])
        k3 = self.split3(Mp3v, Mp[:], 4, 17, "Mp", "Mp3")

        def fn2(e):
            first = last = None
            for (c0, o0) in ((0, 0), (1, 16)):
                for i in range(3):
                    ins = e.matmul(ps[0:64, 5, o0:o0 + 16], c["c_selT"][:], Mp3[:, i, c0:c0 + 16], start=(i == 0), stop=(i == 2))
                    if first is None:
                        first = ins
                    last = ins
            return first, last
        P.op("pe", fn2, reads=k3 + ["c_selT"], writes=[self.bank(5)], name="mq")
        tmp2 = ar.alloc("tmp2", [64, 2, 16], F32)
        mpe = ar.alloc("mpe", [64, 2], F32)
        nb = ar.alloc("nb", [64, 4], F32)
        ohj = c["c_ohj"]
        self.dve(lambda e: e.tensor_tensor(out=tmp2[:], in0=AP(ps, 5 * 512, [[4096, 64], [16, 2], [1, 16]]),
                                           in1=AP(ohj, 0, [[16, 64], [0, 2], [1, 16]]), op=ALU.mult),
                 [self.bank(5), "c_ohj"], ["tmp2"])
        self.dve(lambda e: e.tensor_reduce(out=mpe[:], in_=tmp2[:], axis=AX.X, op=ALU.add), ["tmp2"], ["mpe"])
        Mg = T("Mg")
        self.dve(lambda e: e.tensor_scalar(out=Mg[:], in0=cm[:], scalar1=mpe[:, 0:1], scalar2=None, op0=ALU.max), ["cm", "mpe"], ["Mg"])
        self.dve(lambda e: e.tensor_scalar(out=nb[:, 0:1], in0=mpe[:, 0:1], scalar1=-1.0, scalar2=None, op0=ALU.mult), ["mpe"], ["nb"])
        self.dve(lambda e: e.tensor_scalar(out=nb[:, 1:2], in0=mpe[:, 1:2], scalar1=-1.0, scalar2=math.log(SC_M), op0=ALU.mult, op1=ALU.add), ["mpe"], ["nb"])
        self.dve(lambda e: e.tensor_tensor(out=nb[:, 2:3], in0=mpe[:, 0:1], in1=mpe[:, 1:2], op=ALU.subtract), ["mpe"], ["nb"])
        u, ws, w, fl, d2, dec, ddg = T("u"), T("ws"), T("w"), T("fl"), T("d2"), T("dec", 1), T("ddg", 64)
        self.act(u[:], A[:], AF.Exp, ["A", "nb"], ["u"], bias=nb[:, 0:1])
        self.act(ws[:], A[:], AF.Exp, ["A", "nb"], ["ws"], bias=nb[:, 1:2])
        self.act(w[:], Mg[:], AF.Exp, ["Mg", "mpe"], ["w"], scale=-1.0, bias=mpe[:, 0:1])
        self.dve(lambda e: e.tensor_tensor(out=d2[:], in0=Bn[:], in1=Mg[:], op=ALU.subtract), ["Bn", "Mg"], ["d2"])
        self.act(fl[:], d2[:], AF.Exp, ["d2"], ["fl"])
        self.act(dec[:], nb[:, 2:3], AF.Exp, ["nb"], ["dec"])
        dec3 = ar.alloc("dec3", [64, 3, 2], BF16)
        decf = ar.alloc("decf", [64, 2], F32)
        self.dve(lambda e: e.tensor_copy(out=decf[:], in_=AP(dec, 0, [[1, 64], [0, 2]])), ["dec"], ["decf"])
        k3d = self.split3(dec3, decf[:], 64, 2, "decf", "dec3")
        ddg3 = ar.alloc("ddg3", [64, 3, 64], BF16)
        decs = ar.alloc("decs", [64, 4], F32)
        self.dve(lambda e: e.tensor_copy(out=decs[:, 0:3], in_=dec3[:, :, 0]), k3d, ["decs"])
        for i in range(3):
            self.dve((lambda i: lambda e: e.tensor_scalar(out=ddg3[:, i, :], in0=c["c_identb"][0:64, 0:64], scalar1=decs[:, i:i + 1], scalar2=None, op0=ALU.mult))(i),
                     ["decs", "c_identb"], [("ddg3", i)])

        def fn3(e):
            first = None
            for i, arr in enumerate((u, ws, w, fl)):
                ins = e.transpose(out=ps[:, 6, i * 64:(i + 1) * 64], in_=arr[:], identity=idf[0:64, 0:64])
                if first is None:
                    first = ins
            for i in range(3):
                last = e.matmul(ps[:, 6, 256:320], c["c_onesb"][0:64, :], ddg3[:, i, :], start=(i == 0), stop=(i == 2))
            return first, last
        P.op("pe", fn3, reads=["u", "ws", "w", "fl", "c_onesb", "c_identf"] + [("ddg3", i) for i in range(3)], writes=[self.bank(6)], name="gt_tr")
        self.dve(lambda e: e.tensor_copy(out=gt[:], in_=AP(ps, 6 * 512, [[4096, 128], [64, 5], [1, 64]])), [self.bank(6)], ["gt"])

    def vfam_project(self, l, a, col0, slot, slotkey):
        P, ps = self.P, self.ps
        vf, hT = a["vfam"], self.hT
        self.wload(slot[:], self.din["w_in"][l][:, col0:col0 + 512].rearrange("(k p) n -> p k n", p=128),
                   [(slotkey, i) for i in range(3)], "s%d_0" % slotkey[1])
        slotkey = (slotkey, 0)
        for j in range(NB):
            bk = j % 4
            self.mm(ps[:, bk, :], [(hT[:, k, j * 128:(j + 1) * 128], slot[:, k, :]) for k in range(KC)],
                    reads=[slotkey, ("hT", j // 4)], writes=[self.bank(bk)], name="vproj")
            dst = AP(vf, j * 4 * 129, [[NB * 4 * 129, 128], [129, 4], [1, 128]])
            src = AP(ps, bk * 512, [[4096, 128], [128, 4], [1, 128]])
            self.evac(j, dst, src, [self.bank(bk)], [("vfam", j)])

    def mlstm(self, l, a):
        P, ar, ps, c = self.P, self.ar, self.ps, self.c
        hT, mixT, vf, qT, kT, gt = self.hT, a["mixT"], a["vfam"], a["qT"], a["kT"], a["gt"]
        psb = [ps[:, b, :].bitcast(BF16) for b in range(8)]
        ubuf = ar.alloc("ubuf", [128, 3 + S], BF16)
        diag = ar.alloc("diag", [128, 4, 128], BF16)
        ktok = ar.alloc("ktok", [128, NB, 128], BF16)
        accs = ar.alloc("accs", [128, NB, 129], F32)
        CTf = ar.alloc("CTf", [128, 129], F32)
        CTb = ar.alloc("CTb", [128, 129], BF16)
        scT = [ar.alloc("scT", [128, 128], BF16) for _ in range(2)]
        junk = ar.alloc("junk", [128, 128], BF16)
        sm = ar.alloc("sm", [128, 6, NB], F32)
        hn4 = [ar.alloc("hn4", [128, 4, 128], BF16) for _ in range(2)]
        self.dve(lambda e: e.memset(ubuf[:, 0:3], 0.0), [], ["ubuf"])
        self.dve(lambda e: e.memset(AP(vf, 128, [[NB * 4 * 129, 128], [129, NB * 4]]), 1.0), [], [("vfam", j) for j in range(NB)])
        slots = [(a["slotA"], ("slot", 0)), (a["slotB"], ("slot", 1))]
        self.vfam_project(l, a, VM0, *slots[0])
        for h in range(4):
            slot, sk = slots[(h + 1) % 2]
            w_in = self.din["w_in"][l]
            for i, c0 in enumerate((QM0 + 128 * h, KM0 + 128 * h, OM0 + 128 * h)):
                self.wload(slot[:, :, i * 128:(i + 1) * 128], w_in[:, c0:c0 + 128].rearrange("(k p) n -> p k n", p=128), (sk, i), "s%d_%d" % (sk[1], i))
            for tt in range(NT):
                tsl = slice(tt * TT, (tt + 1) * TT)
                bk = tt % 4
                self.mm(ps[:, bk, :], [(slot[:, k, 256:384], hT[:, k, tsl]) for k in range(KC)],
                        reads=[(sk, 2), ("hT", tt)], writes=[self.bank(bk)], name="oproj")
                self.act(mixT[:, h, tsl], ps[:, bk, :], AF.Tanh, [self.bank(bk)], [("mixT", h, tt)], scale=0.5)
            for i, dstT in enumerate((qT, kT)):
                cch = h if i == 0 else 4 + h
                cw, cb = self.cw, self.cb
                for j in range(4):
                    self.dve((lambda j, cch: lambda e: e.tensor_scalar(out=diag[:, j, :], in0=c["c_identb"][:], scalar1=cw[:, l, j, cch:cch + 1],
                                                                        scalar2=None, op0=ALU.mult))(j, cch),
                             ["c_identb", ("cw", l, j)], ["diag"])
                for tt in range(NT):
                    tsl = slice(tt * TT, (tt + 1) * TT)
                    bk = 4 + tt % 4
                    self.mm(ps[:, bk, :], [(slot[:, k, i * 128:(i + 1) * 128], hT[:, k, tsl]) for k in range(KC)],
                            reads=[(sk, i), ("hT", tt)], writes=[self.bank(bk)], name="qkproj")
                    self.evac(tt, ubuf[:, 3 + tt * TT:3 + (tt + 1) * TT], ps[:, bk, :], [self.bank(bk)], ["ubuf"])
                for tt in range(NT):
                    tsl = slice(tt * TT, (tt + 1) * TT)
                    bk = tt % 4
                    self.mm(ps[:, bk, :], [(diag[:, j, :], ubuf[:, tt * TT + j:tt * TT + j + TT]) for j in range(4)],
                            reads=["diag", "ubuf"], writes=[self.bank(bk)], name="conv")
                    self.act(dstT[:, tsl], ps[:, bk, :], AF.Silu, [self.bank(bk), ("cb", l)], [("qk", i, tt)], bias=cb[:, l, cch:cch + 1])
            for jg in range(4):
                bk = 4 + jg
                self.transposes([(psb[bk][:, cc * 128:(cc + 1) * 128], kT[:, (4 * jg + cc) * 128:(4 * jg + cc + 1) * 128], c["c_identb"][:]) for cc in range(4)],
                                reads=[("qk", 1, jg), "c_identb"], writes=[self.bank(bk)], name="ktr")
                self.dve((lambda jg, bk, h: lambda e: e.tensor_tensor(
                    out=ktok[:, 4 * jg:4 * jg + 4, :], in0=psb[bk][:, 0:512].rearrange("p (c d) -> p c d", c=4),
                    in1=AP(gt, 64 + 16 * jg + h, [[320, 128], [4, 4], [0, 128]]), op=ALU.mult))(jg, bk, h),
                    [self.bank(bk), "gt"], [("ktok", jg)])
            self.dve(lambda e: e.memset(CTf[:], 0.0), [], ["CTf"])
            for j in range(NB):
                bsl = slice(j * 128, (j + 1) * 128)
                b0, b1, b2 = (0, 1, 2) if j % 2 == 0 else (3, 4, 5)
                i = j % 2
                self.mm(ps[:, b0, 0:128], [(kT[:, bsl], qT[:, bsl])], reads=[("qk", 0, j // 4), ("qk", 1, j // 4)],
                        writes=[self.bank(b0)], name="ST")
                self.dve((lambda i, b0, j, h: lambda e: e.scalar_tensor_tensor(out=scT[i][:], in0=ps[:, b0, 0:128], scalar=gt[:, 0, 4 * j + h:4 * j + h + 1],
                                                                               in1=c["c_maskf"][:], op0=ALU.mult, op1=ALU.mult))(i, b0, j, h),
                         [self.bank(b0), "gt", "c_maskf"], [("scT", i)])
                pairs = [(scT[i][:], vf[:, j, h, :])]
                rd = [("scT", i), ("vfam", j)]
                if j > 0:
                    pairs.append((qT[:, bsl], CTb[:]))
                    rd += [("qk", 0, j // 4), "CTb"]
                self.mm(ps[:, b1, 0:129], pairs, reads=rd, writes=[self.bank(b1)], name="acc")
                if j < NB - 1:
                    self.mm(ps[:, b2, 0:129], [(ktok[:, j, :], vf[:, j, h, :])], reads=[("ktok", j // 4), ("vfam", j)],
                            writes=[self.bank(b2)], name="upd")
                    self.dve((lambda b2, j, h: lambda e: e.scalar_tensor_tensor(out=CTf[:], in0=CTf[:], scalar=gt[:, 4, 4 * j + h:4 * j + h + 1],
                                                                                in1=ps[:, b2, 0:129], op0=ALU.mult, op1=ALU.add))(b2, j, h),
                             [self.bank(b2), "gt", "CTf"], ["CTf"])
                    self.act(CTb[:], CTf[:], AF.Copy, ["CTf"], ["CTb"])
                self.dve((lambda b1, j: lambda e: e.tensor_copy(out=accs[:, j, :], in_=ps[:, b1, 0:129]))(b1, j),
                         [self.bank(b1)], [("accs", j)])
                self.act(junk[:], accs[:, j, 0:128], AF.Square, [("accs", j)], ["junk", ("ssq", j)], accum=sm[:, 0, j:j + 1])
            ak = [("accs", j) for j in range(NB)]
            if h == 0:
                self.dump("accs0", accs, ak)
            den = AP(accs, 128, [[NB * 129, 128], [129, NB]])
            w_tm = AP(gt, 2 * 64 + h, [[320, 128], [4, NB]])
            fl_tm = AP(gt, 3 * 64 + h, [[320, 128], [4, NB]])
            s_ = lambda r: sm[:, r, :]
            self.dve(lambda e, w_tm=w_tm: e.tensor_tensor(out=s_(1), in0=den, in1=w_tm, op=ALU.mult), ak + ["gt"], ["sm1"])
            self.dve(lambda e: e.tensor_scalar(out=s_(2), in0=s_(1), scalar1=-1.0, scalar2=None, op0=ALU.mult), ["sm1"], ["sm2"])
            self.dve(lambda e: e.tensor_tensor(out=s_(2), in0=s_(2), in1=s_(1), op=ALU.max), ["sm1", "sm2"], ["sm2"])
            self.dve(lambda e, fl_tm=fl_tm: e.tensor_tensor(out=s_(2), in0=s_(2), in1=fl_tm, op=ALU.max), ["sm2", "gt"], ["sm2"])
            self.dve(lambda e: e.reciprocal(out=s_(3), in_=s_(2)), ["sm2"], ["sm3"])
            self.dve(lambda e, w_tm=w_tm: e.tensor_tensor(out=s_(3), in0=s_(3), in1=w_tm, op=ALU.mult), ["sm3", "gt"], ["sm3"])
            self.dve(lambda e: e.tensor_tensor(out=s_(4), in0=s_(0), in1=s_(3), op=ALU.mult), ["sm3"] + [("ssq", j) for j in range(NB)], ["sm4"])
            self.dve(lambda e: e.tensor_tensor(out=s_(4), in0=s_(4), in1=s_(3), op=ALU.mult), ["sm3", "sm4"], ["sm4"])
            self.dve(lambda e: e.tensor_scalar(out=s_(4), in0=s_(4), scalar1=1.0 / 128, scalar2=None, op0=ALU.mult), ["sm4"], ["sm4"])
            self.rsqrt_small(s_(4), "sm4", HEPS)
            self.dve(lambda e: e.tensor_tensor(out=s_(5), in0=s_(4), in1=s_(3), op=ALU.mult), ["sm3", "sm4"], ["sm5"])
            if h == 0:
                self.dump("sm0", sm, ["sm5", "sm4", "sm3", "sm2", "sm1"] + [("ssq", j) for j in range(NB)])
            for jg in range(4):
                hb = hn4[jg % 2]
                hk = ("hn4", jg % 2)
                self.dve((lambda jg, hb: lambda e: e.tensor_tensor(out=hb[:], in0=accs[:, 4 * jg:4 * jg + 4, 0:128],
                                                                   in1=AP(sm, 5 * NB + 4 * jg, [[6 * NB, 128], [1, 4], [0, 128]]), op=ALU.mult))(jg, hb),
                         ak + ["sm5"], [hk])
                self.dve((lambda hb, h: lambda e: e.tensor_tensor(out=hb[:], in0=hb[:], in1=AP(self.gm_bc, h * 128, [[512, 128], [0, 4], [1, 128]]), op=ALU.mult))(hb, h),
                         [hk, "gm_bc"], [hk])
                bk = 6 + jg % 2
                self.transposes([(psb[bk][:, cc * 128:(cc + 1) * 128], hb[:, cc, :], c["c_identb"][:]) for cc in range(4)],
                                reads=[hk, "c_identb"], writes=[self.bank(bk)], name="hntr")
                msl = mixT[:, h, jg * TT:(jg + 1) * TT]
                self.dve((lambda msl, bk: lambda e: e.scalar_tensor_tensor(out=msl, in0=msl, scalar=1.0, in1=psb[bk][:, 0:512], op0=ALU.add, op1=ALU.mult))(msl, bk),
                         [self.bank(bk), ("mixT", h, jg)], [("mixT", h, jg)])

    def wout_half(self, l, a, f, wo, wkey):
        ps, xT, mixT = self.ps, self.xT, a["mixT"]
        self.wload(wo[:], self.din["w_out"][l][f * 512:(f + 1) * 512, :].rearrange("(k p) n -> p k n", p=128),
                   [(wkey, i) for i in range(3)], "s%d_0" % wkey[1])
        wkey = (wkey, 0)
        n = 0
        for tt in range(NT):
            tsl = slice(tt * TT, (tt + 1) * TT)
            for dm in range(KC):
                bk = n % 8
                n += 1
                self.mm(ps[:, bk, :], [(wo[:, k, dm * 128:(dm + 1) * 128], mixT[:, k, tsl]) for k in range(4)],
                        reads=[wkey] + [("mixT", k, tt) for k in range(4)], writes=[self.bank(bk)], name="wout")
                xs = xT[:, dm, tsl]
                self.dve((lambda xs, bk: lambda e: e.tensor_tensor(out=xs, in0=xs, in1=ps[:, bk, :], op=ALU.add))(xs, bk),
                         [self.bank(bk), ("xT", tt)], [("xT", tt)])

    def attention(self, l, a):
        P, ar, ps, c = self.P, self.ar, self.ps, self.c
        hT, mixT, vf, qT, kT = self.hT, a["mixT"], a["vfam"], a["qT"], a["kT"]
        psb = [ps[:, b, :].bitcast(BF16) for b in range(8)]
        zb = [ar.alloc("zb", [128, TT], BF16) for _ in range(2)]
        t1 = ar.alloc("t1", [128, TT], F32)
        t2 = ar.alloc("t2", [128, TT], F32)
        Pt = [ar.alloc("Pt", [128, 2, TT], BF16) for _ in range(2)]
        Osb = ar.alloc("Osb", [128, 4, 2, 129], F32)
        o1 = ar.alloc("o1", [128, 4, 128], F32)
        o2 = ar.alloc("o2", [128, 4, 128], F32)
        on4 = ar.alloc("on4", [128, 4, 128], BF16)
        sa = ar.alloc("sa", [128, 6, 8], F32)
        slots = [(a["slotA"], ("slot", 0)), (a["slotB"], ("slot", 1))]
        self.vfam_project(l, a, VA0, *slots[0])
        w_in = self.din["w_in"][l]
        cosT, sinT = c["c_cos"], c["c_sin"]
        nlam = self.lam_s[:, 5:6]
        for h in range(4):
            slot, sk = slots[(h + 1) % 2]
            for i, c0 in enumerate((QA0 + 128 * h, KA0 + 128 * h)):
                self.wload(slot[:, :, i * 128:(i + 1) * 128], w_in[:, c0:c0 + 128].rearrange("(k p) n -> p k n", p=128), (sk, i), "s%d_%d" % (sk[1], i))
            for i, dstT in enumerate((qT, kT)):
                for tt in range(NT):
                    tsl = slice(tt * TT, (tt + 1) * TT)
                    b0 = (2 * tt) % 4
                    b1 = b0 + 1
                    z = zb[tt % 2]
                    zk = ("zb", tt % 2)
                    self.mm(ps[:, b0, :], [(slot[:, k, i * 128:(i + 1) * 128], hT[:, k, tsl]) for k in range(KC)],
                            reads=[(sk, i), ("hT", tt)], writes=[self.bank(b0)], name="aproj")
                    self.act(z[:], ps[:, b0, :], AF.Copy, [self.bank(b0)], [zk])
                    self.mm(ps[:, b1, :], [(c["c_perm"][:], z[:])], reads=[zk, "c_perm"], writes=[self.bank(b1)], name="rot")
                    self.dve((lambda b0, tsl: lambda e: e.tensor_tensor(out=t1[:], in0=ps[:, b0, :], in1=cosT[:, tsl], op=ALU.mult))(b0, tsl),
                             [self.bank(b0), "c_cos"], ["t1"])
                    self.dve((lambda b1, tsl: lambda e: e.tensor_tensor(out=t2[:], in0=ps[:, b1, :], in1=sinT[:, tsl], op=ALU.mult))(b1, tsl),
                             [self.bank(b1), "c_sin"], ["t2"])
                    self.dve((lambda dstT, tsl: lambda e: e.tensor_tensor(out=dstT[:, tsl], in0=t1[:], in1=t2[:], op=ALU.add))(dstT, tsl),
                             ["t1", "t2"], [("qk", i, tt)])
            for qt in range(NT if self.att_level >= 2 else 0):
                nkb = 4 * qt + 4
                for kb in range(nkb):
                    di = kb - 4 * qt
                    q0 = max(di, 0) * 128
                    N = TT - q0
                    pi = kb % 2
                    pt = Pt[pi]
                    sb0, sb1 = 2 * pi, 2 * pi + 1

                    def fqk(e, kb=kb, q0=q0, N=N, sb0=sb0, sb1=sb1, qt=qt):
                        i0 = e.matmul(ps[:, sb0, 0:N], kT[0:64, kb * 128:(kb + 1) * 128], qT[0:64, qt * TT + q0:(qt + 1) * TT], start=True, stop=True)
                        i1 = e.matmul(ps[:, sb1, 0:N], kT[64:128, kb * 128:(kb + 1) * 128], qT[64:128, qt * TT + q0:(qt + 1) * TT], start=True, stop=True)
                        return i0, i1
                    P.op("pe", fqk, reads=[("qk", 0, qt), ("qk", 1, kb // 4)], writes=[self.bank(sb0), self.bank(sb1)], name="qk")
                    self.act(pt[:, 0, 0:N], ps[:, sb0, 0:N], AF.Exp, [self.bank(sb0)], [("Pt", pi, 0)], scale=SC_A)
                    self.act(pt[:, 1, 0:N], ps[:, sb1, 0:N], AF.Exp, [self.bank(sb1)], [("Pt", pi, 1)], scale=SC_A)
                    if di >= 0:
                        self.dve((lambda pt: lambda e: e.tensor_tensor(out=pt[:, :, 0:128], in0=pt[:, :, 0:128],
                                                                       in1=AP(c["c_maskb"], 0, [[128, 128], [0, 2], [1, 128]]), op=ALU.mult))(pt),
                                 [("Pt", pi, 0), ("Pt", pi, 1), "c_maskb"], [("Pt", pi, 0), ("Pt", pi, 1)])
                    qs0 = max(di, 0)
                    if self.att_level < 3:
                        continue

                    def fpv(e, kb=kb, qs0=qs0, pt=pt, h=h, qt=qt):
                        first = last = None
                        for qs in range(qs0, 4):
                            for cc in range(2):
                                ins = e.matmul(ps[:, 4 + qs, cc * 129:(cc + 1) * 129], pt[:, cc, (qs - qs0) * 128:(qs - qs0 + 1) * 128], vf[:, kb, h, :],
                                               start=(kb == 0 and cc == 0), stop=(kb == 4 * qt + qs), skip_group_check=True)
                                if first is None:
                                    first = ins
                                last = ins
                        return first, last
                    P.op("pe", fpv, reads=[("Pt", pi, 0), ("Pt", pi, 1), ("vfam", kb)], writes=[self.bank(4 + qs) for qs in range(qs0, 4)], name="pv")
                    if di >= 0:
                        qs = di
                        self.dve((lambda qs: lambda e: e.tensor_copy(out=Osb[:, qs, :, :], in_=ps[:, 4 + qs, 0:258].rearrange("p (c e) -> p c e", c=2)))(qs),
                                 [self.bank(4 + qs)], ["Osb"])
                if self.att_level < 4:
                    continue
                lcol = AP(Osb, 128, [[4 * 2 * 129, 128], [129, 8]])
                self.dve(lambda e: e.reciprocal(out=sa[:, 0, :], in_=lcol), ["Osb"], ["sa0"])
                self.dve(lambda e: e.tensor_scalar(out=sa[:, 1, :], in0=sa[:, 0, :], scalar1=nlam, scalar2=None, op0=ALU.mult), ["sa0", "nlam"], ["sa1"])
                self.dve(lambda e: e.tensor_tensor(out=o1[:], in0=Osb[:, :, 0, 0:128], in1=AP(sa, 0, [[48, 128], [2, 4], [0, 128]]), op=ALU.mult), ["Osb", "sa0"], ["o1"])
                self.dve(lambda e: e.tensor_tensor(out=o2[:], in0=Osb[:, :, 1, 0:128], in1=AP(sa, 8 + 1, [[48, 128], [2, 4], [0, 128]]), op=ALU.mult), ["Osb", "sa1"], ["o2"])
                self.dve(lambda e: e.tensor_tensor(out=o1[:], in0=o1[:], in1=o2[:], op=ALU.add), ["o1", "o2"], ["o1"])
                self.dve(lambda e: e.tensor_tensor(out=o2[:], in0=o1[:], in1=o1[:], op=ALU.mult), ["o1", "o2"], ["o2"])
                self.dve(lambda e: e.tensor_reduce(out=sa[:, 2, 0:4], in_=o2[:], axis=AX.X, op=ALU.add), ["o2"], ["sa2"])
                self.dve(lambda e: e.tensor_scalar(out=sa[:, 2, 0:4], in0=sa[:, 2, 0:4], scalar1=1.0 / 128, scalar2=None, op0=ALU.mult), ["sa2"], ["sa2"])
                self.rsqrt_small(sa[:, 2, 0:4], "sa2", HEPS)
                self.dve(lambda e: e.tensor_tensor(out=o1[:], in0=o1[:], in1=AP(sa, 16, [[48, 128], [1, 4], [0, 128]]), op=ALU.mult), ["o1", "sa2"], ["o1"])
                self.dve((lambda h: lambda e: e.tensor_tensor(out=on4[:], in0=o1[:], in1=AP(self.gd_bc, h * 128, [[512, 128], [0, 4], [1, 128]]), op=ALU.mult))(h),
                         ["o1", "gd_bc"], ["on4"])
                bk = 2 * (qt % 2)
                self.transposes([(psb[bk][:, cc * 128:(cc + 1) * 128], on4[:, cc, :], c["c_identb"][:]) for cc in range(4)],
                                reads=["on4", "c_identb"], writes=[self.bank(bk)], name="ontr")
                self.act(mixT[:, h, qt * TT:(qt + 1) * TT], psb[bk][:, 0:512], AF.Copy, [self.bank(bk)], [("mixT", h, qt)])

    def ffn(self, l):
        P, ar, ps = self.P, self.ar, self.ps
        hT, xT, din = self.hT, self.xT, self.din
        wg = [ar.alloc("fwg", [128, KC, 6 * 128], BF16) for _ in range(2)]
        wu = [ar.alloc("fwu", [128, KC, 6 * 128], BF16) for _ in range(2)]
        wd = [ar.alloc("fwd", [128, 6, D], BF16) for _ in range(2)]
        actb = [ar.alloc("actb", [128, 6, TT], BF16) for _ in range(2)]
        sg = [ar.alloc("sg", [128, TT], BF16) for _ in range(2)]
        n_e = 0
        n_d = 0
        for gi, (c0, ncn) in enumerate(FFN_GROUPS):
            i = gi % 2
            self.wload(wg[i][:, :, 0:ncn * 128], din["w_gate"][l][:, c0 * 128:(c0 + ncn) * 128].rearrange("(k p) n -> p k n", p=128), ("fwg", i), "fwg%d" % i)
            self.wload(wu[i][:, :, 0:ncn * 128], din["w_up"][l][:, c0 * 128:(c0 + ncn) * 128].rearrange("(k p) n -> p k n", p=128), ("fwu", i), "fwu%d" % i)
            self.wload(wd[i][:, 0:ncn, :], din["w_down"][l][c0 * 128:(c0 + ncn) * 128, :].rearrange("(c p) n -> p c n", p=128), ("fwd", i), "fwd%d" % i)
            for tt in range(NT):
                tsl = slice(tt * TT, (tt + 1) * TT)
                ab = actb[tt % 2]
                for hc in range(ncn):
                    bg_, bu_ = (0, 1) if n_e % 2 == 0 else (2, 3)
                    s_ = sg[n_e % 2]
                    n_e += 1
                    self.mm(ps[:, bg_, :], [(wg[i][:, k, hc * 128:(hc + 1) * 128], hT[:, k, tsl]) for k in range(KC)],
                            reads=[("fwg", i), ("hT", tt)], writes=[self.bank(bg_)], name="gate")
                    self.mm(ps[:, bu_, :], [(wu[i][:, k, hc * 128:(hc + 1) * 128], hT[:, k, tsl]) for k in range(KC)],
                            reads=[("fwu", i), ("hT", tt)], writes=[self.bank(bu_)], name="up")
                    self.act(s_[:], ps[:, bg_, :], AF.Silu, [self.bank(bg_)], [("sg", (n_e - 1) % 2)])
                    self.dve((lambda ab, hc, s_, bu_: lambda e: e.tensor_tensor(out=ab[:, hc, :], in0=s_[:], in1=ps[:, bu_, :], op=ALU.mult))(ab, hc, s_, bu_),
                             [("sg", (n_e - 1) % 2), self.bank(bu_)], [("actb", tt % 2, hc)])
                for dm in range(KC):
                    bk = 4 + n_d % 4
                    n_d += 1
                    self.mm(ps[:, bk, :], [(wd[i][:, hc, dm * 128:(dm + 1) * 128], ab[:, hc, :]) for hc in range(ncn)],
                            reads=[("fwd", i)] + [("actb", tt % 2, hc) for hc in range(ncn)], writes=[self.bank(bk)], name="down")
                    xs = xT[:, dm, tsl]
                    self.dve((lambda xs, bk: lambda e: e.tensor_tensor(out=xs, in0=xs, in1=ps[:, bk, :], op=ALU.add))(xs, bk),
                             [self.bank(bk), ("xT", tt)], [("xT", tt)])

    def store(self, sq, dst_dram, normalize):
        P, ar, ps, c = self.P, self.ar, self.ps, self.c
        m = ar.mark()
        yT = [ar.alloc("yT", [128, KC, TT], F32) for _ in range(2)]
        ob = [ar.alloc("ob", [128, D], F32) for _ in range(2)]
        nb_ = 0
        gf = self.gf
        for tt in range(NT):
            if normalize:
                self.norm(lambda cc: gf[:, cc:cc + 1], "gf", out_f32=lambda tt, cc: (yT[tt % 2][:, cc, :], ("yT", tt % 2)), tts=[tt])
            for bb in range(4):
                blk = tt * 4 + bb
                o_ = ob[nb_ % 2]
                ok = ("ob", nb_ % 2)
                nb_ += 1
                for half in range(2):
                    bk = (2 * blk + half) % 8
                    if normalize:
                        srcs = [yT[tt % 2][:, half * 4 + cc, bb * 128:(bb + 1) * 128] for cc in range(4)]
                        rk = [("yT", tt % 2)]
                    else:
                        srcs = [self.xT[:, half * 4 + cc, blk * 128:(blk + 1) * 128] for cc in range(4)]
                        rk = [("xT", tt)]
                    self.transposes([(ps[:, bk, cc * 128:(cc + 1) * 128], srcs[cc], c["c_identf"][:]) for cc in range(4)],
                                    reads=rk + ["c_identf"], writes=[self.bank(bk)], name="otr")
                    self.evac(half, o_[:, half * 512:(half + 1) * 512], ps[:, bk, :], [self.bank(bk)], [ok])
                P.op("sp", (lambda o_, blk: lambda e: e.dma_start(out=dst_dram[sq, blk * 128:(blk + 1) * 128, :], in_=o_[:]))(o_, blk),
                     reads=[ok], name="ostore", dma="st%d" % ((nb_ - 1) % 2))
        P.barrier()
        ar.reset(m)

    def build(self, stage=99):
        P, ar = self.P, self.ar
        self.setup()
        for sq in range(self.nseq):
            self.load_x(sq)
            for l in self.layers:
                if stage < 1:
                    break
                self.layer_params(l)
                g1 = self.g1
                self.norm((lambda l: lambda cc: g1[:, l, cc:cc + 1])(l), ("g1", l))
                P.barrier()
                self.dump("hT", self.hT, [("hT", t) for t in range(NT)])
                m = ar.mark()
                a = self.mixer_alloc()
                if stage >= 2:
                    self.gates(l, a)
                    self.dump("gt", a["gt"], ["gt"])
                m2 = ar.mark()
                if stage >= 3:
                    self.mlstm(l, a)
                    self.dump("mixM", a["mixT"], [("mixT", h, t) for h in range(4) for t in range(NT)])
                if stage >= 4:
                    self.wout_half(l, a, 0, a["woB"], ("slot", 1))
                P.barrier()
                ar.reset(m2)
                if stage >= 5:
                    self.attention(l, a)
                    self.dump("mixA", a["mixT"], [("mixT", h, t) for h in range(4) for t in range(NT)])
                if stage >= 6:
                    self.wout_half(l, a, 1, a["woB"], ("slot", 1))
                    self.dump("xmid", self.xT, [("xT", t) for t in range(NT)])
                P.barrier()
                ar.reset(m)
                if stage >= 7:
                    g2 = self.g2
                    self.norm((lambda l: lambda cc: g2[:, l, cc:cc + 1])(l), ("g2", l))
                    P.barrier()
                    self.ffn(l)
                    P.barrier()
                ar.reset(m)
            self.store(sq, self.out, self.final_norm)
        P.emit()
        return self.nc


_CONSTS = None


def _run(nc_inputs_list, layers, final_norm):
    b = Builder(layers, nseq=2, final_norm=final_norm)
    nc = b.build()
    res = run_bass_kernel_spmd(nc, nc_inputs_list, core_ids=list(range(8)))
    return [r["out"] for r in res.results]


def kernel(**inputs):
    global _CONSTS
    if _CONSTS is None:
        _CONSTS = host_constants()
    x = np.ascontiguousarray(inputs["x"], dtype=np.float32)
    params = {n: np.ascontiguousarray(inputs[n], dtype=np.float32) for n, _ in PARAM_SPECS}
    shards = [x[2 * i:2 * i + 2] for i in range(8)]
    in_maps = []
    for i in range(8):
        mp = {"x": shards[i]}
        mp.update(params)
        mp.update(_CONSTS)
        in_maps.append(mp)
    outs = _run(in_maps, list(range(DEPTH)), True)
    return np.concatenate(outs, axis=0)
```

```python
import math
import contextlib
import numpy as np
import ml_dtypes
import concourse.bass as bass
import concourse.mybir as mybir
from concourse.bass_utils import run_bass_kernel_spmd

F32 = mybir.dt.float32
BF16 = mybir.dt.bfloat16
AF = mybir.ActivationFunctionType
ALU = mybir.AluOpType
AX = mybir.AxisListType

ENGS = ("pe", "act", "dve", "pool", "sp")

D = 1024
KC = 8
S = 2048
NT = 4
TT = 512
NB = 16
DEPTH = 4
INW = 3592
QM0, KM0, VM0, OM0, G0, QA0, KA0, VA0 = 0, 512, 1024, 1536, 2048, 2056, 2568, 3080
FF = 2816
FC = 22
EPS = 1e-6
HEPS = 1e-5
SC_M = 128.0 ** -0.5
SC_A = 64.0 ** -0.5
FFN_GROUPS = [(0, 6), (6, 6), (12, 5), (17, 5)]


class Op:
    __slots__ = ("eng", "fn", "deps", "sig", "dma_sem", "dma_val", "name")

    def __init__(self, eng, fn, name=""):
        self.eng = eng
        self.fn = fn
        self.deps = set()
        self.sig = None
        self.dma_sem = None
        self.dma_val = None
        self.name = name


class Prog:
    def __init__(self, nc):
        self.nc = nc
        self.ops = {e: [] for e in ENGS}
        self.all_ops = []
        self.last_writer = {}
        self.readers = {}
        self.dma_sems = {}
        self.barrier_deps = []

    def op(self, eng, fn, reads=(), writes=(), name="", dma=None, dma_total=False):
        o = Op(eng, fn, name)
        if dma is not None:
            ent = self.dma_sems.setdefault(dma, [eng, 0, dma_total])
            assert ent[0] == eng
            ent[1] += 16
            o.dma_sem = dma
            o.dma_val = ent[1]
        reads = list(reads)
        writes = list(writes)
        ps_reads = [k for k in reads if isinstance(k, tuple) and len(k) == 2 and k[0] == "ps"]
        if ps_reads:
            reads = [k for k in reads if k not in ps_reads]
            writes = writes + [("psr", k[1]) for k in ps_reads]
            for k in ps_reads:
                w = self.last_writer.get(k)
                if w is not None:
                    o.deps.add(w)
                self.readers.setdefault(k, []).append(o)
        for k in reads:
            w = self.last_writer.get(k)
            if w is not None:
                o.deps.add(w)
            self.readers.setdefault(k, []).append(o)
        for k in writes:
            w = self.last_writer.get(k)
            if w is not None:
                o.deps.add(w)
            for r in self.readers.get(k, ()):
                o.deps.add(r)
            self.readers[k] = []
            self.last_writer[k] = o
        for b in self.barrier_deps:
            o.deps.add(b)
        o.deps.discard(o)
        self.ops[eng].append(o)
        self.all_ops.append(o)
        return o

    def capture(self, fn):
        rec = []
        real = self.op
        self.op = lambda *a, **k: rec.append((a, k))
        try:
            fn()
        finally:
            self.op = real
        return rec

    def replay_merged(self, streams, weights=None):
        weights = weights or [1] * len(streams)
        idx = [0] * len(streams)
        while any(idx[i] < len(s) for i, s in enumerate(streams)):
            for i, s in enumerate(streams):
                for _ in range(weights[i]):
                    if idx[i] < len(s):
                        a, k = s[idx[i]]
                        idx[i] += 1
                        self.op(*a, **k)

    def barrier(self):
        deps = [self.ops[e][-1] for e in ENGS if self.ops[e]]
        last_dma = {}
        for o in self.all_ops:
            if o.dma_sem is not None:
                last_dma[o.dma_sem] = o
        self.barrier_deps = deps + list(last_dma.values())

    def emit(self):
        nc = self.nc
        referenced = set()
        for o in self.all_ops:
            for d in o.deps:
                if d.dma_sem is None and not (d.eng == o.eng == "pe"):
                    referenced.add(d)
        for e in ENGS:
            c = 0
            for o in self.ops[e]:
                if o in referenced:
                    c += 1
                    o.sig = c
        with contextlib.ExitStack() as st:
            esem = {e: st.enter_context(nc.semaphore("S_" + e)) for e in ENGS}
            dsem = {n: st.enter_context(nc.semaphore("D_" + n)) for n in self.dma_sems}
            block = st.enter_context(nc.Block())

            def run(e, eng):
                waited = {}
                for o in self.ops[e]:
                    need = {}
                    for d in o.deps:
                        if d.dma_sem is not None:
                            key = ("d", d.dma_sem)
                            val = self.dma_sems[d.dma_sem][1] if self.dma_sems[d.dma_sem][2] else d.dma_val
                        else:
                            if d.eng == e and e == "pe":
                                continue
                            if d.sig is None:
                                continue
                            key = ("e", d.eng)
                            val = d.sig
                        if waited.get(key, 0) >= val:
                            continue
                        if need.get(key, 0) < val:
                            need[key] = val
                    items = list(need.items())
                    for key, val in items:
                        waited[key] = val
                    sems = [(dsem[k[1]] if k[0] == "d" else esem[k[1]], v) for k, v in items]
                    for s_, v in sems[:-1]:
                        eng.wait_ge(s_, v)
                    r = o.fn(eng)
                    first, last = r if isinstance(r, tuple) else (r, r)
                    if sems:
                        s_, v = sems[-1]
                        first._wait_ge(s_, v)
                    if o.dma_sem is not None:
                        last.then_inc(dsem[o.dma_sem], 16)
                    elif o.sig is not None:
                        last.then_inc(esem[e], 1)
                for n, (owner, cnt, _tot) in self.dma_sems.items():
                    if owner == e:
                        eng.wait_ge(dsem[n], cnt)

            @block.tensor
            def _(eng):
                run("pe", eng)

            @block.scalar
            def _(eng):
                run("act", eng)

            @block.vector
            def _(eng):
                run("dve", eng)

            @block.gpsimd
            def _(eng):
                run("pool", eng)

            @block.sync
            def _(eng):
                run("sp", eng)


class Arena:
    def __init__(self, nc, base=16512, top=229344):
        self.nc = nc
        self.cur = base
        self.top = top
        self.n = 0

    def alloc(self, name, shape, dtype):
        sz = int(np.prod(shape[1:])) * mybir.dt.size(dtype)
        off = (self.cur + 31) // 32 * 32
        assert off + sz <= self.top, f"SBUF arena overflow at {name}: need {off + sz - self.top} more bytes"
        self.cur = off + sz
        self.n += 1
        return self.nc.alloc_sbuf_tensor_at(f"{name}_{self.n}", list(shape), dtype, offset=off)

    def mark(self):
        return self.cur

    def reset(self, m):
        self.cur = m


def AP(t, off, dims):
    return bass.AP(t, off, [list(d) for d in dims])


def host_constants():
    c = {}
    I = np.eye(128, dtype=np.float32)
    c["c_identb"] = I.astype(ml_dtypes.bfloat16)
    c["c_identf"] = I
    c["c_onesb"] = np.ones((128, 128), dtype=ml_dtypes.bfloat16)
    s_ = np.arange(128)[:, None]
    t_ = np.arange(128)[None, :]
    tri = (s_ <= t_).astype(np.float32)
    c["c_maskf"] = (tri * SC_M).astype(np.float32)
    c["c_maskb"] = tri.astype(ml_dtypes.bfloat16)
    perm = np.zeros((128, 128), dtype=np.float32)
    for r in range(128):
        i = r % 64
        if i < 32:
            perm[r + 32, r] = -1.0
        else:
            perm[r - 32, r] = 1.0
    c["c_perm"] = perm.astype(ml_dtypes.bfloat16)
    pos = np.arange(S, dtype=np.float32)
    inv = (np.float32(10000.0) ** (-np.arange(0, 64, 2, dtype=np.float32) / np.float32(64))).astype(np.float32)
    ang = (pos[:, None] * inv[None, :]).astype(np.float32)
    cosr = np.cos(ang.astype(np.float64)).T
    sinr = np.sin(ang.astype(np.float64)).T
    f_of_row = np.arange(128) % 32
    c["c_cos"] = cosr[f_of_row].astype(np.float32)
    c["c_sin"] = sinr[f_of_row].astype(np.float32)
    q = np.arange(64)
    jq, hq = q // 4, q % 4
    c["c_mcarry"] = ((hq[:, None] == hq[None, :]) & (jq[:, None] < jq[None, :])).astype(ml_dtypes.bfloat16)
    sel = (hq[:, None] == np.arange(4)[None, :]).astype(np.float32)
    c["c_sel"] = sel.astype(ml_dtypes.bfloat16)
    c["c_ohj"] = (jq[:, None] == np.arange(16)[None, :]).astype(np.float32)
    c["c_selT"] = np.ascontiguousarray(sel.T).astype(ml_dtypes.bfloat16)
    return c


CONST_SPECS = [("c_identb", [128, 128], BF16), ("c_identf", [128, 128], F32), ("c_onesb", [128, 128], BF16),
               ("c_maskf", [128, 128], F32), ("c_maskb", [128, 128], BF16), ("c_perm", [128, 128], BF16),
               ("c_cos", [128, S], F32), ("c_sin", [128, S], F32), ("c_mcarry", [64, 64], BF16),
               ("c_sel", [64, 4], BF16), ("c_ohj", [64, 16], F32), ("c_selT", [4, 64], BF16)]

PARAM_SPECS = [("g_mix", [DEPTH, D]), ("w_in", [DEPTH, D, INW]), ("conv_w", [DEPTH, 4, D]), ("conv_b", [DEPTH, D]),
               ("b_gates", [DEPTH, 8]), ("g_mlstm_head", [DEPTH, 512]), ("lam_q1", [DEPTH, 64]),
               ("lam_k1", [DEPTH, 64]), ("lam_q2", [DEPTH, 64]), ("lam_k2", [DEPTH, 64]),
               ("g_diff_head", [DEPTH, 512]), ("w_out", [DEPTH, D, D]), ("g_ffn", [DEPTH, D]),
               ("w_gate", [DEPTH, D, FF]), ("w_up", [DEPTH, D, FF]), ("w_down", [DEPTH, FF, D]), ("g_final", [D])]


class Builder:
    def __init__(self, layers, nseq=2, final_norm=True, debug=None, pdepth=DEPTH):
        self.layers = list(layers)
        self.nseq = nseq
        self.final_norm = final_norm
        self.debug = {}
        self.debug_names = set(debug or [])
        nc = bass.Bass("TRN2", target_bir_lowering=False)
        self.nc = nc
        self.P = Prog(nc)
        self.ar = Arena(nc)
        self.din = {}
        self.din["x"] = nc.dram_tensor("x", [nseq, S, D], F32, kind="ExternalInput").ap()
        self.pdepth = pdepth
        for n, shp in PARAM_SPECS:
            shp = list(shp)
            if n != "g_final":
                shp[0] = pdepth
            self.din[n] = nc.dram_tensor(n, shp, F32, kind="ExternalInput").ap()
        for n, shp, dt_ in CONST_SPECS:
            self.din[n] = nc.dram_tensor(n, list(shp), dt_, kind="ExternalInput").ap()
        self.out = nc.dram_tensor("out", [nseq, S, D], F32, kind="ExternalOutput").ap()
        self.dbg_out = {}
        for n, shp in self.debug.items():
            self.dbg_out[n] = nc.dram_tensor(n, list(shp), F32, kind="ExternalOutput").ap()
        self.ps = nc.alloc_psum_tensor("ps", [128, 8, 512], F32)
        self.wq_n = 0
        self.uid = 0
        self.att_level = 9

    def dump(self, name, t, keys):
        if name not in self.debug_names:
            return
        shp = [int(v) for v in t.shape]
        dt_ = t.dtype
        d = self.nc.dram_tensor("dbg_" + name, shp, dt_, kind="ExternalOutput").ap()
        self.P.op("sp", lambda e: e.dma_start(out=d, in_=t[:]), reads=keys, name="dump", dma="dbg_" + name)

    def bank(self, b):
        return ("ps", b)

    def mm(self, out_ap, pairs, reads, writes, name="mm", start=True, stop=True, skip=False):
        n = len(pairs)

        def fn(e):
            first = last = None
            for i, (l, r) in enumerate(pairs):
                ins = e.matmul(out_ap, l, r, start=(start and i == 0), stop=(stop and i == n - 1),
                               skip_group_check=skip)
                if first is None:
                    first = ins
                last = ins
            return first, last
        return self.P.op("pe", fn, reads=reads, writes=writes, name=name)

    def transposes(self, items, reads, writes, name="tr"):
        def fn(e):
            first = last = None
            for (o, i, idn) in items:
                ins = e.transpose(out=o, in_=i, identity=idn)
                if first is None:
                    first = ins
                last = ins
            return first, last
        return self.P.op("pe", fn, reads=reads, writes=writes, name=name)

    def wload(self, dst_ap, src_ap, keys, sem, name="wload", total=False):
        if not isinstance(keys, list):
            keys = [keys]

        def fn(e):
            return e.dma_start(out=dst_ap, in_=src_ap)
        return self.P.op("pool", fn, writes=keys, name=name, dma=sem, dma_total=total)

    def sload(self, dst_ap, src_ap, key, sem="setup", name="sload", nonc=False, total=True):
        def fn(e):
            if nonc:
                return e.dma_start(out=dst_ap, in_=src_ap, allow_slow_non_contiguous=True)
            return e.dma_start(out=dst_ap, in_=src_ap)
        return self.P.op("sp", fn, writes=[key], name=name, dma=sem, dma_total=total)

    def setup(self):
        ar, P, din = self.ar, self.P, self.din
        self.xT = ar.alloc("xT", [128, KC, S], F32)
        self.hT = ar.alloc("hT", [128, KC, S], BF16)
        self.c = {}
        for n, shp, dt_ in CONST_SPECS:
            if n in ("c_cos", "c_sin"):
                t = ar.alloc(n, shp, BF16)
                self.c[n] = t
                continue
            t = ar.alloc(n, shp, dt_)
            self.c[n] = t
            self.sload(t[:], din[n], key=n)
        self.g1 = ar.alloc("g1", [128, DEPTH, KC], F32)
        self.g2 = ar.alloc("g2", [128, DEPTH, KC], F32)
        self.gf = ar.alloc("gf", [128, KC], F32)
        self.cw = ar.alloc("cw", [128, DEPTH, 4, KC], F32)
        self.cb = ar.alloc("cb", [128, DEPTH, KC], F32)
        self.bg = ar.alloc("bg", [128, DEPTH, 8], F32)
        self.lamv = ar.alloc("lamv", [128, 4, DEPTH, 64], F32)
        for l in range(self.pdepth):
            self.sload(self.g1[:, l, :], din["g_mix"][l].rearrange("(c p) -> p c", p=128), key=("g1", l), nonc=True)
            self.sload(self.g2[:, l, :], din["g_ffn"][l].rearrange("(c p) -> p c", p=128), key=("g2", l), nonc=True)
            for j in range(4):
                self.sload(self.cw[:, l, j, :], din["conv_w"][l, j].rearrange("(c p) -> p c", p=128), key=("cw", l, j), nonc=True)
            self.sload(self.cb[:, l, :], din["conv_b"][l].rearrange("(c p) -> p c", p=128), key=("cb", l), nonc=True)
        self.sload(self.gf[:], din["g_final"].rearrange("(c p) -> p c", p=128), key="gf", nonc=True)
        bgd = din["b_gates"]
        self.sload(self.bg[:, 0:self.pdepth, :], AP(bgd.tensor, 0, [[0, 128], [8, self.pdepth], [1, 8]]), key="bg")
        for i, n in enumerate(("lam_q1", "lam_k1", "lam_q2", "lam_k2")):
            self.sload(self.lamv[:, i, 0:self.pdepth, :], AP(din[n].tensor, 0, [[0, 128], [64, self.pdepth], [1, 64]]), key=("lamv", i))
        self.wload(self.c["c_cos"][:], din["c_cos"], "c_cos", "wsetup", total=True)
        self.wload(self.c["c_sin"][:], din["c_sin"], "c_sin", "wsetup", total=True)
        self.gm_bc = ar.alloc("gm_bc", [128, 512], F32)
        self.gd_bc = ar.alloc("gd_bc", [128, 512], F32)
        self.lam_s = ar.alloc("lam_s", [128, 8], F32)
        self.lam_t = ar.alloc("lam_t", [128, 2, 64], F32)
        self.phase_base = ar.mark()

    def load_x(self, sq):
        P, ar = self.P, self.ar
        m = ar.mark()
        xin = [ar.alloc("xin", [128, D], F32) for _ in range(2)]
        xd = self.din["x"]
        for b in range(NB):
            t = xin[b % 2]
            self.sload(t[:], xd[sq, b * 128:(b + 1) * 128, :], key=("xin", b % 2), sem="xin%d" % (b % 2), total=False)
            for half in range(2):
                bk = (2 * b + half) % 8
                items = [(self.ps[:, bk, cc * 128:(cc + 1) * 128], t[:, (half * 4 + cc) * 128:(half * 4 + cc + 1) * 128],
                          self.c["c_identf"][:]) for cc in range(4)]
                self.transposes(items, reads=[("xin", b % 2), "c_identf"], writes=[self.bank(bk)], name="xtr")
                src = AP(self.ps, bk * 512, [[4096, 128], [128, 4], [1, 128]])
                dst = self.xT[:, half * 4:(half + 1) * 4, b * 128:(b + 1) * 128]
                eng = "dve" if half == 0 else "act"
                if eng == "dve":
                    P.op("dve", (lambda d_, s_: lambda e: e.tensor_copy(out=d_, in_=s_))(dst, src),
                         reads=[self.bank(bk)], writes=[("xT", b // 4)])
                else:
                    P.op("act", (lambda d_, s_: lambda e: e.activation(out=d_, in_=s_, func=AF.Copy))(dst, src),
                         reads=[self.bank(bk)], writes=[("xT", b // 4)])
        P.barrier()
        ar.reset(m)

    def norm(self, gt_ap_fn, gkey, out_f32=None, tts=None):
        P, ar = self.P, self.ar
        m = ar.mark()
        sq = [ar.alloc("sq", [128, KC, TT], BF16) for _ in range(2)]
        rs = [ar.alloc("rs", [128, TT], F32) for _ in range(2)]
        for tt in (range(NT) if tts is None else tts):
            i = tt % 2
            tsl = slice(tt * TT, (tt + 1) * TT)
            P.op("act", (lambda o_, i_: lambda e: e.activation(out=o_, in_=i_, func=AF.Square))(sq[i][:], self.xT[:, :, tsl]),
                 reads=[("xT", tt)], writes=[("sq", i)])
            bk = i
            self.mm(self.ps[:, bk, :], [(self.c["c_onesb"][:], sq[i][:, c, :]) for c in range(KC)],
                    reads=[("sq", i), "c_onesb"], writes=[self.bank(bk)], name="ssq")
            P.op("act", (lambda o_, i_: lambda e: e.activation(out=o_, in_=i_, func=AF.Ln, scale=1.0 / D, bias=EPS))(rs[i][:], self.ps[:, bk, :]),
                 reads=[self.bank(bk)], writes=[("rs", i)])
            P.op("act", (lambda o_: lambda e: e.activation(out=o_, in_=o_, func=AF.Exp, scale=-0.5))(rs[i][:]),
                 reads=[("rs", i)], writes=[("rs", i)])
            for c in range(KC):
                if out_f32 is None:
                    dst = self.hT[:, c, tsl]
                    wk = ("hT", tt)
                else:
                    dst, wk = out_f32(tt, c)
                P.op("dve", (lambda d_, x_, g_, r_: lambda e: e.scalar_tensor_tensor(out=d_, in0=x_, scalar=g_, in1=r_, op0=ALU.mult, op1=ALU.mult))(
                    dst, self.xT[:, c, tsl], gt_ap_fn(c), rs[i][:]),
                    reads=[("xT", tt), ("rs", i), gkey], writes=[wk])
        ar.reset(m)

    def dve(self, fn, reads, writes, name="dve"):
        return self.P.op("dve", fn, reads=reads, writes=writes, name=name)

    def act(self, out, in_, func, reads, writes, scale=1.0, bias=0.0, accum=None, name="act"):
        def fn(e):
            kw = {}
            if accum is not None:
                kw["accum_out"] = accum
            return e.activation(out=out, in_=in_, func=func, bias=bias, scale=scale, **kw)
        return self.P.op("act", fn, reads=reads, writes=writes, name=name)

    def evac(self, k, out, in_, reads, writes):
        if k % 2 == 0:
            return self.act(out, in_, AF.Copy, reads, writes, name="evac")
        return self.dve(lambda e: e.tensor_copy(out=out, in_=in_), reads, writes, name="evac")

    def rsqrt_small(self, t_ap, key, bias):
        self.act(t_ap, t_ap, AF.Ln, [key], [key], bias=bias)
        self.act(t_ap, t_ap, AF.Exp, [key], [key], scale=-0.5)

    def split3(self, dst, src, np_, n, skey, dkey):
        ar = self.ar
        r1 = ar.alloc("sp_r1", [np_, n], F32)
        r2 = ar.alloc("sp_r2", [np_, n], F32)
        self.uid += 1
        k1, k2 = ("sp_r1", self.uid), ("sp_r2", self.uid)
        self.dve(lambda e: e.tensor_copy(out=dst[:, 0, :], in_=src), [skey], [(dkey, 0)])
        self.dve(lambda e: e.tensor_tensor(out=r1[:], in0=src, in1=dst[:, 0, :], op=ALU.subtract), [skey, (dkey, 0)], [k1])
        self.dve(lambda e: e.tensor_copy(out=dst[:, 1, :], in_=r1[:]), [k1], [(dkey, 1)])
        self.dve(lambda e: e.tensor_tensor(out=r2[:], in0=r1[:], in1=dst[:, 1, :], op=ALU.subtract), [k1, (dkey, 1)], [k2])
        self.dve(lambda e: e.tensor_copy(out=dst[:, 2, :], in_=r2[:]), [k2], [(dkey, 2)])
        return [(dkey, i) for i in range(3)]

    def layer_params(self, l):
        P, din = self.P, self.din
        lam_init = 0.8 - 0.6 * math.exp(-0.3 * l)
        self.sload(self.gm_bc[:], AP(din["g_mlstm_head"].tensor, l * 512, [[0, 128], [1, 512]]), key="gm_bc", sem="gm_bc", total=False)
        self.sload(self.gd_bc[:], AP(din["g_diff_head"].tensor, l * 512, [[0, 128], [1, 512]]), key="gd_bc", sem="gd_bc", total=False)
        gm, gd = self.gm_bc, self.gd_bc
        self.dve(lambda e: e.tensor_scalar(out=gm[:], in0=gm[:], scalar1=0.5, scalar2=None, op0=ALU.mult), ["gm_bc"], ["gm_bc"])
        self.dve(lambda e: e.tensor_scalar(out=gd[:], in0=gd[:], scalar1=1.0 - lam_init, scalar2=None, op0=ALU.mult), ["gd_bc"], ["gd_bc"])
        lv, lt, ls = self.lamv, self.lam_t, self.lam_s
        for i in range(2):
            self.dve((lambda i: lambda e: e.tensor_tensor(out=lt[:, i, :], in0=lv[:, 2 * i, l, :], in1=lv[:, 2 * i + 1, l, :], op=ALU.mult))(i),
                     [("lamv", 2 * i), ("lamv", 2 * i + 1)], ["lam_t"])
        self.dve(lambda e: e.tensor_reduce(out=ls[:, 0:2], in_=lt[:], axis=AX.X, op=ALU.add), ["lam_t"], ["lam_s"])
        self.act(ls[:, 2:4], ls[:, 0:2], AF.Exp, ["lam_s"], ["lam_s"])
        self.dve(lambda e: e.tensor_tensor(out=ls[:, 4:5], in0=ls[:, 3:4], in1=ls[:, 2:3], op=ALU.subtract), ["lam_s"], ["lam_s"])
        self.dve(lambda e: e.tensor_scalar(out=ls[:, 5:6], in0=ls[:, 4:5], scalar1=-lam_init, scalar2=None, op0=ALU.add), ["lam_s"], ["nlam"])

    def mixer_alloc(self):
        ar = self.ar
        a = {}
        a["mixT"] = ar.alloc("mixT", [128, 4, S], BF16)
        a["vfam"] = ar.alloc("vfam", [128, NB, 4, 129], BF16)
        off0 = (ar.cur + 31) // 32 * 32
        a["slotA"] = ar.alloc("slotA", [128, KC, 512], BF16)
        off1 = (ar.cur + 31) // 32 * 32
        a["slotB"] = ar.alloc("slotB", [128, KC, 512], BF16)
        a["woA"] = self.nc.alloc_sbuf_tensor_at("woA_%d" % ar.n, [128, 4, D], BF16, offset=off0)
        a["woB"] = self.nc.alloc_sbuf_tensor_at("woB_%d" % ar.n, [128, 4, D], BF16, offset=off1)
        a["qT"] = ar.alloc("qT", [128, S], BF16)
        a["kT"] = ar.alloc("kT", [128, S], BF16)
        a["wg"] = ar.alloc("wg", [128, KC, 8], BF16)
        a["gt"] = ar.alloc("gt", [128, 5, 64], F32)
        return a

    def gates(self, l, a):
        P, ar, ps, c = self.P, self.ar, self.ps, self.c
        m = ar.mark()
        wg, gt = a["wg"], a["gt"]
        wgf = ar.alloc("wgf", [128, KC, 8], F32)
        self.sload(wgf[:], self.din["w_in"][l][:, G0:G0 + 8].rearrange("(k p) n -> p k n", p=128), key="wgf", sem="wgf", total=False)
        self.dve(lambda e: e.tensor_copy(out=wg[:], in_=wgf[:]), ["wgf"], ["wg"])
        hT = self.hT

        def fn(e):
            first = last = None
            for j in range(NB):
                for k in range(KC):
                    ins = e.matmul(ps[:, 0, j * 8:(j + 1) * 8], hT[:, k, j * 128:(j + 1) * 128], wg[:, k, :],
                                   start=(k == 0), stop=(k == KC - 1))
                    if first is None:
                        first = ins
                    last = ins
            return first, last
        P.op("pe", fn, reads=["wg"] + [("hT", t) for t in range(NT)], writes=[self.bank(0)], name="gates_mm")
        g_tm = ar.alloc("g_tm", [128, 2, 64], F32)
        bg = self.bg
        self.dve(lambda e: e.tensor_tensor(out=AP(g_tm, 0, [[128, 128], [4, NB], [64, 2], [1, 4]]),
                                           in0=AP(ps, 0 * 512, [[4096, 128], [8, NB], [4, 2], [1, 4]]),
                                           in1=AP(bg, l * 8, [[DEPTH * 8, 128], [0, NB], [4, 2], [1, 4]]), op=ALU.add),
                 [self.bank(0), "bg"], ["g_tm"])
        idf = c["c_identf"]
        self.transposes([(ps[0:64, 1, 0:128], g_tm[:, 0, :], idf[:]),
                         (ps[0:64, 1, 128:256], g_tm[:, 1, :], idf[:])],
                        reads=["g_tm", "c_identf"], writes=[self.bank(1)], name="gates_tr")
        T = lambda n, w=128: ar.alloc(n, [64, w], F32)
        ef, lsp, zer, cs, carry, Bn, A, cm, cmxJ = T("ef"), T("lsp"), T("zer"), T("cs"), T("carry", 1), T("Bn"), T("A"), T("cm"), T("cmxJ", 16)
        self.act(ef[:], ps[0:64, 1, 128:256], AF.Exp, [self.bank(1)], ["ef"], scale=-1.0)
        self.act(lsp[:], ef[:], AF.Ln, ["ef"], ["lsp"], bias=1.0)
        self.dve(lambda e: e.memset(zer[:], 0.0), [], ["zer"])
        self.dve(lambda e: e.tensor_tensor_scan(out=cs[:], data0=lsp[:], data1=zer[:], initial=0.0, op0=ALU.add, op1=ALU.add),
                 ["lsp", "zer"], ["cs"])
        tot3 = ar.alloc("tot3", [64, 3, 2], BF16)
        k3 = self.split3(tot3, cs[:, 126:128], 64, 2, "cs", "tot3")
        self.mm(ps[0:64, 2, 0:2], [(c["c_mcarry"][:], tot3[:, i, :]) for i in range(3)], reads=k3 + ["c_mcarry"], writes=[self.bank(2)], name="carry")
        self.dve(lambda e: e.tensor_copy(out=carry[:], in_=ps[0:64, 2, 1:2]), [self.bank(2)], ["carry"])
        self.dve(lambda e: e.tensor_scalar(out=Bn[:], in0=cs[:], scalar1=carry[:], scalar2=None, op0=ALU.add), ["cs", "carry"], ["Bn"])
        self.dve(lambda e: e.tensor_tensor(out=A[:], in0=ps[0:64, 1, 0:128], in1=Bn[:], op=ALU.add), [self.bank(1), "Bn"], ["A"])
        self.dve(lambda e: e.tensor_tensor_scan(out=cm[:], data0=A[:], data1=A[:], initial=0.0, op0=ALU.max, op1=ALU.max), ["A"], ["cm"])
        self.dve(lambda e: e.tensor_scalar(out=cmxJ[:], in0=c["c_ohj"][:], scalar1=cm[:, 127:128], scalar2=None, op0=ALU.mult),
                 ["cm", "c_ohj"], ["cmxJ"])
        cmx3 = ar.alloc("cmx3", [64, 3, 16], BF16)
        k3 = self.split3(cmx3, cmxJ[:], 64, 16, "cmxJ", "cmx3")
        self.mm(ps[0:4, 2, 8:24], [(c["c_sel"][:], cmx3[:, i, :]) for i in range(3)], reads=k3 + ["c_sel"], writes=[self.bank(2)], name="hj")
        hj = ar.alloc("hj", [4, 16], F32)
        Mp = ar.alloc("Mp", [4, 17], F32)
        self.dve(lambda e: e.tensor_copy(out=hj[:], in_=ps[0:4, 2, 8:24]), [self.bank(2)], ["hj"])
        self.dve(lambda e: e.memset(Mp[:], 0.0), [], ["Mp"])
        self.dve(lambda e: e.tensor_tensor_scan(out=Mp[:, 1:17], data0=hj[:], data1=hj[:], initial=0.0, op0=ALU.max, op1=ALU.max),
                 ["hj", "Mp"], ["Mp"])

        Mp3 = ar.alloc("Mp3", [4, 3, 18], BF16)
        self.dve(lambda e: e.memset(Mp3[:], 0.0), [], [("Mp3", i) for i in range(3)])
        Mp3v = AP(Mp3, 0, [[54, 4], [18, 3], [1, 17]])
        k3 = self.split3(Mp3v, Mp[:], 4, 17, "Mp", "Mp3")

        def fn2(e):
            first = last = None
            for (c0, o0) in ((0, 0), (1, 16)):
                for i in range(3):
                    ins = e.matmul(ps[0:64, 3, o0:o0 + 16], c["c_selT"][:], Mp3[:, i, c0:c0 + 16], start=(i == 0), stop=(i == 2))
                    if first is None:
                        first = ins
                    last = ins
            return first, last
        P.op("pe", fn2, reads=k3 + ["c_selT"], writes=[self.bank(3)], name="mq")
        tmp2 = ar.alloc("tmp2", [64, 2, 16], F32)
        mpe = ar.alloc("mpe", [64, 2], F32)
        nb = ar.alloc("nb", [64, 4], F32)
        ohj = c["c_ohj"]
        self.dve(lambda e: e.tensor_tensor(out=tmp2[:], in0=AP(ps, 3 * 512, [[4096, 64], [16, 2], [1, 16]]),
                                           in1=AP(ohj, 0, [[16, 64], [0, 2], [1, 16]]), op=ALU.mult),
                 [self.bank(3), "c_ohj"], ["tmp2"])
        self.dve(lambda e: e.tensor_reduce(out=mpe[:], in_=tmp2[:], axis=AX.X, op=ALU.add), ["tmp2"], ["mpe"])
        Mg = T("Mg")
        self.dve(lambda e: e.tensor_scalar(out=Mg[:], in0=cm[:], scalar1=mpe[:, 0:1], scalar2=None, op0=ALU.max), ["cm", "mpe"], ["Mg"])
        self.dve(lambda e: e.tensor_scalar(out=nb[:, 0:1], in0=mpe[:, 0:1], scalar1=-1.0, scalar2=None, op0=ALU.mult), ["mpe"], ["nb"])
        self.dve(lambda e: e.tensor_scalar(out=nb[:, 1:2], in0=mpe[:, 1:2], scalar1=-1.0, scalar2=math.log(SC_M), op0=ALU.mult, op1=ALU.add), ["mpe"], ["nb"])
        self.dve(lambda e: e.tensor_tensor(out=nb[:, 2:3], in0=mpe[:, 0:1], in1=mpe[:, 1:2], op=ALU.subtract), ["mpe"], ["nb"])
        u, ws, w, fl, d2, dec, ddg = T("u"), T("ws"), T("w"), T("fl"), T("d2"), T("dec", 1), T("ddg", 64)
        self.act(u[:], A[:], AF.Exp, ["A", "nb"], ["u"], bias=nb[:, 0:1])
        self.act(ws[:], A[:], AF.Exp, ["A", "nb"], ["ws"], bias=nb[:, 1:2])
        self.act(w[:], Mg[:], AF.Exp, ["Mg", "mpe"], ["w"], scale=-1.0, bias=mpe[:, 0:1])
        self.dve(lambda e: e.tensor_tensor(out=d2[:], in0=Bn[:], in1=Mg[:], op=ALU.subtract), ["Bn", "Mg"], ["d2"])
        self.act(fl[:], d2[:], AF.Exp, ["d2"], ["fl"])
        self.act(dec[:], nb[:, 2:3], AF.Exp, ["nb"], ["dec"])
        dec3 = ar.alloc("dec3", [64, 3, 2], BF16)
        decf = ar.alloc("decf", [64, 2], F32)
        self.dve(lambda e: e.tensor_copy(out=decf[:], in_=AP(dec, 0, [[1, 64], [0, 2]])), ["dec"], ["decf"])
        k3d = self.split3(dec3, decf[:], 64, 2, "decf", "dec3")
        ddg3 = ar.alloc("ddg3", [64, 3, 64], BF16)
        decs = ar.alloc("decs", [64, 4], F32)
        self.dve(lambda e: e.tensor_copy(out=decs[:, 0:3], in_=dec3[:, :, 0]), k3d, ["decs"])
        for i in range(3):
            self.dve((lambda i: lambda e: e.tensor_scalar(out=ddg3[:, i, :], in0=c["c_identb"][0:64, 0:64], scalar1=decs[:, i:i + 1], scalar2=None, op0=ALU.mult))(i),
                     ["decs", "c_identb"], [("ddg3", i)])

        def fn3(e):
            first = None
            for i, arr in enumerate((u, ws, w, fl)):
                ins = e.transpose(out=ps[:, 4, i * 64:(i + 1) * 64], in_=arr[:], identity=idf[0:64, 0:64])
                if first is None:
                    first = ins
            for i in range(3):
                last = e.matmul(ps[:, 4, 256:320], c["c_onesb"][0:64, :], ddg3[:, i, :], start=(i == 0), stop=(i == 2))
            return first, last
        P.op("pe", fn3, reads=["u", "ws", "w", "fl", "c_onesb", "c_identf"] + [("ddg3", i) for i in range(3)], writes=[self.bank(4)], name="gt_tr")
        self.dve(lambda e: e.tensor_copy(out=gt[:], in_=AP(ps, 4 * 512, [[4096, 128], [64, 5], [1, 64]])), [self.bank(4)], ["gt"])

    def vfam_project(self, l, a, col0, slot, slotkey, banks=(0, 1, 2, 3)):
        P, ps = self.P, self.ps
        vf, hT = a["vfam"], self.hT
        self.wload(slot[:], self.din["w_in"][l][:, col0:col0 + 512].rearrange("(k p) n -> p k n", p=128),
                   [(slotkey, i) for i in range(3)], "s%d_0" % slotkey[1])
        slotkey = (slotkey, 0)
        for j in range(NB):
            bk = banks[j % len(banks)]
            self.mm(ps[:, bk, :], [(hT[:, k, j * 128:(j + 1) * 128], slot[:, k, :]) for k in range(KC)],
                    reads=[slotkey, ("hT", j // 4)], writes=[self.bank(bk)], name="vproj")
            dst = AP(vf, j * 4 * 129, [[NB * 4 * 129, 128], [129, 4], [1, 128]])
            src = AP(ps, bk * 512, [[4096, 128], [128, 4], [1, 128]])
            self.evac(j, dst, src, [self.bank(bk)], [("vfam", j)])

    def mlstm(self, l, a):
        P, ar, ps, c = self.P, self.ar, self.ps, self.c
        hT, mixT, vf, gt = self.hT, a["mixT"], a["vfam"], a["gt"]
        psb = [ps[:, b, :].bitcast(BF16) for b in range(8)]
        m_g = ar.mark()
        gates_ops = P.capture(lambda: self.gates(l, a))
        g_end = ar.mark()
        ar.reset(m_g)
        accs = ar.alloc("accs", [128, NB, 129], F32)
        CTf = ar.alloc("CTf", [128, 129], F32)
        CTb = ar.alloc("CTb", [128, 129], BF16)
        scT = [ar.alloc("scT", [128, 128], BF16) for _ in range(2)]
        junk = ar.alloc("junk", [128, 128], BF16)
        sm = ar.alloc("sm", [128, 6, NB], F32)
        hn4 = [ar.alloc("hn4", [128, 4, 128], BF16) for _ in range(2)]
        if ar.cur < g_end:
            ar.cur = g_end
        ubuf = ar.alloc("ubuf", [128, 3 + S], BF16)
        diag = ar.alloc("diag", [128, 4, 128], BF16)
        ktok = [ar.alloc("ktok", [128, NB, 128], BF16) for _ in range(2)]
        qTs = [a["qT"], ar.alloc("qT2", [128, S], BF16)]
        kTs = [a["kT"], ar.alloc("kT2", [128, S], BF16)]
        slots = [(a["slotA"], ("slot", 0)), (a["slotB"], ("slot", 1))]
        PB = (5, 6, 7)
        w_in = self.din["w_in"][l]
        cw, cb = self.cw, self.cb

        def vpart():
            self.dve(lambda e: e.memset(ubuf[:, 0:3], 0.0), [], ["ubuf"])
            self.dve(lambda e: e.memset(AP(vf, 128, [[NB * 4 * 129, 128], [129, NB * 4]]), 1.0), [], [("vfam", j) for j in range(NB)])
            self.vfam_project(l, a, VM0, *slots[0], banks=PB)

        def proj(h):
            hb = h % 2
            qT, kT = qTs[hb], kTs[hb]
            slot, sk = slots[(h + 1) % 2]
            nb = [0]

            def nbk():
                nb[0] += 1
                return PB[nb[0] % 3]
            for i, c0 in enumerate((QM0 + 128 * h, KM0 + 128 * h, OM0 + 128 * h)):
                self.wload(slot[:, :, i * 128:(i + 1) * 128], w_in[:, c0:c0 + 128].rearrange("(k p) n -> p k n", p=128), (sk, i), "s%d_%d" % (sk[1], i))
            for tt in range(NT):
                tsl = slice(tt * TT, (tt + 1) * TT)
                bk = nbk()
                self.mm(ps[:, bk, :], [(slot[:, k, 256:384], hT[:, k, tsl]) for k in range(KC)],
                        reads=[(sk, 2), ("hT", tt)], writes=[self.bank(bk)], name="oproj")
                self.act(mixT[:, h, tsl], ps[:, bk, :], AF.Tanh, [self.bank(bk)], [("mixT", h, tt)], scale=0.5)
            for i, dstT in enumerate((qT, kT)):
                cch = h if i == 0 else 4 + h
                for j in range(4):
                    self.dve((lambda j, cch: lambda e: e.tensor_scalar(out=diag[:, j, :], in0=c["c_identb"][:], scalar1=cw[:, l, j, cch:cch + 1],
                                                                        scalar2=None, op0=ALU.mult))(j, cch),
                             ["c_identb", ("cw", l, j)], ["diag"])
                for tt in range(NT):
                    tsl = slice(tt * TT, (tt + 1) * TT)
                    bk = nbk()
                    self.mm(ps[:, bk, :], [(slot[:, k, i * 128:(i + 1) * 128], hT[:, k, tsl]) for k in range(KC)],
                            reads=[(sk, i), ("hT", tt)], writes=[self.bank(bk)], name="qkproj")
                    self.evac(tt, ubuf[:, 3 + tt * TT:3 + (tt + 1) * TT], ps[:, bk, :], [self.bank(bk)], ["ubuf"])
                for tt in range(NT):
                    tsl = slice(tt * TT, (tt + 1) * TT)
                    bk = nbk()
                    self.mm(ps[:, bk, :], [(diag[:, j, :], ubuf[:, tt * TT + j:tt * TT + j + TT]) for j in range(4)],
                            reads=["diag", "ubuf"], writes=[self.bank(bk)], name="conv")
                    self.act(dstT[:, tsl], ps[:, bk, :], AF.Silu, [self.bank(bk), ("cb", l)], [("qk", hb, i, tt)], bias=cb[:, l, cch:cch + 1])
            for jg in range(4):
                bk = nbk()
                self.transposes([(psb[bk][:, cc * 128:(cc + 1) * 128], kT[:, (4 * jg + cc) * 128:(4 * jg + cc + 1) * 128], c["c_identb"][:]) for cc in range(4)],
                                reads=[("qk", hb, 1, jg), "c_identb"], writes=[self.bank(bk)], name="ktr")
                self.dve((lambda jg, bk, h, hb: lambda e: e.tensor_tensor(
                    out=ktok[hb][:, 4 * jg:4 * jg + 4, :], in0=psb[bk][:, 0:512].rearrange("p (c d) -> p c d", c=4),
                    in1=AP(gt, 64 + 16 * jg + h, [[320, 128], [4, 4], [0, 128]]), op=ALU.mult))(jg, bk, h, hb),
                    [self.bank(bk), "gt"], [("ktok", hb, jg)])

        def rec(h):
            hb = h % 2
            qT, kT, kt = qTs[hb], kTs[hb], ktok[hb]
            self.dve(lambda e: e.memset(CTf[:], 0.0), [], ["CTf"])
            for j in range(NB):
                bsl = slice(j * 128, (j + 1) * 128)
                i = j % 2
                b0, b1, b2 = i, 2 + i, 4
                self.mm(ps[:, b0, 0:128], [(kT[:, bsl], qT[:, bsl])], reads=[("qk", hb, 0, j // 4), ("qk", hb, 1, j // 4)],
                        writes=[self.bank(b0)], name="ST")
                self.dve((lambda i, b0, j, h: lambda e: e.scalar_tensor_tensor(out=scT[i][:], in0=ps[:, b0, 0:128], scalar=gt[:, 0, 4 * j + h:4 * j + h + 1],
                                                                               in1=c["c_maskf"][:], op0=ALU.mult, op1=ALU.mult))(i, b0, j, h),
                         [self.bank(b0), "gt", "c_maskf"], [("scT", i)])
                pairs = [(scT[i][:], vf[:, j, h, :])]
                rd = [("scT", i), ("vfam", j)]
                if j > 0:
                    pairs.append((qT[:, bsl], CTb[:]))
                    rd += [("qk", hb, 0, j // 4), "CTb"]
                self.mm(ps[:, b1, 0:129], pairs, reads=rd, writes=[self.bank(b1)], name="acc")
                if j < NB - 1:
                    self.mm(ps[:, b2, 0:129], [(kt[:, j, :], vf[:, j, h, :])], reads=[("ktok", hb, j // 4), ("vfam", j)],
                            writes=[self.bank(b2)], name="upd")
                    self.dve((lambda b2, j, h: lambda e: e.scalar_tensor_tensor(out=CTf[:], in0=CTf[:], scalar=gt[:, 4, 4 * j + h:4 * j + h + 1],
                                                                                in1=ps[:, b2, 0:129], op0=ALU.mult, op1=ALU.add))(b2, j, h),
                             [self.bank(b2), "gt", "CTf"], ["CTf"])
                    self.act(CTb[:], CTf[:], AF.Copy, ["CTf"], ["CTb"])
                self.dve((lambda b1, j: lambda e: e.tensor_copy(out=accs[:, j, :], in_=ps[:, b1, 0:129]))(b1, j),
                         [self.bank(b1)], [("accs", j)])
                self.act(junk[:], accs[:, j, 0:128], AF.Square, [("accs", j)], ["junk", ("ssq", j)], accum=sm[:, 0, j:j + 1])
            ak = [("accs", j) for j in range(NB)]
            den = AP(accs, 128, [[NB * 129, 128], [129, NB]])
            w_tm = AP(gt, 2 * 64 + h, [[320, 128], [4, NB]])
            fl_tm = AP(gt, 3 * 64 + h, [[320, 128], [4, NB]])
            s_ = lambda r: sm[:, r, :]
            self.dve(lambda e, w_tm=w_tm: e.tensor_tensor(out=s_(1), in0=den, in1=w_tm, op=ALU.mult), ak + ["gt"], ["sm1"])
            self.dve(lambda e: e.tensor_scalar(out=s_(2), in0=s_(1), scalar1=-1.0, scalar2=None, op0=ALU.mult), ["sm1"], ["sm2"])
            self.dve(lambda e: e.tensor_tensor(out=s_(2), in0=s_(2), in1=s_(1), op=ALU.max), ["sm1", "sm2"], ["sm2"])
            self.dve(lambda e, fl_tm=fl_tm: e.tensor_tensor(out=s_(2), in0=s_(2), in1=fl_tm, op=ALU.max), ["sm2", "gt"], ["sm2"])
            self.dve(lambda e: e.reciprocal(out=s_(3), in_=s_(2)), ["sm2"], ["sm3"])
            self.dve(lambda e, w_tm=w_tm: e.tensor_tensor(out=s_(3), in0=s_(3), in1=w_tm, op=ALU.mult), ["sm3", "gt"], ["sm3"])
            self.dve(lambda e: e.tensor_tensor(out=s_(4), in0=s_(0), in1=s_(3), op=ALU.mult), ["sm3"] + [("ssq", j) for j in range(NB)], ["sm4"])
            self.dve(lambda e: e.tensor_tensor(out=s_(4), in0=s_(4), in1=s_(3), op=ALU.mult), ["sm3", "sm4"], ["sm4"])
            self.dve(lambda e: e.tensor_scalar(out=s_(4), in0=s_(4), scalar1=1.0 / 128, scalar2=None, op0=ALU.mult), ["sm4"], ["sm4"])
            self.rsqrt_small(s_(4), "sm4", HEPS)
            self.dve(lambda e: e.tensor_tensor(out=s_(5), in0=s_(4), in1=s_(3), op=ALU.mult), ["sm3", "sm4"], ["sm5"])
            for jg in range(4):
                hb_ = hn4[jg % 2]
                hk = ("hn4", jg % 2)
                self.dve((lambda jg, hb_: lambda e: e.tensor_tensor(out=hb_[:], in0=accs[:, 4 * jg:4 * jg + 4, 0:128],
                                                                    in1=AP(sm, 5 * NB + 4 * jg, [[6 * NB, 128], [1, 4], [0, 128]]), op=ALU.mult))(jg, hb_),
                         ak + ["sm5"], [hk])
                self.dve((lambda hb_, h: lambda e: e.tensor_tensor(out=hb_[:], in0=hb_[:], in1=AP(self.gm_bc, h * 128, [[512, 128], [0, 4], [1, 128]]), op=ALU.mult))(hb_, h),
                         [hk, "gm_bc"], [hk])
                bk = jg % 2
                self.transposes([(psb[bk][:, cc * 128:(cc + 1) * 128], hb_[:, cc, :], c["c_identb"][:]) for cc in range(4)],
                                reads=[hk, "c_identb"], writes=[self.bank(bk)], name="hntr")
                msl = mixT[:, h, jg * TT:(jg + 1) * TT]
                self.dve((lambda msl, bk: lambda e: e.scalar_tensor_tensor(out=msl, in0=msl, scalar=1.0, in1=psb[bk][:, 0:512], op0=ALU.add, op1=ALU.mult))(msl, bk),
                         [self.bank(bk), ("mixT", h, jg)], [("mixT", h, jg)])

        v_ops = P.capture(vpart)
        P.replay_merged([v_ops, gates_ops], weights=[1, 2])
        p_ops = [P.capture((lambda h: lambda: proj(h))(h)) for h in range(4)]
        r_ops = [P.capture((lambda h: lambda: rec(h))(h)) for h in range(4)]
        P.replay_merged([p_ops[0]])
        for h in range(4):
            if h < 3:
                P.replay_merged([r_ops[h], p_ops[h + 1]], weights=[2, 1])
            else:
                P.replay_merged([r_ops[h]])

    def wout_half(self, l, a, f, wo, wkey):
        ps, xT, mixT = self.ps, self.xT, a["mixT"]
        self.wload(wo[:], self.din["w_out"][l][f * 512:(f + 1) * 512, :].rearrange("(k p) n -> p k n", p=128),
                   [(wkey, i) for i in range(3)], "s%d_0" % wkey[1])
        wkey = (wkey, 0)
        n = 0
        for tt in range(NT):
            tsl = slice(tt * TT, (tt + 1) * TT)
            for dm in range(KC):
                bk = n % 8
                n += 1
                self.mm(ps[:, bk, :], [(wo[:, k, dm * 128:(dm + 1) * 128], mixT[:, k, tsl]) for k in range(4)],
                        reads=[wkey] + [("mixT", k, tt) for k in range(4)], writes=[self.bank(bk)], name="wout")
                xs = xT[:, dm, tsl]
                self.dve((lambda xs, bk: lambda e: e.tensor_tensor(out=xs, in0=xs, in1=ps[:, bk, :], op=ALU.add))(xs, bk),
                         [self.bank(bk), ("xT", tt)], [("xT", tt)])

    def attention(self, l, a):
        P, ar, ps, c = self.P, self.ar, self.ps, self.c
        hT, mixT, vf, qT, kT = self.hT, a["mixT"], a["vfam"], a["qT"], a["kT"]
        psb = [ps[:, b, :].bitcast(BF16) for b in range(8)]
        zb = [ar.alloc("zb", [128, TT], BF16) for _ in range(2)]
        t1 = ar.alloc("t1", [128, TT], F32)
        t2 = ar.alloc("t2", [128, TT], F32)
        Pt = [ar.alloc("Pt", [128, 2, TT], BF16) for _ in range(2)]
        Osb = ar.alloc("Osb", [128, 4, 2, 129], F32)
        o1 = ar.alloc("o1", [128, 4, 128], F32)
        o2 = ar.alloc("o2", [128, 4, 128], F32)
        on4 = ar.alloc("on4", [128, 4, 128], BF16)
        sa = ar.alloc("sa", [128, 6, 8], F32)
        slots = [(a["slotA"], ("slot", 0)), (a["slotB"], ("slot", 1))]
        self.vfam_project(l, a, VA0, *slots[0])
        w_in = self.din["w_in"][l]
        cosT, sinT = c["c_cos"], c["c_sin"]
        nlam = self.lam_s[:, 5:6]
        for h in range(4):
            slot, sk = slots[(h + 1) % 2]
            for i, c0 in enumerate((QA0 + 128 * h, KA0 + 128 * h)):
                self.wload(slot[:, :, i * 128:(i + 1) * 128], w_in[:, c0:c0 + 128].rearrange("(k p) n -> p k n", p=128), (sk, i), "s%d_%d" % (sk[1], i))
            for i, dstT in enumerate((qT, kT)):
                for tt in range(NT):
                    tsl = slice(tt * TT, (tt + 1) * TT)
                    b0 = (2 * tt) % 4
                    b1 = b0 + 1
                    z = zb[tt % 2]
                    zk = ("zb", tt % 2)
                    self.mm(ps[:, b0, :], [(slot[:, k, i * 128:(i + 1) * 128], hT[:, k, tsl]) for k in range(KC)],
                            reads=[(sk, i), ("hT", tt)], writes=[self.bank(b0)], name="aproj")
                    self.act(z[:], ps[:, b0, :], AF.Copy, [self.bank(b0)], [zk])
                    self.mm(ps[:, b1, :], [(c["c_perm"][:], z[:])], reads=[zk, "c_perm"], writes=[self.bank(b1)], name="rot")
                    self.dve((lambda b0, tsl: lambda e: e.tensor_tensor(out=t1[:], in0=ps[:, b0, :], in1=cosT[:, tsl], op=ALU.mult))(b0, tsl),
                             [self.bank(b0), "c_cos"], ["t1"])
                    self.dve((lambda b1, tsl: lambda e: e.tensor_tensor(out=t2[:], in0=ps[:, b1, :], in1=sinT[:, tsl], op=ALU.mult))(b1, tsl),
                             [self.bank(b1), "c_sin"], ["t2"])
                    self.dve((lambda dstT, tsl: lambda e: e.tensor_tensor(out=dstT[:, tsl], in0=t1[:], in1=t2[:], op=ALU.add))(dstT, tsl),
                             ["t1", "t2"], [("qk", i, tt)])
            for qt in range(NT if self.att_level >= 2 else 0):
                nkb = 4 * qt + 4
                for kb in range(nkb):
                    di = kb - 4 * qt
                    q0 = max(di, 0) * 128
                    N = TT - q0
                    pi = kb % 2
                    pt = Pt[pi]
                    sb0, sb1 = 2 * pi, 2 * pi + 1

                    def fqk(e, kb=kb, q0=q0, N=N, sb0=sb0, sb1=sb1, qt=qt):
                        i0 = e.matmul(ps[:, sb0, 0:N], kT[0:64, kb * 128:(kb + 1) * 128], qT[0:64, qt * TT + q0:(qt + 1) * TT], start=True, stop=True)
                        i1 = e.matmul(ps[:, sb1, 0:N], kT[64:128, kb * 128:(kb + 1) * 128], qT[64:128, qt * TT + q0:(qt + 1) * TT], start=True, stop=True)
                        return i0, i1
                    P.op("pe", fqk, reads=[("qk", 0, qt), ("qk", 1, kb // 4)], writes=[self.bank(sb0), self.bank(sb1)], name="qk")
                    self.act(pt[:, 0, 0:N], ps[:, sb0, 0:N], AF.Exp, [self.bank(sb0)], [("Pt", pi, 0)], scale=SC_A)
                    self.act(pt[:, 1, 0:N], ps[:, sb1, 0:N], AF.Exp, [self.bank(sb1)], [("Pt", pi, 1)], scale=SC_A)
                    if di >= 0:
                        self.dve((lambda pt: lambda e: e.tensor_tensor(out=pt[:, :, 0:128], in0=pt[:, :, 0:128],
                                                                       in1=AP(c["c_maskb"], 0, [[128, 128], [0, 2], [1, 128]]), op=ALU.mult))(pt),
                                 [("Pt", pi, 0), ("Pt", pi, 1), "c_maskb"], [("Pt", pi, 0), ("Pt", pi, 1)])
                    qs0 = max(di, 0)
                    if self.att_level < 3:
                        continue

                    def fpv(e, kb=kb, qs0=qs0, pt=pt, h=h, qt=qt):
                        first = last = None
                        for qs in range(qs0, 4):
                            for cc in range(2):
                                ins = e.matmul(ps[:, 4 + qs, cc * 129:(cc + 1) * 129], pt[:, cc, (qs - qs0) * 128:(qs - qs0 + 1) * 128], vf[:, kb, h, :],
                                               start=(kb == 0 and cc == 0), stop=(kb == 4 * qt + qs), skip_group_check=True)
                                if first is None:
                                    first = ins
                                last = ins
                        return first, last
                    P.op("pe", fpv, reads=[("Pt", pi, 0), ("Pt", pi, 1), ("vfam", kb)], writes=[self.bank(4 + qs) for qs in range(qs0, 4)], name="pv")
                    if di >= 0:
                        qs = di
                        self.dve((lambda qs: lambda e: e.tensor_copy(out=Osb[:, qs, :, :], in_=ps[:, 4 + qs, 0:258].rearrange("p (c e) -> p c e", c=2)))(qs),
                                 [self.bank(4 + qs)], ["Osb"])
                if self.att_level < 4:
                    continue
                lcol = AP(Osb, 128, [[4 * 2 * 129, 128], [129, 8]])
                self.dve(lambda e: e.reciprocal(out=sa[:, 0, :], in_=lcol), ["Osb"], ["sa0"])
                self.dve(lambda e: e.tensor_scalar(out=sa[:, 1, :], in0=sa[:, 0, :], scalar1=nlam, scalar2=None, op0=ALU.mult), ["sa0", "nlam"], ["sa1"])
                self.dve(lambda e: e.tensor_tensor(out=o1[:], in0=Osb[:, :, 0, 0:128], in1=AP(sa, 0, [[48, 128], [2, 4], [0, 128]]), op=ALU.mult), ["Osb", "sa0"], ["o1"])
                self.dve(lambda e: e.tensor_tensor(out=o2[:], in0=Osb[:, :, 1, 0:128], in1=AP(sa, 8 + 1, [[48, 128], [2, 4], [0, 128]]), op=ALU.mult), ["Osb", "sa1"], ["o2"])
                self.dve(lambda e: e.tensor_tensor(out=o1[:], in0=o1[:], in1=o2[:], op=ALU.add), ["o1", "o2"], ["o1"])
                self.dve(lambda e: e.tensor_tensor(out=o2[:], in0=o1[:], in1=o1[:], op=ALU.mult), ["o1", "o2"], ["o2"])
                self.dve(lambda e: e.tensor_reduce(out=sa[:, 2, 0:4], in_=o2[:], axis=AX.X, op=ALU.add), ["o2"], ["sa2"])
                self.dve(lambda e: e.tensor_scalar(out=sa[:, 2, 0:4], in0=sa[:, 2, 0:4], scalar1=1.0 / 128, scalar2=None, op0=ALU.mult), ["sa2"], ["sa2"])
                self.rsqrt_small(sa[:, 2, 0:4], "sa2", HEPS)
                self.dve(lambda e: e.tensor_tensor(out=o1[:], in0=o1[:], in1=AP(sa, 16, [[48, 128], [1, 4], [0, 128]]), op=ALU.mult), ["o1", "sa2"], ["o1"])
                self.dve((lambda h: lambda e: e.tensor_tensor(out=on4[:], in0=o1[:], in1=AP(self.gd_bc, h * 128, [[512, 128], [0, 4], [1, 128]]), op=ALU.mult))(h),
                         ["o1", "gd_bc"], ["on4"])
                bk = 2 * (qt % 2)
                self.transposes([(psb[bk][:, cc * 128:(cc + 1) * 128], on4[:, cc, :], c["c_identb"][:]) for cc in range(4)],
                                reads=["on4", "c_identb"], writes=[self.bank(bk)], name="ontr")
                self.act(mixT[:, h, qt * TT:(qt + 1) * TT], psb[bk][:, 0:512], AF.Copy, [self.bank(bk)], [("mixT", h, qt)])

    def ffn(self, l):
        P, ar, ps = self.P, self.ar, self.ps
        hT, xT, din = self.hT, self.xT, self.din
        wg = [ar.alloc("fwg", [128, KC, 6 * 128], BF16) for _ in range(2)]
        wu = [ar.alloc("fwu", [128, KC, 6 * 128], BF16) for _ in range(2)]
        wd = [ar.alloc("fwd", [128, 6, D], BF16) for _ in range(2)]
        actb = [ar.alloc("actb", [128, 6, TT], BF16) for _ in range(2)]
        sg = [ar.alloc("sg", [128, TT], BF16) for _ in range(2)]
        n_e = 0
        n_d = [0]
        step = 0
        pending = None

        def down(i, ncn, tt, ab, abk):
            tsl = slice(tt * TT, (tt + 1) * TT)
            for dm in range(KC):
                bk = 4 + n_d[0] % 4
                n_d[0] += 1
                self.mm(ps[:, bk, :], [(wd[i][:, hc, dm * 128:(dm + 1) * 128], ab[:, hc, :]) for hc in range(ncn)],
                        reads=[("fwd", i)] + [("actb", abk, hc) for hc in range(ncn)], writes=[self.bank(bk)], name="down")
                xs = xT[:, dm, tsl]
                self.dve((lambda xs, bk: lambda e: e.tensor_tensor(out=xs, in0=xs, in1=ps[:, bk, :], op=ALU.add))(xs, bk),
                         [self.bank(bk), ("xT", tt)], [("xT", tt)])

        for gi, (c0, ncn) in enumerate(FFN_GROUPS):
            i = gi % 2
            self.wload(wg[i][:, :, 0:ncn * 128], din["w_gate"][l][:, c0 * 128:(c0 + ncn) * 128].rearrange("(k p) n -> p k n", p=128), ("fwg", i), "fwg%d" % i)
            self.wload(wu[i][:, :, 0:ncn * 128], din["w_up"][l][:, c0 * 128:(c0 + ncn) * 128].rearrange("(k p) n -> p k n", p=128), ("fwu", i), "fwu%d" % i)
            self.wload(wd[i][:, 0:ncn, :], din["w_down"][l][c0 * 128:(c0 + ncn) * 128, :].rearrange("(c p) n -> p c n", p=128), ("fwd", i), "fwd%d" % i)
            for tt in range(NT):
                tsl = slice(tt * TT, (tt + 1) * TT)
                abk = step % 2
                ab = actb[abk]
                step += 1
                for hc in range(ncn):
                    bg_, bu_ = (0, 1) if n_e % 2 == 0 else (2, 3)
                    s_ = sg[n_e % 2]
                    sgk = ("sg", n_e % 2)
                    n_e += 1
                    self.mm(ps[:, bg_, :], [(wg[i][:, k, hc * 128:(hc + 1) * 128], hT[:, k, tsl]) for k in range(KC)],
                            reads=[("fwg", i), ("hT", tt)], writes=[self.bank(bg_)], name="gate")
                    self.mm(ps[:, bu_, :], [(wu[i][:, k, hc * 128:(hc + 1) * 128], hT[:, k, tsl]) for k in range(KC)],
                            reads=[("fwu", i), ("hT", tt)], writes=[self.bank(bu_)], name="up")
                    self.act(s_[:], ps[:, bg_, :], AF.Silu, [self.bank(bg_)], [sgk])
                    self.dve((lambda ab, hc, s_, bu_: lambda e: e.tensor_tensor(out=ab[:, hc, :], in0=s_[:], in1=ps[:, bu_, :], op=ALU.mult))(ab, hc, s_, bu_),
                             [sgk, self.bank(bu_)], [("actb", abk, hc)])
                if pending is not None:
                    down(*pending)
                pending = (i, ncn, tt, ab, abk)
        down(*pending)

    def store(self, sq, dst_dram, normalize):
        P, ar, ps, c = self.P, self.ar, self.ps, self.c
        m = ar.mark()
        yT = [ar.alloc("yT", [128, KC, TT], F32) for _ in range(2)]
        ob = [ar.alloc("ob", [128, D], F32) for _ in range(2)]
        nb_ = 0
        gf = self.gf
        for tt in range(NT):
            if normalize:
                self.norm(lambda cc: gf[:, cc:cc + 1], "gf", out_f32=lambda tt, cc: (yT[tt % 2][:, cc, :], ("yT", tt % 2)), tts=[tt])
            for bb in range(4):
                blk = tt * 4 + bb
                o_ = ob[nb_ % 2]
                ok = ("ob", nb_ % 2)
                nb_ += 1
                for half in range(2):
                    bk = (2 * blk + half) % 8
                    if normalize:
                        srcs = [yT[tt % 2][:, half * 4 + cc, bb * 128:(bb + 1) * 128] for cc in range(4)]
                        rk = [("yT", tt % 2)]
                    else:
                        srcs = [self.xT[:, half * 4 + cc, blk * 128:(blk + 1) * 128] for cc in range(4)]
                        rk = [("xT", tt)]
                    self.transposes([(ps[:, bk, cc * 128:(cc + 1) * 128], srcs[cc], c["c_identf"][:]) for cc in range(4)],
                                    reads=rk + ["c_identf"], writes=[self.bank(bk)], name="otr")
                    self.evac(half, o_[:, half * 512:(half + 1) * 512], ps[:, bk, :], [self.bank(bk)], [ok])
                P.op("sp", (lambda o_, blk: lambda e: e.dma_start(out=dst_dram[sq, blk * 128:(blk + 1) * 128, :], in_=o_[:]))(o_, blk),
                     reads=[ok], name="ostore", dma="st%d" % ((nb_ - 1) % 2))
        P.barrier()
        ar.reset(m)

    def build(self, stage=99):
        P, ar = self.P, self.ar
        self.setup()
        for sq in range(self.nseq):
            self.load_x(sq)
            for l in self.layers:
                if stage < 1:
                    break
                self.layer_params(l)
                g1 = self.g1
                self.norm((lambda l: lambda cc: g1[:, l, cc:cc + 1])(l), ("g1", l))
                P.barrier()
                self.dump("hT", self.hT, [("hT", t) for t in range(NT)])
                m = ar.mark()
                a = self.mixer_alloc()
                m2 = ar.mark()
                if stage >= 3:
                    self.mlstm(l, a)
                    self.dump("mixM", a["mixT"], [("mixT", h, t) for h in range(4) for t in range(NT)])
                if stage >= 4:
                    self.wout_half(l, a, 0, a["woB"], ("slot", 1))
                P.barrier()
                ar.reset(m2)
                if stage >= 5:
                    self.attention(l, a)
                    self.dump("mixA", a["mixT"], [("mixT", h, t) for h in range(4) for t in range(NT)])
                if stage >= 6:
                    self.wout_half(l, a, 1, a["woB"], ("slot", 1))
                    self.dump("xmid", self.xT, [("xT", t) for t in range(NT)])
                P.barrier()
                ar.reset(m)
                if stage >= 7:
                    g2 = self.g2
                    self.norm((lambda l: lambda cc: g2[:, l, cc:cc + 1])(l), ("g2", l))
                    P.barrier()
                    self.ffn(l)
                    P.barrier()
                ar.reset(m)
            self.store(sq, self.out, self.final_norm)
        P.emit()
        return self.nc


_CONSTS = None


def _run(nc_inputs_list, layers, final_norm):
    b = Builder(layers, nseq=2, final_norm=final_norm)
    nc = b.build()
    res = run_bass_kernel_spmd(nc, nc_inputs_list, core_ids=list(range(8)))
    return [r["out"] for r in res.results]


def kernel(**inputs):
    global _CONSTS
    if _CONSTS is None:
        _CONSTS = host_constants()
    x = np.ascontiguousarray(inputs["x"], dtype=np.float32)
    params = {n: np.ascontiguousarray(inputs[n], dtype=np.float32) for n, _ in PARAM_SPECS}
    shards = [x[2 * i:2 * i + 2] for i in range(8)]
    in_maps = []
    for i in range(8):
        mp = {"x": shards[i]}
        mp.update(params)
        mp.update(_CONSTS)
        in_maps.append(mp)
    outs = _run(in_maps, list(range(DEPTH)), True)
    return np.concatenate(outs, axis=0)
```

```python
import math
import contextlib
import numpy as np
import ml_dtypes
import concourse.bass as bass
import concourse.mybir as mybir
from concourse.bass_utils import run_bass_kernel_spmd

F32 = mybir.dt.float32
BF16 = mybir.dt.bfloat16
AF = mybir.ActivationFunctionType
ALU = mybir.AluOpType
AX = mybir.AxisListType

ENGS = ("pe", "act", "dve", "pool", "sp")

D = 1024
KC = 8
S = 2048
NT = 4
TT = 512
NB = 16
DEPTH = 4
INW = 3592
QM0, KM0, VM0, OM0, G0, QA0, KA0, VA0 = 0, 512, 1024, 1536, 2048, 2056, 2568, 3080
FF = 2816
FC = 22
EPS = 1e-6
HEPS = 1e-5
SC_M = 128.0 ** -0.5
SC_A = 64.0 ** -0.5
FFN_GROUPS = [(0, 6), (6, 6), (12, 5), (17, 5)]


class Op:
    __slots__ = ("eng", "fn", "deps", "sig", "dma_sem", "dma_val", "name")

    def __init__(self, eng, fn, name=""):
        self.eng = eng
        self.fn = fn
        self.deps = set()
        self.sig = None
        self.dma_sem = None
        self.dma_val = None
        self.name = name


class Prog:
    def __init__(self, nc):
        self.nc = nc
        self.ops = {e: [] for e in ENGS}
        self.all_ops = []
        self.last_writer = {}
        self.readers = {}
        self.dma_sems = {}
        self.barrier_deps = []

    def op(self, eng, fn, reads=(), writes=(), name="", dma=None, dma_total=False):
        o = Op(eng, fn, name)
        if dma is not None:
            ent = self.dma_sems.setdefault(dma, [eng, 0, dma_total])
            assert ent[0] == eng
            ent[1] += 16
            o.dma_sem = dma
            o.dma_val = ent[1]
        reads = list(reads)
        writes = list(writes)
        ps_reads = [k for k in reads if isinstance(k, tuple) and len(k) == 2 and k[0] == "ps"]
        if ps_reads:
            reads = [k for k in reads if k not in ps_reads]
            writes = writes + [("psr", k[1]) for k in ps_reads]
            for k in ps_reads:
                w = self.last_writer.get(k)
                if w is not None:
                    o.deps.add(w)
                self.readers.setdefault(k, []).append(o)
        for k in reads:
            w = self.last_writer.get(k)
            if w is not None:
                o.deps.add(w)
            self.readers.setdefault(k, []).append(o)
        for k in writes:
            w = self.last_writer.get(k)
            if w is not None:
                o.deps.add(w)
            for r in self.readers.get(k, ()):
                o.deps.add(r)
            self.readers[k] = []
            self.last_writer[k] = o
        for b in self.barrier_deps:
            o.deps.add(b)
        o.deps.discard(o)
        self.ops[eng].append(o)
        self.all_ops.append(o)
        return o

    def capture(self, fn):
        rec = []
        real = self.op
        self.op = lambda *a, **k: rec.append((a, k))
        try:
            fn()
        finally:
            self.op = real
        return rec

    def replay_merged(self, streams, weights=None):
        weights = weights or [1] * len(streams)
        idx = [0] * len(streams)
        while any(idx[i] < len(s) for i, s in enumerate(streams)):
            for i, s in enumerate(streams):
                for _ in range(weights[i]):
                    if idx[i] < len(s):
                        a, k = s[idx[i]]
                        idx[i] += 1
                        self.op(*a, **k)

    def barrier(self):
        deps = [self.ops[e][-1] for e in ENGS if self.ops[e]]
        last_dma = {}
        for o in self.all_ops:
            if o.dma_sem is not None:
                last_dma[o.dma_sem] = o
        self.barrier_deps = deps + list(last_dma.values())

    def emit(self):
        nc = self.nc
        referenced = set()
        for o in self.all_ops:
            for d in o.deps:
                if d.dma_sem is None and not (d.eng == o.eng == "pe"):
                    referenced.add(d)
        for e in ENGS:
            c = 0
            for o in self.ops[e]:
                if o in referenced:
                    c += 1
                    o.sig = c
        with contextlib.ExitStack() as st:
            esem = {e: st.enter_context(nc.semaphore("S_" + e)) for e in ENGS}
            dsem = {n: st.enter_context(nc.semaphore("D_" + n)) for n in self.dma_sems}
            block = st.enter_context(nc.Block())

            def run(e, eng):
                waited = {}
                for o in self.ops[e]:
                    need = {}
                    for d in o.deps:
                        if d.dma_sem is not None:
                            key = ("d", d.dma_sem)
                            val = self.dma_sems[d.dma_sem][1] if self.dma_sems[d.dma_sem][2] else d.dma_val
                        else:
                            if d.eng == e and e == "pe":
                                continue
                            if d.sig is None:
                                continue
                            key = ("e", d.eng)
                            val = d.sig
                        if waited.get(key, 0) >= val:
                            continue
                        if need.get(key, 0) < val:
                            need[key] = val
                    items = list(need.items())
                    for key, val in items:
                        waited[key] = val
                    sems = [(dsem[k[1]] if k[0] == "d" else esem[k[1]], v) for k, v in items]
                    for s_, v in sems[:-1]:
                        eng.wait_ge(s_, v)
                    r = o.fn(eng)
                    first, last = r if isinstance(r, tuple) else (r, r)
                    if sems:
                        s_, v = sems[-1]
                        first._wait_ge(s_, v)
                    if o.dma_sem is not None:
                        last.then_inc(dsem[o.dma_sem], 16)
                    elif o.sig is not None:
                        last.then_inc(esem[e], 1)
                for n, (owner, cnt, _tot) in self.dma_sems.items():
                    if owner == e:
                        eng.wait_ge(dsem[n], cnt)

            @block.tensor
            def _(eng):
                run("pe", eng)

            @block.scalar
            def _(eng):
                run("act", eng)

            @block.vector
            def _(eng):
                run("dve", eng)

            @block.gpsimd
            def _(eng):
                run("pool", eng)

            @block.sync
            def _(eng):
                run("sp", eng)


class Arena:
    def __init__(self, nc, base=16512, top=229344):
        self.nc = nc
        self.cur = base
        self.top = top
        self.n = 0

    def alloc(self, name, shape, dtype):
        sz = int(np.prod(shape[1:])) * mybir.dt.size(dtype)
        off = (self.cur + 31) // 32 * 32
        assert off + sz <= self.top, f"SBUF arena overflow at {name}: need {off + sz - self.top} more bytes"
        self.cur = off + sz
        self.n += 1
        return self.nc.alloc_sbuf_tensor_at(f"{name}_{self.n}", list(shape), dtype, offset=off)

    def mark(self):
        return self.cur

    def reset(self, m):
        self.cur = m


def AP(t, off, dims):
    return bass.AP(t, off, [list(d) for d in dims])


def host_constants():
    c = {}
    I = np.eye(128, dtype=np.float32)
    c["c_identb"] = I.astype(ml_dtypes.bfloat16)
    c["c_identf"] = I
    c["c_onesb"] = np.ones((128, 128), dtype=ml_dtypes.bfloat16)
    s_ = np.arange(128)[:, None]
    t_ = np.arange(128)[None, :]
    tri = (s_ <= t_).astype(np.float32)
    c["c_maskf"] = (tri * SC_M).astype(np.float32)
    c["c_maskb"] = tri.astype(ml_dtypes.bfloat16)
    perm = np.zeros((128, 128), dtype=np.float32)
    for r in range(128):
        i = r % 64
        if i < 32:
            perm[r + 32, r] = -1.0
        else:
            perm[r - 32, r] = 1.0
    c["c_perm"] = perm.astype(ml_dtypes.bfloat16)
    pos = np.arange(S, dtype=np.float32)
    inv = (np.float32(10000.0) ** (-np.arange(0, 64, 2, dtype=np.float32) / np.float32(64))).astype(np.float32)
    ang = (pos[:, None] * inv[None, :]).astype(np.float32)
    cosr = np.cos(ang.astype(np.float64)).T
    sinr = np.sin(ang.astype(np.float64)).T
    f_of_row = np.arange(128) % 32
    c["c_cos"] = cosr[f_of_row].astype(np.float32)
    c["c_sin"] = sinr[f_of_row].astype(np.float32)
    q = np.arange(64)
    jq, hq = q // 4, q % 4
    c["c_mcarry"] = ((hq[:, None] == hq[None, :]) & (jq[:, None] < jq[None, :])).astype(ml_dtypes.bfloat16)
    sel = (hq[:, None] == np.arange(4)[None, :]).astype(np.float32)
    c["c_sel"] = sel.astype(ml_dtypes.bfloat16)
    c["c_ohj"] = (jq[:, None] == np.arange(16)[None, :]).astype(np.float32)
    c["c_selT"] = np.ascontiguousarray(sel.T).astype(ml_dtypes.bfloat16)
    return c


CONST_SPECS = [("c_identb", [128, 128], BF16), ("c_identf", [128, 128], F32), ("c_onesb", [128, 128], BF16),
               ("c_maskf", [128, 128], F32), ("c_maskb", [128, 128], BF16), ("c_perm", [128, 128], BF16),
               ("c_cos", [128, S], F32), ("c_sin", [128, S], F32), ("c_mcarry", [64, 64], BF16),
               ("c_sel", [64, 4], BF16), ("c_ohj", [64, 16], F32), ("c_selT", [4, 64], BF16)]

PARAM_SPECS = [("g_mix", [DEPTH, D]), ("w_in", [DEPTH, D, INW]), ("conv_w", [DEPTH, 4, D]), ("conv_b", [DEPTH, D]),
               ("b_gates", [DEPTH, 8]), ("g_mlstm_head", [DEPTH, 512]), ("lam_q1", [DEPTH, 64]),
               ("lam_k1", [DEPTH, 64]), ("lam_q2", [DEPTH, 64]), ("lam_k2", [DEPTH, 64]),
               ("g_diff_head", [DEPTH, 512]), ("w_out", [DEPTH, D, D]), ("g_ffn", [DEPTH, D]),
               ("w_gate", [DEPTH, D, FF]), ("w_up", [DEPTH, D, FF]), ("w_down", [DEPTH, FF, D]), ("g_final", [D])]


class Builder:
    def __init__(self, layers, nseq=2, final_norm=True, debug=None, pdepth=DEPTH):
        self.layers = list(layers)
        self.nseq = nseq
        self.final_norm = final_norm
        self.debug = {}
        self.debug_names = set(debug or [])
        nc = bass.Bass("TRN2", target_bir_lowering=False)
        self.nc = nc
        self.P = Prog(nc)
        self.ar = Arena(nc)
        self.din = {}
        self.din["x"] = nc.dram_tensor("x", [nseq, S, D], F32, kind="ExternalInput").ap()
        self.pdepth = pdepth
        for n, shp in PARAM_SPECS:
            shp = list(shp)
            if n != "g_final":
                shp[0] = pdepth
            self.din[n] = nc.dram_tensor(n, shp, F32, kind="ExternalInput").ap()
        for n, shp, dt_ in CONST_SPECS:
            self.din[n] = nc.dram_tensor(n, list(shp), dt_, kind="ExternalInput").ap()
        self.out = nc.dram_tensor("out", [nseq, S, D], F32, kind="ExternalOutput").ap()
        self.dbg_out = {}
        for n, shp in self.debug.items():
            self.dbg_out[n] = nc.dram_tensor(n, list(shp), F32, kind="ExternalOutput").ap()
        self.ps = nc.alloc_psum_tensor("ps", [128, 8, 512], F32)
        self.wq_n = 0
        self.uid = 0
        self.att_level = 9

    def dump(self, name, t, keys):
        if name not in self.debug_names:
            return
        shp = [int(v) for v in t.shape]
        dt_ = t.dtype
        d = self.nc.dram_tensor("dbg_" + name, shp, dt_, kind="ExternalOutput").ap()
        self.P.op("sp", lambda e: e.dma_start(out=d, in_=t[:]), reads=keys, name="dump", dma="dbg_" + name)

    def bank(self, b):
        return ("ps", b)

    def mm(self, out_ap, pairs, reads, writes, name="mm", start=True, stop=True, skip=False):
        n = len(pairs)

        def fn(e):
            first = last = None
            for i, (l, r) in enumerate(pairs):
                ins = e.matmul(out_ap, l, r, start=(start and i == 0), stop=(stop and i == n - 1),
                               skip_group_check=skip)
                if first is None:
                    first = ins
                last = ins
            return first, last
        return self.P.op("pe", fn, reads=reads, writes=writes, name=name)

    def transposes(self, items, reads, writes, name="tr"):
        def fn(e):
            first = last = None
            for (o, i, idn) in items:
                ins = e.transpose(out=o, in_=i, identity=idn)
                if first is None:
                    first = ins
                last = ins
            return first, last
        return self.P.op("pe", fn, reads=reads, writes=writes, name=name)

    def wload(self, dst_ap, src_ap, keys, sem, name="wload", total=False):
        if not isinstance(keys, list):
            keys = [keys]

        def fn(e):
            return e.dma_start(out=dst_ap, in_=src_ap)
        return self.P.op("pool", fn, writes=keys, name=name, dma=sem, dma_total=total)

    def sload(self, dst_ap, src_ap, key, sem="setup", name="sload", nonc=False, total=True):
        def fn(e):
            if nonc:
                return e.dma_start(out=dst_ap, in_=src_ap, allow_slow_non_contiguous=True)
            return e.dma_start(out=dst_ap, in_=src_ap)
        return self.P.op("sp", fn, writes=[key], name=name, dma=sem, dma_total=total)

    def setup(self):
        ar, P, din = self.ar, self.P, self.din
        self.xT = ar.alloc("xT", [128, KC, S], F32)
        self.hT = ar.alloc("hT", [128, KC, S], BF16)
        self.c = {}
        for n, shp, dt_ in CONST_SPECS:
            if n in ("c_cos", "c_sin"):
                t = ar.alloc(n, shp, BF16)
                self.c[n] = t
                continue
            t = ar.alloc(n, shp, dt_)
            self.c[n] = t
            self.sload(t[:], din[n], key=n)
        self.g1 = ar.alloc("g1", [128, DEPTH, KC], F32)
        self.g2 = ar.alloc("g2", [128, DEPTH, KC], F32)
        self.gf = ar.alloc("gf", [128, KC], F32)
        self.cw = ar.alloc("cw", [128, DEPTH, 4, KC], F32)
        self.cb = ar.alloc("cb", [128, DEPTH, KC], F32)
        self.bg = ar.alloc("bg", [128, DEPTH, 8], F32)
        self.lamv = ar.alloc("lamv", [128, 4, DEPTH, 64], F32)
        for l in range(self.pdepth):
            self.sload(self.g1[:, l, :], din["g_mix"][l].rearrange("(c p) -> p c", p=128), key=("g1", l), nonc=True)
            self.sload(self.g2[:, l, :], din["g_ffn"][l].rearrange("(c p) -> p c", p=128), key=("g2", l), nonc=True)
            for j in range(4):
                self.sload(self.cw[:, l, j, :], din["conv_w"][l, j].rearrange("(c p) -> p c", p=128), key=("cw", l, j), nonc=True)
            self.sload(self.cb[:, l, :], din["conv_b"][l].rearrange("(c p) -> p c", p=128), key=("cb", l), nonc=True)
        self.sload(self.gf[:], din["g_final"].rearrange("(c p) -> p c", p=128), key="gf", nonc=True)
        bgd = din["b_gates"]
        self.sload(self.bg[:, 0:self.pdepth, :], AP(bgd.tensor, 0, [[0, 128], [8, self.pdepth], [1, 8]]), key="bg")
        for i, n in enumerate(("lam_q1", "lam_k1", "lam_q2", "lam_k2")):
            self.sload(self.lamv[:, i, 0:self.pdepth, :], AP(din[n].tensor, 0, [[0, 128], [64, self.pdepth], [1, 64]]), key=("lamv", i))
        self.wload(self.c["c_cos"][:], din["c_cos"], "c_cos", "wsetup", total=True)
        self.wload(self.c["c_sin"][:], din["c_sin"], "c_sin", "wsetup", total=True)
        self.gm_bc = ar.alloc("gm_bc", [128, 512], F32)
        self.gd_bc = ar.alloc("gd_bc", [128, 512], F32)
        self.lam_s = ar.alloc("lam_s", [128, 8], F32)
        self.lam_t = ar.alloc("lam_t", [128, 2, 64], F32)
        self.phase_base = ar.mark()

    def load_x(self, sq):
        P, ar = self.P, self.ar
        m = ar.mark()
        xin = [ar.alloc("xin", [128, D], F32) for _ in range(2)]
        xd = self.din["x"]
        for b in range(NB):
            t = xin[b % 2]
            self.sload(t[:], xd[sq, b * 128:(b + 1) * 128, :], key=("xin", b % 2), sem="xin%d" % (b % 2), total=False)
            for half in range(2):
                bk = (2 * b + half) % 8
                items = [(self.ps[:, bk, cc * 128:(cc + 1) * 128], t[:, (half * 4 + cc) * 128:(half * 4 + cc + 1) * 128],
                          self.c["c_identf"][:]) for cc in range(4)]
                self.transposes(items, reads=[("xin", b % 2), "c_identf"], writes=[self.bank(bk)], name="xtr")
                src = AP(self.ps, bk * 512, [[4096, 128], [128, 4], [1, 128]])
                dst = self.xT[:, half * 4:(half + 1) * 4, b * 128:(b + 1) * 128]
                eng = "dve" if half == 0 else "act"
                if eng == "dve":
                    P.op("dve", (lambda d_, s_: lambda e: e.tensor_copy(out=d_, in_=s_))(dst, src),
                         reads=[self.bank(bk)], writes=[("xT", b // 4)])
                else:
                    P.op("act", (lambda d_, s_: lambda e: e.activation(out=d_, in_=s_, func=AF.Copy))(dst, src),
                         reads=[self.bank(bk)], writes=[("xT", b // 4)])
        P.barrier()
        ar.reset(m)

    def norm(self, gt_ap_fn, gkey, out_f32=None, tts=None):
        P, ar = self.P, self.ar
        m = ar.mark()
        sq = [ar.alloc("sq", [128, KC, TT], BF16) for _ in range(2)]
        rs = [ar.alloc("rs", [128, TT], F32) for _ in range(2)]
        for tt in (range(NT) if tts is None else tts):
            i = tt % 2
            tsl = slice(tt * TT, (tt + 1) * TT)
            P.op("act", (lambda o_, i_: lambda e: e.activation(out=o_, in_=i_, func=AF.Square))(sq[i][:], self.xT[:, :, tsl]),
                 reads=[("xT", tt)], writes=[("sq", i)])
            bk = i
            self.mm(self.ps[:, bk, :], [(self.c["c_onesb"][:], sq[i][:, c, :]) for c in range(KC)],
                    reads=[("sq", i), "c_onesb"], writes=[self.bank(bk)], name="ssq")
            P.op("act", (lambda o_, i_: lambda e: e.activation(out=o_, in_=i_, func=AF.Ln, scale=1.0 / D, bias=EPS))(rs[i][:], self.ps[:, bk, :]),
                 reads=[self.bank(bk)], writes=[("rs", i)])
            P.op("act", (lambda o_: lambda e: e.activation(out=o_, in_=o_, func=AF.Exp, scale=-0.5))(rs[i][:]),
                 reads=[("rs", i)], writes=[("rs", i)])
            for c in range(KC):
                if out_f32 is None:
                    dst = self.hT[:, c, tsl]
                    wk = ("hT", tt)
                else:
                    dst, wk = out_f32(tt, c)
                P.op("dve", (lambda d_, x_, g_, r_: lambda e: e.scalar_tensor_tensor(out=d_, in0=x_, scalar=g_, in1=r_, op0=ALU.mult, op1=ALU.mult))(
                    dst, self.xT[:, c, tsl], gt_ap_fn(c), rs[i][:]),
                    reads=[("xT", tt), ("rs", i), gkey], writes=[wk])
        ar.reset(m)

    def dve(self, fn, reads, writes, name="dve"):
        return self.P.op("dve", fn, reads=reads, writes=writes, name=name)

    def act(self, out, in_, func, reads, writes, scale=1.0, bias=0.0, accum=None, name="act"):
        def fn(e):
            kw = {}
            if accum is not None:
                kw["accum_out"] = accum
            return e.activation(out=out, in_=in_, func=func, bias=bias, scale=scale, **kw)
        return self.P.op("act", fn, reads=reads, writes=writes, name=name)

    def evac(self, k, out, in_, reads, writes):
        if k % 2 == 0:
            return self.act(out, in_, AF.Copy, reads, writes, name="evac")
        return self.dve(lambda e: e.tensor_copy(out=out, in_=in_), reads, writes, name="evac")

    def rsqrt_small(self, t_ap, key, bias):
        self.act(t_ap, t_ap, AF.Ln, [key], [key], bias=bias)
        self.act(t_ap, t_ap, AF.Exp, [key], [key], scale=-0.5)

    def split3(self, dst, src, np_, n, skey, dkey):
        ar = self.ar
        r1 = ar.alloc("sp_r1", [np_, n], F32)
        r2 = ar.alloc("sp_r2", [np_, n], F32)
        self.uid += 1
        k1, k2 = ("sp_r1", self.uid), ("sp_r2", self.uid)
        self.dve(lambda e: e.tensor_copy(out=dst[:, 0, :], in_=src), [skey], [(dkey, 0)])
        self.dve(lambda e: e.tensor_tensor(out=r1[:], in0=src, in1=dst[:, 0, :], op=ALU.subtract), [skey, (dkey, 0)], [k1])
        self.dve(lambda e: e.tensor_copy(out=dst[:, 1, :], in_=r1[:]), [k1], [(dkey, 1)])
        self.dve(lambda e: e.tensor_tensor(out=r2[:], in0=r1[:], in1=dst[:, 1, :], op=ALU.subtract), [k1, (dkey, 1)], [k2])
        self.dve(lambda e: e.tensor_copy(out=dst[:, 2, :], in_=r2[:]), [k2], [(dkey, 2)])
        return [(dkey, i) for i in range(3)]

    def layer_params(self, l):
        P, din = self.P, self.din
        lam_init = 0.8 - 0.6 * math.exp(-0.3 * l)
        self.sload(self.gm_bc[:], AP(din["g_mlstm_head"].tensor, l * 512, [[0, 128], [1, 512]]), key="gm_bc", sem="gm_bc", total=False)
        self.sload(self.gd_bc[:], AP(din["g_diff_head"].tensor, l * 512, [[0, 128], [1, 512]]), key="gd_bc", sem="gd_bc", total=False)
        gm, gd = self.gm_bc, self.gd_bc
        self.dve(lambda e: e.tensor_scalar(out=gm[:], in0=gm[:], scalar1=0.5, scalar2=None, op0=ALU.mult), ["gm_bc"], ["gm_bc"])
        self.dve(lambda e: e.tensor_scalar(out=gd[:], in0=gd[:], scalar1=1.0 - lam_init, scalar2=None, op0=ALU.mult), ["gd_bc"], ["gd_bc"])
        lv, lt, ls = self.lamv, self.lam_t, self.lam_s
        for i in range(2):
            self.dve((lambda i: lambda e: e.tensor_tensor(out=lt[:, i, :], in0=lv[:, 2 * i, l, :], in1=lv[:, 2 * i + 1, l, :], op=ALU.mult))(i),
                     [("lamv", 2 * i), ("lamv", 2 * i + 1)], ["lam_t"])
        self.dve(lambda e: e.tensor_reduce(out=ls[:, 0:2], in_=lt[:], axis=AX.X, op=ALU.add), ["lam_t"], ["lam_s"])
        self.act(ls[:, 2:4], ls[:, 0:2], AF.Exp, ["lam_s"], ["lam_s"])
        self.dve(lambda e: e.tensor_tensor(out=ls[:, 4:5], in0=ls[:, 3:4], in1=ls[:, 2:3], op=ALU.subtract), ["lam_s"], ["lam_s"])
        self.dve(lambda e: e.tensor_scalar(out=ls[:, 5:6], in0=ls[:, 4:5], scalar1=-lam_init, scalar2=None, op0=ALU.add), ["lam_s"], ["nlam"])

    def mixer_alloc(self):
        ar = self.ar
        a = {}
        a["mixT"] = ar.alloc("mixT", [128, 4, S], BF16)
        a["vfam"] = ar.alloc("vfam", [128, NB, 4, 129], BF16)
        off0 = (ar.cur + 31) // 32 * 32
        a["slotA"] = ar.alloc("slotA", [128, KC, 512], BF16)
        off1 = (ar.cur + 31) // 32 * 32
        a["slotB"] = ar.alloc("slotB", [128, KC, 512], BF16)
        a["woA"] = self.nc.alloc_sbuf_tensor_at("woA_%d" % ar.n, [128, 4, D], BF16, offset=off0)
        a["woB"] = self.nc.alloc_sbuf_tensor_at("woB_%d" % ar.n, [128, 4, D], BF16, offset=off1)
        a["qT"] = ar.alloc("qT", [128, S], BF16)
        a["kT"] = ar.alloc("kT", [128, S], BF16)
        a["wg"] = ar.alloc("wg", [128, KC, 8], BF16)
        a["gt"] = ar.alloc("gt", [128, 5, 64], F32)
        return a

    def gates(self, l, a):
        P, ar, ps, c = self.P, self.ar, self.ps, self.c
        m = ar.mark()
        wg, gt = a["wg"], a["gt"]
        wgf = ar.alloc("wgf", [128, KC, 8], F32)
        self.sload(wgf[:], self.din["w_in"][l][:, G0:G0 + 8].rearrange("(k p) n -> p k n", p=128), key="wgf", sem="wgf", total=False)
        self.dve(lambda e: e.tensor_copy(out=wg[:], in_=wgf[:]), ["wgf"], ["wg"])
        hT = self.hT

        def fn(e):
            first = last = None
            for j in range(NB):
                for k in range(KC):
                    ins = e.matmul(ps[:, 0, j * 8:(j + 1) * 8], hT[:, k, j * 128:(j + 1) * 128], wg[:, k, :],
                                   start=(k == 0), stop=(k == KC - 1))
                    if first is None:
                        first = ins
                    last = ins
            return first, last
        P.op("pe", fn, reads=["wg"] + [("hT", t) for t in range(NT)], writes=[self.bank(0)], name="gates_mm")
        g_tm = ar.alloc("g_tm", [128, 2, 64], F32)
        bg = self.bg
        self.dve(lambda e: e.tensor_tensor(out=AP(g_tm, 0, [[128, 128], [4, NB], [64, 2], [1, 4]]),
                                           in0=AP(ps, 0 * 512, [[4096, 128], [8, NB], [4, 2], [1, 4]]),
                                           in1=AP(bg, l * 8, [[DEPTH * 8, 128], [0, NB], [4, 2], [1, 4]]), op=ALU.add),
                 [self.bank(0), "bg"], ["g_tm"])
        idf = c["c_identf"]
        self.transposes([(ps[0:64, 1, 0:128], g_tm[:, 0, :], idf[:]),
                         (ps[0:64, 1, 128:256], g_tm[:, 1, :], idf[:])],
                        reads=["g_tm", "c_identf"], writes=[self.bank(1)], name="gates_tr")
        T = lambda n, w=128: ar.alloc(n, [64, w], F32)
        ef, lsp, zer, cs, carry, Bn, A, cm, cmxJ = T("ef"), T("lsp"), T("zer"), T("cs"), T("carry", 1), T("Bn"), T("A"), T("cm"), T("cmxJ", 16)
        self.act(ef[:], ps[0:64, 1, 128:256], AF.Exp, [self.bank(1)], ["ef"], scale=-1.0)
        self.act(lsp[:], ef[:], AF.Ln, ["ef"], ["lsp"], bias=1.0)
        self.dve(lambda e: e.memset(zer[:], 0.0), [], ["zer"])
        self.dve(lambda e: e.tensor_tensor_scan(out=cs[:], data0=lsp[:], data1=zer[:], initial=0.0, op0=ALU.add, op1=ALU.add),
                 ["lsp", "zer"], ["cs"])
        tot3 = ar.alloc("tot3", [64, 3, 2], BF16)
        k3 = self.split3(tot3, cs[:, 126:128], 64, 2, "cs", "tot3")
        self.mm(ps[0:64, 2, 0:2], [(c["c_mcarry"][:], tot3[:, i, :]) for i in range(3)], reads=k3 + ["c_mcarry"], writes=[self.bank(2)], name="carry")
        self.dve(lambda e: e.tensor_copy(out=carry[:], in_=ps[0:64, 2, 1:2]), [self.bank(2)], ["carry"])
        self.dve(lambda e: e.tensor_scalar(out=Bn[:], in0=cs[:], scalar1=carry[:], scalar2=None, op0=ALU.add), ["cs", "carry"], ["Bn"])
        self.dve(lambda e: e.tensor_tensor(out=A[:], in0=ps[0:64, 1, 0:128], in1=Bn[:], op=ALU.add), [self.bank(1), "Bn"], ["A"])
        self.dve(lambda e: e.tensor_tensor_scan(out=cm[:], data0=A[:], data1=A[:], initial=0.0, op0=ALU.max, op1=ALU.max), ["A"], ["cm"])
        self.dve(lambda e: e.tensor_scalar(out=cmxJ[:], in0=c["c_ohj"][:], scalar1=cm[:, 127:128], scalar2=None, op0=ALU.mult),
                 ["cm", "c_ohj"], ["cmxJ"])
        cmx3 = ar.alloc("cmx3", [64, 3, 16], BF16)
        k3 = self.split3(cmx3, cmxJ[:], 64, 16, "cmxJ", "cmx3")
        self.mm(ps[0:4, 2, 8:24], [(c["c_sel"][:], cmx3[:, i, :]) for i in range(3)], reads=k3 + ["c_sel"], writes=[self.bank(2)], name="hj")
        hj = ar.alloc("hj", [4, 16], F32)
        Mp = ar.alloc("Mp", [4, 17], F32)
        self.dve(lambda e: e.tensor_copy(out=hj[:], in_=ps[0:4, 2, 8:24]), [self.bank(2)], ["hj"])
        self.dve(lambda e: e.memset(Mp[:], 0.0), [], ["Mp"])
        self.dve(lambda e: e.tensor_tensor_scan(out=Mp[:, 1:17], data0=hj[:], data1=hj[:], initial=0.0, op0=ALU.max, op1=ALU.max),
                 ["hj", "Mp"], ["Mp"])

        Mp3 = ar.alloc("Mp3", [4, 3, 18], BF16)
        self.dve(lambda e: e.memset(Mp3[:], 0.0), [], [("Mp3", i) for i in range(3)])
        Mp3v = AP(Mp3, 0, [[54, 4], [18, 3], [1, 17]])
        k3 = self.split3(Mp3v, Mp[:], 4, 17, "Mp", "Mp3")

        def fn2(e):
            first = last = None
            for (c0, o0) in ((0, 0), (1, 16)):
                for i in range(3):
                    ins = e.matmul(ps[0:64, 3, o0:o0 + 16], c["c_selT"][:], Mp3[:, i, c0:c0 + 16], start=(i == 0), stop=(i == 2))
                    if first is None:
                        first = ins
                    last = ins
            return first, last
        P.op("pe", fn2, reads=k3 + ["c_selT"], writes=[self.bank(3)], name="mq")
        tmp2 = ar.alloc("tmp2", [64, 2, 16], F32)
        mpe = ar.alloc("mpe", [64, 2], F32)
        nb = ar.alloc("nb", [64, 4], F32)
        ohj = c["c_ohj"]
        self.dve(lambda e: e.tensor_tensor(out=tmp2[:], in0=AP(ps, 3 * 512, [[4096, 64], [16, 2], [1, 16]]),
                                           in1=AP(ohj, 0, [[16, 64], [0, 2], [1, 16]]), op=ALU.mult),
                 [self.bank(3), "c_ohj"], ["tmp2"])
        self.dve(lambda e: e.tensor_reduce(out=mpe[:], in_=tmp2[:], axis=AX.X, op=ALU.add), ["tmp2"], ["mpe"])
        Mg = T("Mg")
        self.dve(lambda e: e.tensor_scalar(out=Mg[:], in0=cm[:], scalar1=mpe[:, 0:1], scalar2=None, op0=ALU.max), ["cm", "mpe"], ["Mg"])
        self.dve(lambda e: e.tensor_scalar(out=nb[:, 0:1], in0=mpe[:, 0:1], scalar1=-1.0, scalar2=None, op0=ALU.mult), ["mpe"], ["nb"])
        self.dve(lambda e: e.tensor_scalar(out=nb[:, 1:2], in0=mpe[:, 1:2], scalar1=-1.0, scalar2=math.log(SC_M), op0=ALU.mult, op1=ALU.add), ["mpe"], ["nb"])
        self.dve(lambda e: e.tensor_tensor(out=nb[:, 2:3], in0=mpe[:, 0:1], in1=mpe[:, 1:2], op=ALU.subtract), ["mpe"], ["nb"])
        u, ws, w, fl, d2, dec, ddg = T("u"), T("ws"), T("w"), T("fl"), T("d2"), T("dec", 1), T("ddg", 64)
        self.act(u[:], A[:], AF.Exp, ["A", "nb"], ["u"], bias=nb[:, 0:1])
        self.act(ws[:], A[:], AF.Exp, ["A", "nb"], ["ws"], bias=nb[:, 1:2])
        self.act(w[:], Mg[:], AF.Exp, ["Mg", "mpe"], ["w"], scale=-1.0, bias=mpe[:, 0:1])
        self.dve(lambda e: e.tensor_tensor(out=d2[:], in0=Bn[:], in1=Mg[:], op=ALU.subtract), ["Bn", "Mg"], ["d2"])
        self.act(fl[:], d2[:], AF.Exp, ["d2"], ["fl"])
        self.act(dec[:], nb[:, 2:3], AF.Exp, ["nb"], ["dec"])
        dec3 = ar.alloc("dec3", [64, 3, 2], BF16)
        decf = ar.alloc("decf", [64, 2], F32)
        self.dve(lambda e: e.tensor_copy(out=decf[:], in_=AP(dec, 0, [[1, 64], [0, 2]])), ["dec"], ["decf"])
        k3d = self.split3(dec3, decf[:], 64, 2, "decf", "dec3")
        ddg3 = ar.alloc("ddg3", [64, 3, 64], BF16)
        decs = ar.alloc("decs", [64, 4], F32)
        self.dve(lambda e: e.tensor_copy(out=decs[:, 0:3], in_=dec3[:, :, 0]), k3d, ["decs"])
        for i in range(3):
            self.dve((lambda i: lambda e: e.tensor_scalar(out=ddg3[:, i, :], in0=c["c_identb"][0:64, 0:64], scalar1=decs[:, i:i + 1], scalar2=None, op0=ALU.mult))(i),
                     ["decs", "c_identb"], [("ddg3", i)])

        def fn3(e):
            first = None
            for i, arr in enumerate((u, ws, w, fl)):
                ins = e.transpose(out=ps[:, 4, i * 64:(i + 1) * 64], in_=arr[:], identity=idf[0:64, 0:64])
                if first is None:
                    first = ins
            for i in range(3):
                last = e.matmul(ps[:, 4, 256:320], c["c_onesb"][0:64, :], ddg3[:, i, :], start=(i == 0), stop=(i == 2))
            return first, last
        P.op("pe", fn3, reads=["u", "ws", "w", "fl", "c_onesb", "c_identf"] + [("ddg3", i) for i in range(3)], writes=[self.bank(4)], name="gt_tr")
        self.dve(lambda e: e.tensor_copy(out=gt[:], in_=AP(ps, 4 * 512, [[4096, 128], [64, 5], [1, 64]])), [self.bank(4)], ["gt"])

    def vfam_project(self, l, a, col0, slot, slotkey, banks=(0, 1, 2, 3)):
        P, ps = self.P, self.ps
        vf, hT = a["vfam"], self.hT
        self.wload(slot[:], self.din["w_in"][l][:, col0:col0 + 512].rearrange("(k p) n -> p k n", p=128),
                   [(slotkey, i) for i in range(3)], "s%d_0" % slotkey[1])
        slotkey = (slotkey, 0)
        for j in range(NB):
            bk = banks[j % len(banks)]
            self.mm(ps[:, bk, :], [(hT[:, k, j * 128:(j + 1) * 128], slot[:, k, :]) for k in range(KC)],
                    reads=[slotkey, ("hT", j // 4)], writes=[self.bank(bk)], name="vproj")
            dst = AP(vf, j * 4 * 129, [[NB * 4 * 129, 128], [129, 4], [1, 128]])
            src = AP(ps, bk * 512, [[4096, 128], [128, 4], [1, 128]])
            self.evac(j, dst, src, [self.bank(bk)], [("vfam", j)])

    def mlstm(self, l, a):
        P, ar, ps, c = self.P, self.ar, self.ps, self.c
        hT, mixT, vf, gt = self.hT, a["mixT"], a["vfam"], a["gt"]
        psb = [ps[:, b, :].bitcast(BF16) for b in range(8)]
        m_g = ar.mark()
        gates_ops = P.capture(lambda: self.gates(l, a))
        g_end = ar.mark()
        ar.reset(m_g)
        accs = ar.alloc("accs", [128, NB, 129], F32)
        CTf = ar.alloc("CTf", [128, 129], F32)
        CTb = ar.alloc("CTb", [128, 129], BF16)
        scT = [ar.alloc("scT", [128, 128], BF16) for _ in range(2)]
        junk = ar.alloc("junk", [128, 128], BF16)
        sm = ar.alloc("sm", [128, 6, NB], F32)
        hn4 = [ar.alloc("hn4", [128, 4, 128], BF16) for _ in range(2)]
        if ar.cur < g_end:
            ar.cur = g_end
        ubuf = ar.alloc("ubuf", [128, 3 + S], BF16)
        diag = ar.alloc("diag", [128, 4, 128], BF16)
        ktok = [ar.alloc("ktok", [128, NB, 128], BF16) for _ in range(2)]
        qTs = [a["qT"], ar.alloc("qT2", [128, S], BF16)]
        kTs = [a["kT"], ar.alloc("kT2", [128, S], BF16)]
        slots = [(a["slotA"], ("slot", 0)), (a["slotB"], ("slot", 1))]
        PB = (5, 6, 7)
        w_in = self.din["w_in"][l]
        cw, cb = self.cw, self.cb

        def vpart():
            self.dve(lambda e: e.memset(ubuf[:, 0:3], 0.0), [], ["ubuf"])
            self.dve(lambda e: e.memset(AP(vf, 128, [[NB * 4 * 129, 128], [129, NB * 4]]), 1.0), [], [("vfam", j) for j in range(NB)])
            self.vfam_project(l, a, VM0, *slots[0], banks=PB)

        def proj(h):
            hb = h % 2
            qT, kT = qTs[hb], kTs[hb]
            slot, sk = slots[(h + 1) % 2]
            nb = [0]

            def nbk():
                nb[0] += 1
                return PB[nb[0] % 3]
            for i, c0 in enumerate((QM0 + 128 * h, KM0 + 128 * h, OM0 + 128 * h)):
                self.wload(slot[:, :, i * 128:(i + 1) * 128], w_in[:, c0:c0 + 128].rearrange("(k p) n -> p k n", p=128), (sk, i), "s%d_%d" % (sk[1], i))
            for tt in range(NT):
                tsl = slice(tt * TT, (tt + 1) * TT)
                bk = nbk()
                self.mm(ps[:, bk, :], [(slot[:, k, 256:384], hT[:, k, tsl]) for k in range(KC)],
                        reads=[(sk, 2), ("hT", tt)], writes=[self.bank(bk)], name="oproj")
                self.act(mixT[:, h, tsl], ps[:, bk, :], AF.Tanh, [self.bank(bk)], [("mixT", h, tt)], scale=0.5)
            for i, dstT in enumerate((qT, kT)):
                cch = h if i == 0 else 4 + h
                for j in range(4):
                    self.dve((lambda j, cch: lambda e: e.tensor_scalar(out=diag[:, j, :], in0=c["c_identb"][:], scalar1=cw[:, l, j, cch:cch + 1],
                                                                        scalar2=None, op0=ALU.mult))(j, cch),
                             ["c_identb", ("cw", l, j)], ["diag"])
                for tt in range(NT):
                    tsl = slice(tt * TT, (tt + 1) * TT)
                    bk = nbk()
                    self.mm(ps[:, bk, :], [(slot[:, k, i * 128:(i + 1) * 128], hT[:, k, tsl]) for k in range(KC)],
                            reads=[(sk, i), ("hT", tt)], writes=[self.bank(bk)], name="qkproj")
                    self.evac(tt, ubuf[:, 3 + tt * TT:3 + (tt + 1) * TT], ps[:, bk, :], [self.bank(bk)], ["ubuf"])
                for tt in range(NT):
                    tsl = slice(tt * TT, (tt + 1) * TT)
                    bk = nbk()
                    self.mm(ps[:, bk, :], [(diag[:, j, :], ubuf[:, tt * TT + j:tt * TT + j + TT]) for j in range(4)],
                            reads=["diag", "ubuf"], writes=[self.bank(bk)], name="conv")
                    self.act(dstT[:, tsl], ps[:, bk, :], AF.Silu, [self.bank(bk), ("cb", l)], [("qk", hb, i, tt)], bias=cb[:, l, cch:cch + 1])
            for jg in range(4):
                bk = nbk()
                self.transposes([(psb[bk][:, cc * 128:(cc + 1) * 128], kT[:, (4 * jg + cc) * 128:(4 * jg + cc + 1) * 128], c["c_identb"][:]) for cc in range(4)],
                                reads=[("qk", hb, 1, jg), "c_identb"], writes=[self.bank(bk)], name="ktr")
                self.dve((lambda jg, bk, h, hb: lambda e: e.tensor_tensor(
                    out=ktok[hb][:, 4 * jg:4 * jg + 4, :], in0=psb[bk][:, 0:512].rearrange("p (c d) -> p c d", c=4),
                    in1=AP(gt, 64 + 16 * jg + h, [[320, 128], [4, 4], [0, 128]]), op=ALU.mult))(jg, bk, h, hb),
                    [self.bank(bk), "gt"], [("ktok", hb, jg)])

        def rec(h):
            hb = h % 2
            qT, kT, kt = qTs[hb], kTs[hb], ktok[hb]
            self.dve(lambda e: e.memset(CTf[:], 0.0), [], ["CTf"])
            for j in range(NB):
                bsl = slice(j * 128, (j + 1) * 128)
                i = j % 2
                b0, b1, b2 = i, 2 + i, 4
                self.mm(ps[:, b0, 0:128], [(kT[:, bsl], qT[:, bsl])], reads=[("qk", hb, 0, j // 4), ("qk", hb, 1, j // 4)],
                        writes=[self.bank(b0)], name="ST")
                self.dve((lambda i, b0, j, h: lambda e: e.scalar_tensor_tensor(out=scT[i][:], in0=ps[:, b0, 0:128], scalar=gt[:, 0, 4 * j + h:4 * j + h + 1],
                                                                               in1=c["c_maskf"][:], op0=ALU.mult, op1=ALU.mult))(i, b0, j, h),
                         [self.bank(b0), "gt", "c_maskf"], [("scT", i)])
                pairs = [(scT[i][:], vf[:, j, h, :])]
                rd = [("scT", i), ("vfam", j)]
                if j > 0:
                    pairs.append((qT[:, bsl], CTb[:]))
                    rd += [("qk", hb, 0, j // 4), "CTb"]
                self.mm(ps[:, b1, 0:129], pairs, reads=rd, writes=[self.bank(b1)], name="acc")
                if j < NB - 1:
                    self.mm(ps[:, b2, 0:129], [(kt[:, j, :], vf[:, j, h, :])], reads=[("ktok", hb, j // 4), ("vfam", j)],
                            writes=[self.bank(b2)], name="upd")
                    self.dve((lambda b2, j, h: lambda e: e.scalar_tensor_tensor(out=CTf[:], in0=CTf[:], scalar=gt[:, 4, 4 * j + h:4 * j + h + 1],
                                                                                in1=ps[:, b2, 0:129], op0=ALU.mult, op1=ALU.add))(b2, j, h),
                             [self.bank(b2), "gt", "CTf"], ["CTf"])
                    self.act(CTb[:], CTf[:], AF.Copy, ["CTf"], ["CTb"])
                self.dve((lambda b1, j: lambda e: e.tensor_copy(out=accs[:, j, :], in_=ps[:, b1, 0:129]))(b1, j),
                         [self.bank(b1)], [("accs", j)])
                self.act(junk[:], accs[:, j, 0:128], AF.Square, [("accs", j)], ["junk", ("ssq", j)], accum=sm[:, 0, j:j + 1])
            ak = [("accs", j) for j in range(NB)]
            den = AP(accs, 128, [[NB * 129, 128], [129, NB]])
            w_tm = AP(gt, 2 * 64 + h, [[320, 128], [4, NB]])
            fl_tm = AP(gt, 3 * 64 + h, [[320, 128], [4, NB]])
            s_ = lambda r: sm[:, r, :]
            self.dve(lambda e, w_tm=w_tm: e.tensor_tensor(out=s_(1), in0=den, in1=w_tm, op=ALU.mult), ak + ["gt"], ["sm1"])
            self.dve(lambda e: e.tensor_scalar(out=s_(2), in0=s_(1), scalar1=-1.0, scalar2=None, op0=ALU.mult), ["sm1"], ["sm2"])
            self.dve(lambda e: e.tensor_tensor(out=s_(2), in0=s_(2), in1=s_(1), op=ALU.max), ["sm1", "sm2"], ["sm2"])
            self.dve(lambda e, fl_tm=fl_tm: e.tensor_tensor(out=s_(2), in0=s_(2), in1=fl_tm, op=ALU.max), ["sm2", "gt"], ["sm2"])
            self.dve(lambda e: e.reciprocal(out=s_(3), in_=s_(2)), ["sm2"], ["sm3"])
            self.dve(lambda e, w_tm=w_tm: e.tensor_tensor(out=s_(3), in0=s_(3), in1=w_tm, op=ALU.mult), ["sm3", "gt"], ["sm3"])
            self.dve(lambda e: e.tensor_tensor(out=s_(4), in0=s_(0), in1=s_(3), op=ALU.mult), ["sm3"] + [("ssq", j) for j in range(NB)], ["sm4"])
            self.dve(lambda e: e.tensor_tensor(out=s_(4), in0=s_(4), in1=s_(3), op=ALU.mult), ["sm3", "sm4"], ["sm4"])
            self.dve(lambda e: e.tensor_scalar(out=s_(4), in0=s_(4), scalar1=1.0 / 128, scalar2=None, op0=ALU.mult), ["sm4"], ["sm4"])
            self.rsqrt_small(s_(4), "sm4", HEPS)
            self.dve(lambda e: e.tensor_tensor(out=s_(5), in0=s_(4), in1=s_(3), op=ALU.mult), ["sm3", "sm4"], ["sm5"])
            for jg in range(4):
                hb_ = hn4[jg % 2]
                hk = ("hn4", jg % 2)
                self.dve((lambda jg, hb_: lambda e: e.tensor_tensor(out=hb_[:], in0=accs[:, 4 * jg:4 * jg + 4, 0:128],
                                                                    in1=AP(sm, 5 * NB + 4 * jg, [[6 * NB, 128], [1, 4], [0, 128]]), op=ALU.mult))(jg, hb_),
                         ak + ["sm5"], [hk])
                self.dve((lambda hb_, h: lambda e: e.tensor_tensor(out=hb_[:], in0=hb_[:], in1=AP(self.gm_bc, h * 128, [[512, 128], [0, 4], [1, 128]]), op=ALU.mult))(hb_, h),
                         [hk, "gm_bc"], [hk])
                bk = jg % 2
                self.transposes([(psb[bk][:, cc * 128:(cc + 1) * 128], hb_[:, cc, :], c["c_identb"][:]) for cc in range(4)],
                                reads=[hk, "c_identb"], writes=[self.bank(bk)], name="hntr")
                msl = mixT[:, h, jg * TT:(jg + 1) * TT]
                self.dve((lambda msl, bk: lambda e: e.scalar_tensor_tensor(out=msl, in0=msl, scalar=1.0, in1=psb[bk][:, 0:512], op0=ALU.add, op1=ALU.mult))(msl, bk),
                         [self.bank(bk), ("mixT", h, jg)], [("mixT", h, jg)])

        v_ops = P.capture(vpart)
        P.replay_merged([v_ops, gates_ops], weights=[1, 2])
        p_ops = [P.capture((lambda h: lambda: proj(h))(h)) for h in range(4)]
        r_ops = [P.capture((lambda h: lambda: rec(h))(h)) for h in range(4)]
        P.replay_merged([p_ops[0]])
        for h in range(4):
            if h < 3:
                P.replay_merged([r_ops[h], p_ops[h + 1]], weights=[2, 1])
            else:
                P.replay_merged([r_ops[h]])

    def wout_half(self, l, a, f, wo, wkey):
        ps, xT, mixT = self.ps, self.xT, a["mixT"]
        self.wload(wo[:], self.din["w_out"][l][f * 512:(f + 1) * 512, :].rearrange("(k p) n -> p k n", p=128),
                   [(wkey, i) for i in range(3)], "s%d_0" % wkey[1])
        wkey = (wkey, 0)
        n = 0
        for tt in range(NT):
            tsl = slice(tt * TT, (tt + 1) * TT)
            for dm in range(KC):
                bk = n % 8
                n += 1
                self.mm(ps[:, bk, :], [(wo[:, k, dm * 128:(dm + 1) * 128], mixT[:, k, tsl]) for k in range(4)],
                        reads=[wkey] + [("mixT", k, tt) for k in range(4)], writes=[self.bank(bk)], name="wout")
                xs = xT[:, dm, tsl]
                self.dve((lambda xs, bk: lambda e: e.tensor_tensor(out=xs, in0=xs, in1=ps[:, bk, :], op=ALU.add))(xs, bk),
                         [self.bank(bk), ("xT", tt)], [("xT", tt)])

    def attention(self, l, a):
        P, ar, ps, c = self.P, self.ar, self.ps, self.c
        hT, mixT, vf, qT, kT = self.hT, a["mixT"], a["vfam"], a["qT"], a["kT"]
        psb = [ps[:, b, :].bitcast(BF16) for b in range(8)]
        zb = [ar.alloc("zb", [128, TT], BF16) for _ in range(2)]
        t1 = ar.alloc("t1", [128, TT], F32)
        t2 = ar.alloc("t2", [128, TT], F32)
        Pt = [ar.alloc("Pt", [128, 2, TT], BF16) for _ in range(2)]
        Osb = ar.alloc("Osb", [128, 4, 2, 129], F32)
        o1 = ar.alloc("o1", [128, 4, 128], F32)
        o2 = ar.alloc("o2", [128, 4, 128], F32)
        on4 = ar.alloc("on4", [128, 4, 128], BF16)
        sa = ar.alloc("sa", [128, 6, 8], F32)
        slots = [(a["slotA"], ("slot", 0)), (a["slotB"], ("slot", 1))]
        self.vfam_project(l, a, VA0, *slots[0])
        w_in = self.din["w_in"][l]
        cosT, sinT = c["c_cos"], c["c_sin"]
        nlam = self.lam_s[:, 5:6]
        for h in range(4):
            slot, sk = slots[(h + 1) % 2]
            for i, c0 in enumerate((QA0 + 128 * h, KA0 + 128 * h)):
                self.wload(slot[:, :, i * 128:(i + 1) * 128], w_in[:, c0:c0 + 128].rearrange("(k p) n -> p k n", p=128), (sk, i), "s%d_%d" % (sk[1], i))
            for i, dstT in enumerate((qT, kT)):
                def a_proj(tt, i=i):
                    tsl = slice(tt * TT, (tt + 1) * TT)
                    b0 = (2 * tt) % 4
                    z = zb[tt % 2]
                    zk = ("zb", tt % 2)
                    self.mm(ps[:, b0, :], [(slot[:, k, i * 128:(i + 1) * 128], hT[:, k, tsl]) for k in range(KC)],
                            reads=[(sk, i), ("hT", tt)], writes=[self.bank(b0)], name="aproj")
                    self.dve((lambda z, b0: lambda e: e.tensor_copy(out=z[:], in_=ps[:, b0, :]))(z, b0), [self.bank(b0)], [zk])

                def a_rot(tt, i=i, dstT=dstT):
                    tsl = slice(tt * TT, (tt + 1) * TT)
                    b0 = (2 * tt) % 4
                    b1 = b0 + 1
                    z = zb[tt % 2]
                    zk = ("zb", tt % 2)
                    self.mm(ps[:, b1, :], [(c["c_perm"][:], z[:])], reads=[zk, "c_perm"], writes=[self.bank(b1)], name="rot")
                    self.dve((lambda b0, tsl: lambda e: e.tensor_tensor(out=t1[:], in0=ps[:, b0, :], in1=cosT[:, tsl], op=ALU.mult))(b0, tsl),
                             [self.bank(b0), "c_cos"], ["t1"])
                    self.dve((lambda b1, tsl: lambda e: e.tensor_tensor(out=t2[:], in0=ps[:, b1, :], in1=sinT[:, tsl], op=ALU.mult))(b1, tsl),
                             [self.bank(b1), "c_sin"], ["t2"])
                    self.dve((lambda dstT, tsl: lambda e: e.tensor_tensor(out=dstT[:, tsl], in0=t1[:], in1=t2[:], op=ALU.add))(dstT, tsl),
                             ["t1", "t2"], [("qk", i, tt)])
                a_proj(0)
                for tt in range(NT):
                    if tt + 1 < NT:
                        a_proj(tt + 1)
                    a_rot(tt)
            for qt in range(NT if self.att_level >= 2 else 0):
                nkb = 4 * qt + 4

                def step_qk(kb, qt=qt, h=h):
                    di = kb - 4 * qt
                    q0 = max(di, 0) * 128
                    N = TT - q0
                    pi = kb % 2
                    pt = Pt[pi]
                    sb0, sb1 = 2 * pi, 2 * pi + 1

                    def fqk(e):
                        i0 = e.matmul(ps[:, sb0, 0:N], kT[0:64, kb * 128:(kb + 1) * 128], qT[0:64, qt * TT + q0:(qt + 1) * TT], start=True, stop=True)
                        i1 = e.matmul(ps[:, sb1, 0:N], kT[64:128, kb * 128:(kb + 1) * 128], qT[64:128, qt * TT + q0:(qt + 1) * TT], start=True, stop=True)
                        return i0, i1
                    P.op("pe", fqk, reads=[("qk", 0, qt), ("qk", 1, kb // 4)], writes=[self.bank(sb0), self.bank(sb1)], name="qk")
                    self.act(pt[:, 0, 0:N], ps[:, sb0, 0:N], AF.Exp, [self.bank(sb0)], [("Pt", pi, 0)], scale=SC_A)
                    self.act(pt[:, 1, 0:N], ps[:, sb1, 0:N], AF.Exp, [self.bank(sb1)], [("Pt", pi, 1)], scale=SC_A)
                    if di >= 0:
                        self.dve(lambda e: e.tensor_tensor(out=pt[:, :, 0:128], in0=pt[:, :, 0:128],
                                                           in1=AP(c["c_maskb"], 0, [[128, 128], [0, 2], [1, 128]]), op=ALU.mult),
                                 [("Pt", pi, 0), ("Pt", pi, 1), "c_maskb"], [("Pt", pi, 0), ("Pt", pi, 1)])

                def step_pv(kb, qt=qt, h=h):
                    di = kb - 4 * qt
                    pi = kb % 2
                    pt = Pt[pi]
                    qs0 = max(di, 0)

                    def fpv(e):
                        first = last = None
                        for qs in range(qs0, 4):
                            for cc in range(2):
                                ins = e.matmul(ps[:, 4 + qs, cc * 129:(cc + 1) * 129], pt[:, cc, (qs - qs0) * 128:(qs - qs0 + 1) * 128], vf[:, kb, h, :],
                                               start=(kb == 0 and cc == 0), stop=(kb == 4 * qt + qs), skip_group_check=True)
                                if first is None:
                                    first = ins
                                last = ins
                        return first, last
                    P.op("pe", fpv, reads=[("Pt", pi, 0), ("Pt", pi, 1), ("vfam", kb)], writes=[self.bank(4 + qs) for qs in range(qs0, 4)], name="pv")
                    if di >= 0:
                        qs = di
                        self.dve(lambda e: e.tensor_copy(out=Osb[:, qs, :, :], in_=ps[:, 4 + qs, 0:258].rearrange("p (c e) -> p c e", c=2)),
                                 [self.bank(4 + qs)], ["Osb"])
                step_qk(0)
                for kb in range(nkb):
                    if kb + 1 < nkb:
                        step_qk(kb + 1)
                    if self.att_level >= 3:
                        step_pv(kb)
                if self.att_level < 4:
                    continue
                lcol = AP(Osb, 128, [[4 * 2 * 129, 128], [129, 8]])
                self.dve(lambda e: e.reciprocal(out=sa[:, 0, :], in_=lcol), ["Osb"], ["sa0"])
                self.dve(lambda e: e.tensor_scalar(out=sa[:, 1, :], in0=sa[:, 0, :], scalar1=nlam, scalar2=None, op0=ALU.mult), ["sa0", "nlam"], ["sa1"])
                self.dve(lambda e: e.tensor_tensor(out=o1[:], in0=Osb[:, :, 0, 0:128], in1=AP(sa, 0, [[48, 128], [2, 4], [0, 128]]), op=ALU.mult), ["Osb", "sa0"], ["o1"])
                self.dve(lambda e: e.tensor_tensor(out=o2[:], in0=Osb[:, :, 1, 0:128], in1=AP(sa, 8 + 1, [[48, 128], [2, 4], [0, 128]]), op=ALU.mult), ["Osb", "sa1"], ["o2"])
                self.dve(lambda e: e.tensor_tensor(out=o1[:], in0=o1[:], in1=o2[:], op=ALU.add), ["o1", "o2"], ["o1"])
                self.dve(lambda e: e.tensor_tensor(out=o2[:], in0=o1[:], in1=o1[:], op=ALU.mult), ["o1", "o2"], ["o2"])
                self.dve(lambda e: e.tensor_reduce(out=sa[:, 2, 0:4], in_=o2[:], axis=AX.X, op=ALU.add), ["o2"], ["sa2"])
                self.dve(lambda e: e.tensor_scalar(out=sa[:, 2, 0:4], in0=sa[:, 2, 0:4], scalar1=1.0 / 128, scalar2=None, op0=ALU.mult), ["sa2"], ["sa2"])
                self.rsqrt_small(sa[:, 2, 0:4], "sa2", HEPS)
                self.dve(lambda e: e.tensor_tensor(out=o1[:], in0=o1[:], in1=AP(sa, 16, [[48, 128], [1, 4], [0, 128]]), op=ALU.mult), ["o1", "sa2"], ["o1"])
                self.dve((lambda h: lambda e: e.tensor_tensor(out=on4[:], in0=o1[:], in1=AP(self.gd_bc, h * 128, [[512, 128], [0, 4], [1, 128]]), op=ALU.mult))(h),
                         ["o1", "gd_bc"], ["on4"])
                bk = 2 * (qt % 2)
                self.transposes([(psb[bk][:, cc * 128:(cc + 1) * 128], on4[:, cc, :], c["c_identb"][:]) for cc in range(4)],
                                reads=["on4", "c_identb"], writes=[self.bank(bk)], name="ontr")
                self.act(mixT[:, h, qt * TT:(qt + 1) * TT], psb[bk][:, 0:512], AF.Copy, [self.bank(bk)], [("mixT", h, qt)])

    def ffn(self, l):
        P, ar, ps = self.P, self.ar, self.ps
        hT, xT, din = self.hT, self.xT, self.din
        wg = [ar.alloc("fwg", [128, KC, 6 * 128], BF16) for _ in range(2)]
        wu = [ar.alloc("fwu", [128, KC, 6 * 128], BF16) for _ in range(2)]
        wd = [ar.alloc("fwd", [128, 6, D], BF16) for _ in range(2)]
        actb = [ar.alloc("actb", [128, 6, TT], BF16) for _ in range(2)]
        sg = [ar.alloc("sg", [128, TT], BF16) for _ in range(2)]
        n_e = 0
        n_d = [0]
        step = 0
        pending = None

        def down(i, ncn, tt, ab, abk):
            tsl = slice(tt * TT, (tt + 1) * TT)
            for dm in range(KC):
                bk = 4 + n_d[0] % 4
                n_d[0] += 1
                self.mm(ps[:, bk, :], [(wd[i][:, hc, dm * 128:(dm + 1) * 128], ab[:, hc, :]) for hc in range(ncn)],
                        reads=[("fwd", i)] + [("actb", abk, hc) for hc in range(ncn)], writes=[self.bank(bk)], name="down")
                xs = xT[:, dm, tsl]
                self.dve((lambda xs, bk: lambda e: e.tensor_tensor(out=xs, in0=xs, in1=ps[:, bk, :], op=ALU.add))(xs, bk),
                         [self.bank(bk), ("xT", tt)], [("xT", tt)])

        for gi, (c0, ncn) in enumerate(FFN_GROUPS):
            i = gi % 2
            self.wload(wg[i][:, :, 0:ncn * 128], din["w_gate"][l][:, c0 * 128:(c0 + ncn) * 128].rearrange("(k p) n -> p k n", p=128), ("fwg", i), "fwg%d" % i)
            self.wload(wu[i][:, :, 0:ncn * 128], din["w_up"][l][:, c0 * 128:(c0 + ncn) * 128].rearrange("(k p) n -> p k n", p=128), ("fwu", i), "fwu%d" % i)
            self.wload(wd[i][:, 0:ncn, :], din["w_down"][l][c0 * 128:(c0 + ncn) * 128, :].rearrange("(c p) n -> p c n", p=128), ("fwd", i), "fwd%d" % i)
            for tt in range(NT):
                tsl = slice(tt * TT, (tt + 1) * TT)
                abk = step % 2
                ab = actb[abk]
                step += 1
                for hc in range(ncn):
                    bg_, bu_ = (0, 1) if n_e % 2 == 0 else (2, 3)
                    s_ = sg[n_e % 2]
                    sgk = ("sg", n_e % 2)
                    n_e += 1
                    self.mm(ps[:, bg_, :], [(wg[i][:, k, hc * 128:(hc + 1) * 128], hT[:, k, tsl]) for k in range(KC)],
                            reads=[("fwg", i), ("hT", tt)], writes=[self.bank(bg_)], name="gate")
                    self.mm(ps[:, bu_, :], [(wu[i][:, k, hc * 128:(hc + 1) * 128], hT[:, k, tsl]) for k in range(KC)],
                            reads=[("fwu", i), ("hT", tt)], writes=[self.bank(bu_)], name="up")
                    self.act(s_[:], ps[:, bg_, :], AF.Silu, [self.bank(bg_)], [sgk])
                    self.dve((lambda ab, hc, s_, bu_: lambda e: e.tensor_tensor(out=ab[:, hc, :], in0=s_[:], in1=ps[:, bu_, :], op=ALU.mult))(ab, hc, s_, bu_),
                             [sgk, self.bank(bu_)], [("actb", abk, hc)])
                if pending is not None:
                    down(*pending)
                pending = (i, ncn, tt, ab, abk)
        down(*pending)

    def store(self, sq, dst_dram, normalize):
        P, ar, ps, c = self.P, self.ar, self.ps, self.c
        m = ar.mark()
        yT = [ar.alloc("yT", [128, KC, TT], F32) for _ in range(2)]
        ob = [ar.alloc("ob", [128, D], F32) for _ in range(2)]
        nb_ = 0
        gf = self.gf
        for tt in range(NT):
            if normalize:
                self.norm(lambda cc: gf[:, cc:cc + 1], "gf", out_f32=lambda tt, cc: (yT[tt % 2][:, cc, :], ("yT", tt % 2)), tts=[tt])
            for bb in range(4):
                blk = tt * 4 + bb
                o_ = ob[nb_ % 2]
                ok = ("ob", nb_ % 2)
                nb_ += 1
                for half in range(2):
                    bk = (2 * blk + half) % 8
                    if normalize:
                        srcs = [yT[tt % 2][:, half * 4 + cc, bb * 128:(bb + 1) * 128] for cc in range(4)]
                        rk = [("yT", tt % 2)]
                    else:
                        srcs = [self.xT[:, half * 4 + cc, blk * 128:(blk + 1) * 128] for cc in range(4)]
                        rk = [("xT", tt)]
                    self.transposes([(ps[:, bk, cc * 128:(cc + 1) * 128], srcs[cc], c["c_identf"][:]) for cc in range(4)],
                                    reads=rk + ["c_identf"], writes=[self.bank(bk)], name="otr")
                    self.evac(half, o_[:, half * 512:(half + 1) * 512], ps[:, bk, :], [self.bank(bk)], [ok])
                P.op("sp", (lambda o_, blk: lambda e: e.dma_start(out=dst_dram[sq, blk * 128:(blk + 1) * 128, :], in_=o_[:]))(o_, blk),
                     reads=[ok], name="ostore", dma="st%d" % ((nb_ - 1) % 2))
        P.barrier()
        ar.reset(m)

    def build(self, stage=99):
        P, ar = self.P, self.ar
        self.setup()
        for sq in range(self.nseq):
            self.load_x(sq)
            for l in self.layers:
                if stage < 1:
                    break
                self.layer_params(l)
                g1 = self.g1
                self.norm((lambda l: lambda cc: g1[:, l, cc:cc + 1])(l), ("g1", l))
                P.barrier()
                self.dump("hT", self.hT, [("hT", t) for t in range(NT)])
                m = ar.mark()
                a = self.mixer_alloc()
                m2 = ar.mark()
                if stage >= 3:
                    self.mlstm(l, a)
                    self.dump("mixM", a["mixT"], [("mixT", h, t) for h in range(4) for t in range(NT)])
                if stage >= 4:
                    self.wout_half(l, a, 0, a["woB"], ("slot", 1))
                P.barrier()
                ar.reset(m2)
                if stage >= 5:
                    self.attention(l, a)
                    self.dump("mixA", a["mixT"], [("mixT", h, t) for h in range(4) for t in range(NT)])
                if stage >= 6:
                    self.wout_half(l, a, 1, a["woB"], ("slot", 1))
                    self.dump("xmid", self.xT, [("xT", t) for t in range(NT)])
                P.barrier()
                ar.reset(m)
                if stage >= 7:
                    g2 = self.g2
                    self.norm((lambda l: lambda cc: g2[:, l, cc:cc + 1])(l), ("g2", l))
                    P.barrier()
                    self.ffn(l)
                    P.barrier()
                ar.reset(m)
            self.store(sq, self.out, self.final_norm)
        P.emit()
        return self.nc


_CONSTS = None


def _run(nc_inputs_list, layers, final_norm):
    b = Builder(layers, nseq=2, final_norm=final_norm)
    nc = b.build()
    res = run_bass_kernel_spmd(nc, nc_inputs_list, core_ids=list(range(8)))
    return [r["out"] for r in res.results]


def kernel(**inputs):
    global _CONSTS
    if _CONSTS is None:
        _CONSTS = host_constants()
    x = np.ascontiguousarray(inputs["x"], dtype=np.float32)
    params = {n: np.ascontiguousarray(inputs[n], dtype=np.float32) for n, _ in PARAM_SPECS}
    shards = [x[2 * i:2 * i + 2] for i in range(8)]
    in_maps = []
    for i in range(8):
        mp = {"x": shards[i]}
        mp.update(params)
        mp.update(_CONSTS)
        in_maps.append(mp)
    outs = _run(in_maps, list(range(DEPTH)), True)
    return np.concatenate(outs, axis=0)
```

```python
import math
import contextlib
import numpy as np
import ml_dtypes
import concourse.bass as bass
import concourse.mybir as mybir
from concourse.bass_utils import run_bass_kernel_spmd

F32 = mybir.dt.float32
BF16 = mybir.dt.bfloat16
AF = mybir.ActivationFunctionType
ALU = mybir.AluOpType
AX = mybir.AxisListType

ENGS = ("pe", "act", "dve", "pool", "sp")

D = 1024
KC = 8
S = 2048
NT = 4
TT = 512
NB = 16
DEPTH = 4
INW = 3592
QM0, KM0, VM0, OM0, G0, QA0, KA0, VA0 = 0, 512, 1024, 1536, 2048, 2056, 2568, 3080
FF = 2816
FC = 22
EPS = 1e-6
HEPS = 1e-5
SC_M = 128.0 ** -0.5
SC_A = 64.0 ** -0.5
FFN_GROUPS = [(0, 6), (6, 6), (12, 5), (17, 5)]


class Op:
    __slots__ = ("eng", "fn", "deps", "sig", "dma_sem", "dma_val", "name")

    def __init__(self, eng, fn, name=""):
        self.eng = eng
        self.fn = fn
        self.deps = set()
        self.sig = None
        self.dma_sem = None
        self.dma_val = None
        self.name = name


class Prog:
    def __init__(self, nc):
        self.nc = nc
        self.ops = {e: [] for e in ENGS}
        self.all_ops = []
        self.last_writer = {}
        self.readers = {}
        self.dma_sems = {}
        self.barrier_deps = []

    def op(self, eng, fn, reads=(), writes=(), name="", dma=None, dma_total=False):
        o = Op(eng, fn, name)
        if dma is not None:
            ent = self.dma_sems.setdefault(dma, [eng, 0, dma_total])
            assert ent[0] == eng
            ent[1] += 16
            o.dma_sem = dma
            o.dma_val = ent[1]
        reads = list(reads)
        writes = list(writes)
        ps_reads = [k for k in reads if isinstance(k, tuple) and len(k) == 2 and k[0] == "ps"]
        if ps_reads:
            reads = [k for k in reads if k not in ps_reads]
            writes = writes + [("psr", k[1]) for k in ps_reads]
            for k in ps_reads:
                w = self.last_writer.get(k)
                if w is not None:
                    o.deps.add(w)
                self.readers.setdefault(k, []).append(o)
        for k in reads:
            w = self.last_writer.get(k)
            if w is not None:
                o.deps.add(w)
            self.readers.setdefault(k, []).append(o)
        for k in writes:
            w = self.last_writer.get(k)
            if w is not None:
                o.deps.add(w)
            for r in self.readers.get(k, ()):
                o.deps.add(r)
            self.readers[k] = []
            self.last_writer[k] = o
        for b in self.barrier_deps:
            o.deps.add(b)
        o.deps.discard(o)
        self.ops[eng].append(o)
        self.all_ops.append(o)
        return o

    def capture(self, fn):
        rec = []
        real = self.op
        self.op = lambda *a, **k: rec.append((a, k))
        try:
            fn()
        finally:
            self.op = real
        return rec

    def replay_merged(self, streams, weights=None):
        weights = weights or [1] * len(streams)
        idx = [0] * len(streams)
        while any(idx[i] < len(s) for i, s in enumerate(streams)):
            for i, s in enumerate(streams):
                for _ in range(weights[i]):
                    if idx[i] < len(s):
                        a, k = s[idx[i]]
                        idx[i] += 1
                        self.op(*a, **k)

    def barrier(self):
        deps = [self.ops[e][-1] for e in ENGS if self.ops[e]]
        last_dma = {}
        for o in self.all_ops:
            if o.dma_sem is not None:
                last_dma[o.dma_sem] = o
        self.barrier_deps = deps + list(last_dma.values())

    def emit(self):
        nc = self.nc
        referenced = set()
        for o in self.all_ops:
            for d in o.deps:
                if d.dma_sem is None and not (d.eng == o.eng == "pe"):
                    referenced.add(d)
        for e in ENGS:
            c = 0
            for o in self.ops[e]:
                if o in referenced:
                    c += 1
                    o.sig = c
        with contextlib.ExitStack() as st:
            esem = {e: st.enter_context(nc.semaphore("S_" + e)) for e in ENGS}
            dsem = {n: st.enter_context(nc.semaphore("D_" + n)) for n in self.dma_sems}
            block = st.enter_context(nc.Block())

            def run(e, eng):
                waited = {}
                for o in self.ops[e]:
                    need = {}
                    for d in o.deps:
                        if d.dma_sem is not None:
                            key = ("d", d.dma_sem)
                            val = self.dma_sems[d.dma_sem][1] if self.dma_sems[d.dma_sem][2] else d.dma_val
                        else:
                            if d.eng == e and e == "pe":
                                continue
                            if d.sig is None:
                                continue
                            key = ("e", d.eng)
                            val = d.sig
                        if waited.get(key, 0) >= val:
                            continue
                        if need.get(key, 0) < val:
                            need[key] = val
                    items = list(need.items())
                    for key, val in items:
                        waited[key] = val
                    sems = [(dsem[k[1]] if k[0] == "d" else esem[k[1]], v) for k, v in items]
                    for s_, v in sems[:-1]:
                        eng.wait_ge(s_, v)
                    r = o.fn(eng)
                    first, last = r if isinstance(r, tuple) else (r, r)
                    if sems:
                        s_, v = sems[-1]
                        first._wait_ge(s_, v)
                    if o.dma_sem is not None:
                        last.then_inc(dsem[o.dma_sem], 16)
                    elif o.sig is not None:
                        last.then_inc(esem[e], 1)
                for n, (owner, cnt, _tot) in self.dma_sems.items():
                    if owner == e:
                        eng.wait_ge(dsem[n], cnt)

            @block.tensor
            def _(eng):
                run("pe", eng)

            @block.scalar
            def _(eng):
                run("act", eng)

            @block.vector
            def _(eng):
                run("dve", eng)

            @block.gpsimd
            def _(eng):
                run("pool", eng)

            @block.sync
            def _(eng):
                run("sp", eng)


class Arena:
    def __init__(self, nc, base=16512, top=229344):
        self.nc = nc
        self.cur = base
        self.top = top
        self.n = 0

    def alloc(self, name, shape, dtype):
        sz = int(np.prod(shape[1:])) * mybir.dt.size(dtype)
        off = (self.cur + 31) // 32 * 32
        assert off + sz <= self.top, f"SBUF arena overflow at {name}: need {off + sz - self.top} more bytes"
        self.cur = off + sz
        self.n += 1
        return self.nc.alloc_sbuf_tensor_at(f"{name}_{self.n}", list(shape), dtype, offset=off)

    def mark(self):
        return self.cur

    def reset(self, m):
        self.cur = m


def AP(t, off, dims):
    return bass.AP(t, off, [list(d) for d in dims])


def host_constants():
    c = {}
    I = np.eye(128, dtype=np.float32)
    c["c_identb"] = I.astype(ml_dtypes.bfloat16)
    c["c_identf"] = I
    c["c_onesb"] = np.ones((128, 128), dtype=ml_dtypes.bfloat16)
    s_ = np.arange(128)[:, None]
    t_ = np.arange(128)[None, :]
    tri = (s_ <= t_).astype(np.float32)
    c["c_maskf"] = (tri * SC_M).astype(np.float32)
    c["c_maskb"] = tri.astype(ml_dtypes.bfloat16)
    perm = np.zeros((128, 128), dtype=np.float32)
    for r in range(128):
        i = r % 64
        if i < 32:
            perm[r + 32, r] = -1.0
        else:
            perm[r - 32, r] = 1.0
    c["c_perm"] = perm.astype(ml_dtypes.bfloat16)
    pos = np.arange(S, dtype=np.float32)
    inv = (np.float32(10000.0) ** (-np.arange(0, 64, 2, dtype=np.float32) / np.float32(64))).astype(np.float32)
    ang = (pos[:, None] * inv[None, :]).astype(np.float32)
    cosr = np.cos(ang.astype(np.float64)).T
    sinr = np.sin(ang.astype(np.float64)).T
    f_of_row = np.arange(128) % 32
    c["c_cos"] = cosr[f_of_row].astype(np.float32)
    c["c_sin"] = sinr[f_of_row].astype(np.float32)
    q = np.arange(64)
    jq, hq = q // 4, q % 4
    c["c_mcarry"] = ((hq[:, None] == hq[None, :]) & (jq[:, None] < jq[None, :])).astype(ml_dtypes.bfloat16)
    sel = (hq[:, None] == np.arange(4)[None, :]).astype(np.float32)
    c["c_sel"] = sel.astype(ml_dtypes.bfloat16)
    c["c_ohj"] = (jq[:, None] == np.arange(16)[None, :]).astype(np.float32)
    c["c_selT"] = np.ascontiguousarray(sel.T).astype(ml_dtypes.bfloat16)
    return c


CONST_SPECS = [("c_identb", [128, 128], BF16), ("c_identf", [128, 128], F32), ("c_onesb", [128, 128], BF16),
               ("c_maskf", [128, 128], F32), ("c_maskb", [128, 128], BF16), ("c_perm", [128, 128], BF16),
               ("c_cos", [128, S], F32), ("c_sin", [128, S], F32), ("c_mcarry", [64, 64], BF16),
               ("c_sel", [64, 4], BF16), ("c_ohj", [64, 16], F32), ("c_selT", [4, 64], BF16)]

PARAM_SPECS = [("g_mix", [DEPTH, D]), ("w_in", [DEPTH, D, INW]), ("conv_w", [DEPTH, 4, D]), ("conv_b", [DEPTH, D]),
               ("b_gates", [DEPTH, 8]), ("g_mlstm_head", [DEPTH, 512]), ("lam_q1", [DEPTH, 64]),
               ("lam_k1", [DEPTH, 64]), ("lam_q2", [DEPTH, 64]), ("lam_k2", [DEPTH, 64]),
               ("g_diff_head", [DEPTH, 512]), ("w_out", [DEPTH, D, D]), ("g_ffn", [DEPTH, D]),
               ("w_gate", [DEPTH, D, FF]), ("w_up", [DEPTH, D, FF]), ("w_down", [DEPTH, FF, D]), ("g_final", [D])]


class Builder:
    def __init__(self, layers, nseq=2, final_norm=True, debug=None, pdepth=DEPTH):
        self.layers = list(layers)
        self.nseq = nseq
        self.final_norm = final_norm
        self.debug = {}
        self.debug_names = set(debug or [])
        nc = bass.Bass("TRN2", target_bir_lowering=False)
        self.nc = nc
        self.P = Prog(nc)
        self.ar = Arena(nc)
        self.din = {}
        self.din["x"] = nc.dram_tensor("x", [nseq, S, D], F32, kind="ExternalInput").ap()
        self.pdepth = pdepth
        for n, shp in PARAM_SPECS:
            shp = list(shp)
            if n != "g_final":
                shp[0] = pdepth
            self.din[n] = nc.dram_tensor(n, shp, F32, kind="ExternalInput").ap()
        for n, shp, dt_ in CONST_SPECS:
            self.din[n] = nc.dram_tensor(n, list(shp), dt_, kind="ExternalInput").ap()
        self.out = nc.dram_tensor("out", [nseq, S, D], F32, kind="ExternalOutput").ap()
        self.dbg_out = {}
        for n, shp in self.debug.items():
            self.dbg_out[n] = nc.dram_tensor(n, list(shp), F32, kind="ExternalOutput").ap()
        self.ps = nc.alloc_psum_tensor("ps", [128, 8, 512], F32)
        self.wq_n = 0
        self.uid = 0
        self.att_level = 9

    def dump(self, name, t, keys):
        if name not in self.debug_names:
            return
        shp = [int(v) for v in t.shape]
        dt_ = t.dtype
        d = self.nc.dram_tensor("dbg_" + name, shp, dt_, kind="ExternalOutput").ap()
        self.P.op("sp", lambda e: e.dma_start(out=d, in_=t[:]), reads=keys, name="dump", dma="dbg_" + name)

    def bank(self, b):
        return ("ps", b)

    def mm(self, out_ap, pairs, reads, writes, name="mm", start=True, stop=True, skip=False):
        n = len(pairs)

        def fn(e):
            first = last = None
            for i, (l, r) in enumerate(pairs):
                ins = e.matmul(out_ap, l, r, start=(start and i == 0), stop=(stop and i == n - 1),
                               skip_group_check=skip)
                if first is None:
                    first = ins
                last = ins
            return first, last
        return self.P.op("pe", fn, reads=reads, writes=writes, name=name)

    def transposes(self, items, reads, writes, name="tr"):
        def fn(e):
            first = last = None
            for (o, i, idn) in items:
                ins = e.transpose(out=o, in_=i, identity=idn)
                if first is None:
                    first = ins
                last = ins
            return first, last
        return self.P.op("pe", fn, reads=reads, writes=writes, name=name)

    def wload(self, dst_ap, src_ap, keys, sem, name="wload", total=False):
        if not isinstance(keys, list):
            keys = [keys]

        def fn(e):
            return e.dma_start(out=dst_ap, in_=src_ap)
        return self.P.op("pool", fn, writes=keys, name=name, dma=sem, dma_total=total)

    def sload(self, dst_ap, src_ap, key, sem="setup", name="sload", nonc=False, total=True):
        def fn(e):
            if nonc:
                return e.dma_start(out=dst_ap, in_=src_ap, allow_slow_non_contiguous=True)
            return e.dma_start(out=dst_ap, in_=src_ap)
        return self.P.op("sp", fn, writes=[key], name=name, dma=sem, dma_total=total)

    def setup(self):
        ar, P, din = self.ar, self.P, self.din
        self.xT = ar.alloc("xT", [128, KC, S], F32)
        self.hT = ar.alloc("hT", [128, KC, S], BF16)
        self.c = {}
        for n, shp, dt_ in CONST_SPECS:
            if n in ("c_cos", "c_sin"):
                t = ar.alloc(n, shp, BF16)
                self.c[n] = t
                continue
            t = ar.alloc(n, shp, dt_)
            self.c[n] = t
            self.sload(t[:], din[n], key=n)
        self.g1 = ar.alloc("g1", [128, DEPTH, KC], F32)
        self.g2 = ar.alloc("g2", [128, DEPTH, KC], F32)
        self.gf = ar.alloc("gf", [128, KC], F32)
        self.cw = ar.alloc("cw", [128, DEPTH, 4, KC], F32)
        self.cb = ar.alloc("cb", [128, DEPTH, KC], F32)
        self.bg = ar.alloc("bg", [128, DEPTH, 8], F32)
        self.lamv = ar.alloc("lamv", [128, 4, DEPTH, 64], F32)
        for l in range(self.pdepth):
            self.sload(self.g1[:, l, :], din["g_mix"][l].rearrange("(c p) -> p c", p=128), key=("g1", l), nonc=True)
            self.sload(self.g2[:, l, :], din["g_ffn"][l].rearrange("(c p) -> p c", p=128), key=("g2", l), nonc=True)
            for j in range(4):
                self.sload(self.cw[:, l, j, :], din["conv_w"][l, j].rearrange("(c p) -> p c", p=128), key=("cw", l, j), nonc=True)
            self.sload(self.cb[:, l, :], din["conv_b"][l].rearrange("(c p) -> p c", p=128), key=("cb", l), nonc=True)
        self.sload(self.gf[:], din["g_final"].rearrange("(c p) -> p c", p=128), key="gf", nonc=True)
        bgd = din["b_gates"]
        self.sload(self.bg[:, 0:self.pdepth, :], AP(bgd.tensor, 0, [[0, 128], [8, self.pdepth], [1, 8]]), key="bg")
        for i, n in enumerate(("lam_q1", "lam_k1", "lam_q2", "lam_k2")):
            self.sload(self.lamv[:, i, 0:self.pdepth, :], AP(din[n].tensor, 0, [[0, 128], [64, self.pdepth], [1, 64]]), key=("lamv", i))
        self.wload(self.c["c_cos"][:], din["c_cos"], "c_cos", "wsetup", total=True)
        self.wload(self.c["c_sin"][:], din["c_sin"], "c_sin", "wsetup", total=True)
        self.gm_bc = ar.alloc("gm_bc", [128, 512], F32)
        self.gd_bc = ar.alloc("gd_bc", [128, 512], F32)
        self.lam_s = ar.alloc("lam_s", [128, 8], F32)
        self.lam_t = ar.alloc("lam_t", [128, 2, 64], F32)
        self.phase_base = ar.mark()

    def load_x(self, sq):
        P, ar = self.P, self.ar
        m = ar.mark()
        xin = [ar.alloc("xin", [128, D], F32) for _ in range(2)]
        xd = self.din["x"]
        for b in range(NB):
            t = xin[b % 2]
            self.sload(t[:], xd[sq, b * 128:(b + 1) * 128, :], key=("xin", b % 2), sem="xin%d" % (b % 2), total=False)
            for half in range(2):
                bk = (2 * b + half) % 8
                items = [(self.ps[:, bk, cc * 128:(cc + 1) * 128], t[:, (half * 4 + cc) * 128:(half * 4 + cc + 1) * 128],
                          self.c["c_identf"][:]) for cc in range(4)]
                self.transposes(items, reads=[("xin", b % 2), "c_identf"], writes=[self.bank(bk)], name="xtr")
                src = AP(self.ps, bk * 512, [[4096, 128], [128, 4], [1, 128]])
                dst = self.xT[:, half * 4:(half + 1) * 4, b * 128:(b + 1) * 128]
                eng = "dve" if half == 0 else "act"
                if eng == "dve":
                    P.op("dve", (lambda d_, s_: lambda e: e.tensor_copy(out=d_, in_=s_))(dst, src),
                         reads=[self.bank(bk)], writes=[("xT", b // 4)])
                else:
                    P.op("act", (lambda d_, s_: lambda e: e.activation(out=d_, in_=s_, func=AF.Copy))(dst, src),
                         reads=[self.bank(bk)], writes=[("xT", b // 4)])
        P.barrier()
        ar.reset(m)

    def norm(self, gt_ap_fn, gkey, out_f32=None, tts=None):
        P, ar = self.P, self.ar
        m = ar.mark()
        sq = [ar.alloc("sq", [128, KC, TT], BF16) for _ in range(2)]
        rs = [ar.alloc("rs", [128, TT], F32) for _ in range(2)]
        for tt in (range(NT) if tts is None else tts):
            i = tt % 2
            tsl = slice(tt * TT, (tt + 1) * TT)
            P.op("act", (lambda o_, i_: lambda e: e.activation(out=o_, in_=i_, func=AF.Square))(sq[i][:], self.xT[:, :, tsl]),
                 reads=[("xT", tt)], writes=[("sq", i)])
            bk = i
            self.mm(self.ps[:, bk, :], [(self.c["c_onesb"][:], sq[i][:, c, :]) for c in range(KC)],
                    reads=[("sq", i), "c_onesb"], writes=[self.bank(bk)], name="ssq")
            P.op("act", (lambda o_, i_: lambda e: e.activation(out=o_, in_=i_, func=AF.Ln, scale=1.0 / D, bias=EPS))(rs[i][:], self.ps[:, bk, :]),
                 reads=[self.bank(bk)], writes=[("rs", i)])
            P.op("act", (lambda o_: lambda e: e.activation(out=o_, in_=o_, func=AF.Exp, scale=-0.5))(rs[i][:]),
                 reads=[("rs", i)], writes=[("rs", i)])
            for c in range(KC):
                if out_f32 is None:
                    dst = self.hT[:, c, tsl]
                    wk = ("hT", tt)
                else:
                    dst, wk = out_f32(tt, c)
                P.op("dve", (lambda d_, x_, g_, r_: lambda e: e.scalar_tensor_tensor(out=d_, in0=x_, scalar=g_, in1=r_, op0=ALU.mult, op1=ALU.mult))(
                    dst, self.xT[:, c, tsl], gt_ap_fn(c), rs[i][:]),
                    reads=[("xT", tt), ("rs", i), gkey], writes=[wk])
        ar.reset(m)

    def dve(self, fn, reads, writes, name="dve"):
        return self.P.op("dve", fn, reads=reads, writes=writes, name=name)

    def act(self, out, in_, func, reads, writes, scale=1.0, bias=0.0, accum=None, name="act"):
        def fn(e):
            kw = {}
            if accum is not None:
                kw["accum_out"] = accum
            return e.activation(out=out, in_=in_, func=func, bias=bias, scale=scale, **kw)
        return self.P.op("act", fn, reads=reads, writes=writes, name=name)

    def evac(self, k, out, in_, reads, writes):
        if k % 2 == 0:
            return self.act(out, in_, AF.Copy, reads, writes, name="evac")
        return self.dve(lambda e: e.tensor_copy(out=out, in_=in_), reads, writes, name="evac")

    def rsqrt_small(self, t_ap, key, bias):
        self.act(t_ap, t_ap, AF.Ln, [key], [key], bias=bias)
        self.act(t_ap, t_ap, AF.Exp, [key], [key], scale=-0.5)

    def split3(self, dst, src, np_, n, skey, dkey):
        ar = self.ar
        r1 = ar.alloc("sp_r1", [np_, n], F32)
        r2 = ar.alloc("sp_r2", [np_, n], F32)
        self.uid += 1
        k1, k2 = ("sp_r1", self.uid), ("sp_r2", self.uid)
        self.dve(lambda e: e.tensor_copy(out=dst[:, 0, :], in_=src), [skey], [(dkey, 0)])
        self.dve(lambda e: e.tensor_tensor(out=r1[:], in0=src, in1=dst[:, 0, :], op=ALU.subtract), [skey, (dkey, 0)], [k1])
        self.dve(lambda e: e.tensor_copy(out=dst[:, 1, :], in_=r1[:]), [k1], [(dkey, 1)])
        self.dve(lambda e: e.tensor_tensor(out=r2[:], in0=r1[:], in1=dst[:, 1, :], op=ALU.subtract), [k1, (dkey, 1)], [k2])
        self.dve(lambda e: e.tensor_copy(out=dst[:, 2, :], in_=r2[:]), [k2], [(dkey, 2)])
        return [(dkey, i) for i in range(3)]

    def layer_params(self, l):
        P, din = self.P, self.din
        lam_init = 0.8 - 0.6 * math.exp(-0.3 * l)
        self.sload(self.gm_bc[:], AP(din["g_mlstm_head"].tensor, l * 512, [[0, 128], [1, 512]]), key="gm_bc", sem="gm_bc", total=False)
        self.sload(self.gd_bc[:], AP(din["g_diff_head"].tensor, l * 512, [[0, 128], [1, 512]]), key="gd_bc", sem="gd_bc", total=False)
        gm, gd = self.gm_bc, self.gd_bc
        self.dve(lambda e: e.tensor_scalar(out=gm[:], in0=gm[:], scalar1=0.5, scalar2=None, op0=ALU.mult), ["gm_bc"], ["gm_bc"])
        self.dve(lambda e: e.tensor_scalar(out=gd[:], in0=gd[:], scalar1=1.0 - lam_init, scalar2=None, op0=ALU.mult), ["gd_bc"], ["gd_bc"])
        lv, lt, ls = self.lamv, self.lam_t, self.lam_s
        for i in range(2):
            self.dve((lambda i: lambda e: e.tensor_tensor(out=lt[:, i, :], in0=lv[:, 2 * i, l, :], in1=lv[:, 2 * i + 1, l, :], op=ALU.mult))(i),
                     [("lamv", 2 * i), ("lamv", 2 * i + 1)], ["lam_t"])
        self.dve(lambda e: e.tensor_reduce(out=ls[:, 0:2], in_=lt[:], axis=AX.X, op=ALU.add), ["lam_t"], ["lam_s"])
        self.act(ls[:, 2:4], ls[:, 0:2], AF.Exp, ["lam_s"], ["lam_s"])
        self.dve(lambda e: e.tensor_tensor(out=ls[:, 4:5], in0=ls[:, 3:4], in1=ls[:, 2:3], op=ALU.subtract), ["lam_s"], ["lam_s"])
        self.dve(lambda e: e.tensor_scalar(out=ls[:, 5:6], in0=ls[:, 4:5], scalar1=-lam_init, scalar2=None, op0=ALU.add), ["lam_s"], ["nlam"])

    def mixer_alloc(self):
        ar = self.ar
        a = {}
        a["mixT"] = ar.alloc("mixT", [128, 4, S], BF16)
        a["vfam"] = ar.alloc("vfam", [128, NB, 4, 129], BF16)
        off0 = (ar.cur + 31) // 32 * 32
        a["slotA"] = ar.alloc("slotA", [128, KC, 512], BF16)
        off1 = (ar.cur + 31) // 32 * 32
        a["slotB"] = ar.alloc("slotB", [128, KC, 512], BF16)
        a["woA"] = self.nc.alloc_sbuf_tensor_at("woA_%d" % ar.n, [128, 4, D], BF16, offset=off0)
        a["woB"] = self.nc.alloc_sbuf_tensor_at("woB_%d" % ar.n, [128, 4, D], BF16, offset=off1)
        a["qT"] = ar.alloc("qT", [128, S], BF16)
        a["kT"] = ar.alloc("kT", [128, S], BF16)
        a["wg"] = ar.alloc("wg", [128, KC, 8], BF16)
        a["gt"] = ar.alloc("gt", [128, 5, 64], F32)
        return a

    def gates(self, l, a):
        P, ar, ps, c = self.P, self.ar, self.ps, self.c
        m = ar.mark()
        wg, gt = a["wg"], a["gt"]
        wgf = ar.alloc("wgf", [128, KC, 8], F32)
        self.sload(wgf[:], self.din["w_in"][l][:, G0:G0 + 8].rearrange("(k p) n -> p k n", p=128), key="wgf", sem="wgf", total=False)
        self.dve(lambda e: e.tensor_copy(out=wg[:], in_=wgf[:]), ["wgf"], ["wg"])
        hT = self.hT

        def fn(e):
            first = last = None
            for j in range(NB):
                for k in range(KC):
                    ins = e.matmul(ps[:, 0, j * 8:(j + 1) * 8], hT[:, k, j * 128:(j + 1) * 128], wg[:, k, :],
                                   start=(k == 0), stop=(k == KC - 1))
                    if first is None:
                        first = ins
                    last = ins
            return first, last
        P.op("pe", fn, reads=["wg"] + [("hT", t) for t in range(NT)], writes=[self.bank(0)], name="gates_mm")
        g_tm = ar.alloc("g_tm", [128, 2, 64], F32)
        bg = self.bg
        self.dve(lambda e: e.tensor_tensor(out=AP(g_tm, 0, [[128, 128], [4, NB], [64, 2], [1, 4]]),
                                           in0=AP(ps, 0 * 512, [[4096, 128], [8, NB], [4, 2], [1, 4]]),
                                           in1=AP(bg, l * 8, [[DEPTH * 8, 128], [0, NB], [4, 2], [1, 4]]), op=ALU.add),
                 [self.bank(0), "bg"], ["g_tm"])
        idf = c["c_identf"]
        self.transposes([(ps[0:64, 1, 0:128], g_tm[:, 0, :], idf[:]),
                         (ps[0:64, 1, 128:256], g_tm[:, 1, :], idf[:])],
                        reads=["g_tm", "c_identf"], writes=[self.bank(1)], name="gates_tr")
        T = lambda n, w=128: ar.alloc(n, [64, w], F32)
        ef, lsp, zer, cs, carry, Bn, A, cm, cmxJ = T("ef"), T("lsp"), T("zer"), T("cs"), T("carry", 1), T("Bn"), T("A"), T("cm"), T("cmxJ", 16)
        self.act(ef[:], ps[0:64, 1, 128:256], AF.Exp, [self.bank(1)], ["ef"], scale=-1.0)
        self.act(lsp[:], ef[:], AF.Ln, ["ef"], ["lsp"], bias=1.0)
        self.dve(lambda e: e.memset(zer[:], 0.0), [], ["zer"])
        self.dve(lambda e: e.tensor_tensor_scan(out=cs[:], data0=lsp[:], data1=zer[:], initial=0.0, op0=ALU.add, op1=ALU.add),
                 ["lsp", "zer"], ["cs"])
        tot3 = ar.alloc("tot3", [64, 3, 2], BF16)
        k3 = self.split3(tot3, cs[:, 126:128], 64, 2, "cs", "tot3")
        self.mm(ps[0:64, 2, 0:2], [(c["c_mcarry"][:], tot3[:, i, :]) for i in range(3)], reads=k3 + ["c_mcarry"], writes=[self.bank(2)], name="carry")
        self.dve(lambda e: e.tensor_copy(out=carry[:], in_=ps[0:64, 2, 1:2]), [self.bank(2)], ["carry"])
        self.dve(lambda e: e.tensor_scalar(out=Bn[:], in0=cs[:], scalar1=carry[:], scalar2=None, op0=ALU.add), ["cs", "carry"], ["Bn"])
        self.dve(lambda e: e.tensor_tensor(out=A[:], in0=ps[0:64, 1, 0:128], in1=Bn[:], op=ALU.add), [self.bank(1), "Bn"], ["A"])
        self.dve(lambda e: e.tensor_tensor_scan(out=cm[:], data0=A[:], data1=A[:], initial=0.0, op0=ALU.max, op1=ALU.max), ["A"], ["cm"])
        self.dve(lambda e: e.tensor_scalar(out=cmxJ[:], in0=c["c_ohj"][:], scalar1=cm[:, 127:128], scalar2=None, op0=ALU.mult),
                 ["cm", "c_ohj"], ["cmxJ"])
        cmx3 = ar.alloc("cmx3", [64, 3, 16], BF16)
        k3 = self.split3(cmx3, cmxJ[:], 64, 16, "cmxJ", "cmx3")
        self.mm(ps[0:4, 2, 8:24], [(c["c_sel"][:], cmx3[:, i, :]) for i in range(3)], reads=k3 + ["c_sel"], writes=[self.bank(2)], name="hj")
        hj = ar.alloc("hj", [4, 16], F32)
        Mp = ar.alloc("Mp", [4, 17], F32)
        self.dve(lambda e: e.tensor_copy(out=hj[:], in_=ps[0:4, 2, 8:24]), [self.bank(2)], ["hj"])
        self.dve(lambda e: e.memset(Mp[:], 0.0), [], ["Mp"])
        self.dve(lambda e: e.tensor_tensor_scan(out=Mp[:, 1:17], data0=hj[:], data1=hj[:], initial=0.0, op0=ALU.max, op1=ALU.max),
                 ["hj", "Mp"], ["Mp"])

        Mp3 = ar.alloc("Mp3", [4, 3, 18], BF16)
        self.dve(lambda e: e.memset(Mp3[:], 0.0), [], [("Mp3", i) for i in range(3)])
        Mp3v = AP(Mp3, 0, [[54, 4], [18, 3], [1, 17]])
        k3 = self.split3(Mp3v, Mp[:], 4, 17, "Mp", "Mp3")

        def fn2(e):
            first = last = None
            for (c0, o0) in ((0, 0), (1, 16)):
                for i in range(3):
                    ins = e.matmul(ps[0:64, 3, o0:o0 + 16], c["c_selT"][:], Mp3[:, i, c0:c0 + 16], start=(i == 0), stop=(i == 2))
                    if first is None:
                        first = ins
                    last = ins
            return first, last
        P.op("pe", fn2, reads=k3 + ["c_selT"], writes=[self.bank(3)], name="mq")
        tmp2 = ar.alloc("tmp2", [64, 2, 16], F32)
        mpe = ar.alloc("mpe", [64, 2], F32)
        nb = ar.alloc("nb", [64, 4], F32)
        ohj = c["c_ohj"]
        self.dve(lambda e: e.tensor_tensor(out=tmp2[:], in0=AP(ps, 3 * 512, [[4096, 64], [16, 2], [1, 16]]),
                                           in1=AP(ohj, 0, [[16, 64], [0, 2], [1, 16]]), op=ALU.mult),
                 [self.bank(3), "c_ohj"], ["tmp2"])
        self.dve(lambda e: e.tensor_reduce(out=mpe[:], in_=tmp2[:], axis=AX.X, op=ALU.add), ["tmp2"], ["mpe"])
        Mg = T("Mg")
        self.dve(lambda e: e.tensor_scalar(out=Mg[:], in0=cm[:], scalar1=mpe[:, 0:1], scalar2=None, op0=ALU.max), ["cm", "mpe"], ["Mg"])
        self.dve(lambda e: e.tensor_scalar(out=nb[:, 0:1], in0=mpe[:, 0:1], scalar1=-1.0, scalar2=None, op0=ALU.mult), ["mpe"], ["nb"])
        self.dve(lambda e: e.tensor_scalar(out=nb[:, 1:2], in0=mpe[:, 1:2], scalar1=-1.0, scalar2=math.log(SC_M), op0=ALU.mult, op1=ALU.add), ["mpe"], ["nb"])
        self.dve(lambda e: e.tensor_tensor(out=nb[:, 2:3], in0=mpe[:, 0:1], in1=mpe[:, 1:2], op=ALU.subtract), ["mpe"], ["nb"])
        u, ws, w, fl, d2, dec, ddg = T("u"), T("ws"), T("w"), T("fl"), T("d2"), T("dec", 1), T("ddg", 64)
        self.act(u[:], A[:], AF.Exp, ["A", "nb"], ["u"], bias=nb[:, 0:1])
        self.act(ws[:], A[:], AF.Exp, ["A", "nb"], ["ws"], bias=nb[:, 1:2])
        self.act(w[:], Mg[:], AF.Exp, ["Mg", "mpe"], ["w"], scale=-1.0, bias=mpe[:, 0:1])
        self.dve(lambda e: e.tensor_tensor(out=d2[:], in0=Bn[:], in1=Mg[:], op=ALU.subtract), ["Bn", "Mg"], ["d2"])
        self.act(fl[:], d2[:], AF.Exp, ["d2"], ["fl"])
        self.act(dec[:], nb[:, 2:3], AF.Exp, ["nb"], ["dec"])
        dec3 = ar.alloc("dec3", [64, 3, 2], BF16)
        decf = ar.alloc("decf", [64, 2], F32)
        self.dve(lambda e: e.tensor_copy(out=decf[:], in_=AP(dec, 0, [[1, 64], [0, 2]])), ["dec"], ["decf"])
        k3d = self.split3(dec3, decf[:], 64, 2, "decf", "dec3")
        ddg3 = ar.alloc("ddg3", [64, 3, 64], BF16)
        decs = ar.alloc("decs", [64, 4], F32)
        self.dve(lambda e: e.tensor_copy(out=decs[:, 0:3], in_=dec3[:, :, 0]), k3d, ["decs"])
        for i in range(3):
            self.dve((lambda i: lambda e: e.tensor_scalar(out=ddg3[:, i, :], in0=c["c_identb"][0:64, 0:64], scalar1=decs[:, i:i + 1], scalar2=None, op0=ALU.mult))(i),
                     ["decs", "c_identb"], [("ddg3", i)])

        def fn3(e):
            first = None
            for i, arr in enumerate((u, ws, w, fl)):
                ins = e.transpose(out=ps[:, 4, i * 64:(i + 1) * 64], in_=arr[:], identity=idf[0:64, 0:64])
                if first is None:
                    first = ins
            for i in range(3):
                last = e.matmul(ps[:, 4, 256:320], c["c_onesb"][0:64, :], ddg3[:, i, :], start=(i == 0), stop=(i == 2))
            return first, last
        P.op("pe", fn3, reads=["u", "ws", "w", "fl", "c_onesb", "c_identf"] + [("ddg3", i) for i in range(3)], writes=[self.bank(4)], name="gt_tr")
        self.dve(lambda e: e.tensor_copy(out=gt[:], in_=AP(ps, 4 * 512, [[4096, 128], [64, 5], [1, 64]])), [self.bank(4)], ["gt"])

    def vfam_project(self, l, a, col0, slot, slotkey, banks=(0, 1, 2, 3)):
        P, ps = self.P, self.ps
        vf, hT = a["vfam"], self.hT
        self.wload(slot[:], self.din["w_in"][l][:, col0:col0 + 512].rearrange("(k p) n -> p k n", p=128),
                   [(slotkey, i) for i in range(3)], "s%d_0" % slotkey[1])
        slotkey = (slotkey, 0)
        for j in range(NB):
            bk = banks[j % len(banks)]
            self.mm(ps[:, bk, :], [(hT[:, k, j * 128:(j + 1) * 128], slot[:, k, :]) for k in range(KC)],
                    reads=[slotkey, ("hT", j // 4)], writes=[self.bank(bk)], name="vproj")
            dst = AP(vf, j * 4 * 129, [[NB * 4 * 129, 128], [129, 4], [1, 128]])
            src = AP(ps, bk * 512, [[4096, 128], [128, 4], [1, 128]])
            self.evac(j, dst, src, [self.bank(bk)], [("vfam", j)])

    def mlstm(self, l, a):
        P, ar, ps, c = self.P, self.ar, self.ps, self.c
        hT, mixT, vf, gt = self.hT, a["mixT"], a["vfam"], a["gt"]
        psb = [ps[:, b, :].bitcast(BF16) for b in range(8)]
        m_g = ar.mark()
        gates_ops = P.capture(lambda: self.gates(l, a))
        g_end = ar.mark()
        ar.reset(m_g)
        accs = ar.alloc("accs", [128, NB, 129], F32)
        CTf = ar.alloc("CTf", [128, 129], F32)
        CTb = [ar.alloc("CTb", [128, 129], BF16) for _ in range(2)]
        scT = [ar.alloc("scT", [128, 128], BF16) for _ in range(2)]
        junk = ar.alloc("junk", [128, 128], BF16)
        sm = ar.alloc("sm", [128, 6, NB], F32)
        hn4 = [ar.alloc("hn4", [128, 4, 128], BF16) for _ in range(2)]
        if ar.cur < g_end:
            ar.cur = g_end
        ubuf = ar.alloc("ubuf", [128, 3 + S], BF16)
        diag = ar.alloc("diag", [128, 4, 128], BF16)
        ktok = [ar.alloc("ktok", [128, NB, 128], BF16) for _ in range(2)]
        qTs = [a["qT"], ar.alloc("qT2", [128, S], BF16)]
        kTs = [a["kT"], ar.alloc("kT2", [128, S], BF16)]
        slots = [(a["slotA"], ("slot", 0)), (a["slotB"], ("slot", 1))]
        PB = (6, 7)
        w_in = self.din["w_in"][l]
        cw, cb = self.cw, self.cb

        def vpart():
            self.dve(lambda e: e.memset(ubuf[:, 0:3], 0.0), [], ["ubuf"])
            self.dve(lambda e: e.memset(AP(vf, 128, [[NB * 4 * 129, 128], [129, NB * 4]]), 1.0), [], [("vfam", j) for j in range(NB)])
            self.vfam_project(l, a, VM0, *slots[0], banks=PB)

        def proj(h):
            hb = h % 2
            qT, kT = qTs[hb], kTs[hb]
            slot, sk = slots[(h + 1) % 2]
            nb = [0]

            def nbk():
                nb[0] += 1
                return PB[nb[0] % 2]
            for i, c0 in enumerate((QM0 + 128 * h, KM0 + 128 * h, OM0 + 128 * h)):
                self.wload(slot[:, :, i * 128:(i + 1) * 128], w_in[:, c0:c0 + 128].rearrange("(k p) n -> p k n", p=128), (sk, i), "s%d_%d" % (sk[1], i))
            for tt in range(NT):
                tsl = slice(tt * TT, (tt + 1) * TT)
                bk = nbk()
                self.mm(ps[:, bk, :], [(slot[:, k, 256:384], hT[:, k, tsl]) for k in range(KC)],
                        reads=[(sk, 2), ("hT", tt)], writes=[self.bank(bk)], name="oproj")
                self.act(mixT[:, h, tsl], ps[:, bk, :], AF.Tanh, [self.bank(bk)], [("mixT", h, tt)], scale=0.5)
            for i, dstT in enumerate((qT, kT)):
                cch = h if i == 0 else 4 + h
                for j in range(4):
                    self.dve((lambda j, cch: lambda e: e.tensor_scalar(out=diag[:, j, :], in0=c["c_identb"][:], scalar1=cw[:, l, j, cch:cch + 1],
                                                                        scalar2=None, op0=ALU.mult))(j, cch),
                             ["c_identb", ("cw", l, j)], ["diag"])
                for tt in range(NT):
                    tsl = slice(tt * TT, (tt + 1) * TT)
                    bk = nbk()
                    self.mm(ps[:, bk, :], [(slot[:, k, i * 128:(i + 1) * 128], hT[:, k, tsl]) for k in range(KC)],
                            reads=[(sk, i), ("hT", tt)], writes=[self.bank(bk)], name="qkproj")
                    self.evac(tt, ubuf[:, 3 + tt * TT:3 + (tt + 1) * TT], ps[:, bk, :], [self.bank(bk)], ["ubuf"])
                for tt in range(NT):
                    tsl = slice(tt * TT, (tt + 1) * TT)
                    bk = nbk()
                    self.mm(ps[:, bk, :], [(diag[:, j, :], ubuf[:, tt * TT + j:tt * TT + j + TT]) for j in range(4)],
                            reads=["diag", "ubuf"], writes=[self.bank(bk)], name="conv")
                    self.act(dstT[:, tsl], ps[:, bk, :], AF.Silu, [self.bank(bk), ("cb", l)], [("qk", hb, i, tt)], bias=cb[:, l, cch:cch + 1])
            for jg in range(4):
                bk = nbk()
                self.transposes([(psb[bk][:, cc * 128:(cc + 1) * 128], kT[:, (4 * jg + cc) * 128:(4 * jg + cc + 1) * 128], c["c_identb"][:]) for cc in range(4)],
                                reads=[("qk", hb, 1, jg), "c_identb"], writes=[self.bank(bk)], name="ktr")
                self.dve((lambda jg, bk, h, hb: lambda e: e.tensor_tensor(
                    out=ktok[hb][:, 4 * jg:4 * jg + 4, :], in0=psb[bk][:, 0:512].rearrange("p (c d) -> p c d", c=4),
                    in1=AP(gt, 64 + 16 * jg + h, [[320, 128], [4, 4], [0, 128]]), op=ALU.mult))(jg, bk, h, hb),
                    [self.bank(bk), "gt"], [("ktok", hb, jg)])

        def rec(h):
            hb = h % 2
            qT, kT, kt = qTs[hb], kTs[hb], ktok[hb]
            self.dve(lambda e: e.memset(CTf[:], 0.0), [], ["CTf"])

            def f_st(j):
                bsl = slice(j * 128, (j + 1) * 128)
                i = j % 2
                self.mm(ps[:, i, 0:128], [(kT[:, bsl], qT[:, bsl])], reads=[("qk", hb, 0, j // 4), ("qk", hb, 1, j // 4)],
                        writes=[self.bank(i)], name="ST")
                self.dve(lambda e: e.scalar_tensor_tensor(out=scT[i][:], in0=ps[:, i, 0:128], scalar=gt[:, 0, 4 * j + h:4 * j + h + 1],
                                                          in1=c["c_maskf"][:], op0=ALU.mult, op1=ALU.mult),
                         [self.bank(i), "gt", "c_maskf"], [("scT", i)])

            def f_upd(j):
                b2 = 4 + j % 2
                self.mm(ps[:, b2, 0:129], [(kt[:, j, :], vf[:, j, h, :])], reads=[("ktok", hb, j // 4), ("vfam", j)],
                        writes=[self.bank(b2)], name="upd")
                self.dve(lambda e: e.scalar_tensor_tensor(out=CTf[:], in0=CTf[:], scalar=gt[:, 4, 4 * j + h:4 * j + h + 1],
                                                          in1=ps[:, b2, 0:129], op0=ALU.mult, op1=ALU.add),
                         [self.bank(b2), "gt", "CTf"], ["CTf"])
                self.act(CTb[j % 2][:], CTf[:], AF.Copy, ["CTf"], [("CTb", j % 2)])

            def f_acc(j):
                bsl = slice(j * 128, (j + 1) * 128)
                i = j % 2
                b1 = 2 + i
                pairs = [(scT[i][:], vf[:, j, h, :])]
                rd = [("scT", i), ("vfam", j)]
                if j > 0:
                    pairs.append((qT[:, bsl], CTb[(j - 1) % 2][:]))
                    rd += [("qk", hb, 0, j // 4), ("CTb", (j - 1) % 2)]
                self.mm(ps[:, b1, 0:129], pairs, reads=rd, writes=[self.bank(b1)], name="acc")
                self.dve(lambda e: e.tensor_copy(out=accs[:, j, :], in_=ps[:, b1, 0:129]), [self.bank(b1)], [("accs", j)])
                self.act(junk[:], accs[:, j, 0:128], AF.Square, [("accs", j)], ["junk", ("ssq", j)], accum=sm[:, 0, j:j + 1])

            f_st(0)
            f_upd(0)
            for j in range(NB):
                if j + 1 < NB:
                    f_st(j + 1)
                f_acc(j)
                if j + 1 < NB - 1:
                    f_upd(j + 1)
            ak = [("accs", j) for j in range(NB)]
            den = AP(accs, 128, [[NB * 129, 128], [129, NB]])
            w_tm = AP(gt, 2 * 64 + h, [[320, 128], [4, NB]])
            fl_tm = AP(gt, 3 * 64 + h, [[320, 128], [4, NB]])
            s_ = lambda r: sm[:, r, :]
            self.dve(lambda e, w_tm=w_tm: e.tensor_tensor(out=s_(1), in0=den, in1=w_tm, op=ALU.mult), ak + ["gt"], ["sm1"])
            self.dve(lambda e: e.tensor_scalar(out=s_(2), in0=s_(1), scalar1=-1.0, scalar2=None, op0=ALU.mult), ["sm1"], ["sm2"])
            self.dve(lambda e: e.tensor_tensor(out=s_(2), in0=s_(2), in1=s_(1), op=ALU.max), ["sm1", "sm2"], ["sm2"])
            self.dve(lambda e, fl_tm=fl_tm: e.tensor_tensor(out=s_(2), in0=s_(2), in1=fl_tm, op=ALU.max), ["sm2", "gt"], ["sm2"])
            self.dve(lambda e: e.reciprocal(out=s_(3), in_=s_(2)), ["sm2"], ["sm3"])
            self.dve(lambda e, w_tm=w_tm: e.tensor_tensor(out=s_(3), in0=s_(3), in1=w_tm, op=ALU.mult), ["sm3", "gt"], ["sm3"])
            self.dve(lambda e: e.tensor_tensor(out=s_(4), in0=s_(0), in1=s_(3), op=ALU.mult), ["sm3"] + [("ssq", j) for j in range(NB)], ["sm4"])
            self.dve(lambda e: e.tensor_tensor(out=s_(4), in0=s_(4), in1=s_(3), op=ALU.mult), ["sm3", "sm4"], ["sm4"])
            self.dve(lambda e: e.tensor_scalar(out=s_(4), in0=s_(4), scalar1=1.0 / 128, scalar2=None, op0=ALU.mult), ["sm4"], ["sm4"])
            self.rsqrt_small(s_(4), "sm4", HEPS)
            self.dve(lambda e: e.tensor_tensor(out=s_(5), in0=s_(4), in1=s_(3), op=ALU.mult), ["sm3", "sm4"], ["sm5"])
            for jg in range(4):
                hb_ = hn4[jg % 2]
                hk = ("hn4", jg % 2)
                self.dve((lambda jg, hb_: lambda e: e.tensor_tensor(out=hb_[:], in0=accs[:, 4 * jg:4 * jg + 4, 0:128],
                                                                    in1=AP(sm, 5 * NB + 4 * jg, [[6 * NB, 128], [1, 4], [0, 128]]), op=ALU.mult))(jg, hb_),
                         ak + ["sm5"], [hk])
                self.dve((lambda hb_, h: lambda e: e.tensor_tensor(out=hb_[:], in0=hb_[:], in1=AP(self.gm_bc, h * 128, [[512, 128], [0, 4], [1, 128]]), op=ALU.mult))(hb_, h),
                         [hk, "gm_bc"], [hk])
                bk = jg % 2
                self.transposes([(psb[bk][:, cc * 128:(cc + 1) * 128], hb_[:, cc, :], c["c_identb"][:]) for cc in range(4)],
                                reads=[hk, "c_identb"], writes=[self.bank(bk)], name="hntr")
                msl = mixT[:, h, jg * TT:(jg + 1) * TT]
                self.dve((lambda msl, bk: lambda e: e.scalar_tensor_tensor(out=msl, in0=msl, scalar=1.0, in1=psb[bk][:, 0:512], op0=ALU.add, op1=ALU.mult))(msl, bk),
                         [self.bank(bk), ("mixT", h, jg)], [("mixT", h, jg)])

        v_ops = P.capture(vpart)
        P.replay_merged([v_ops, gates_ops], weights=[1, 2])
        p_ops = [P.capture((lambda h: lambda: proj(h))(h)) for h in range(4)]
        r_ops = [P.capture((lambda h: lambda: rec(h))(h)) for h in range(4)]
        P.replay_merged([p_ops[0]])
        for h in range(4):
            if h < 3:
                P.replay_merged([r_ops[h], p_ops[h + 1]], weights=[2, 1])
            else:
                P.replay_merged([r_ops[h]])

    def wout_half(self, l, a, f, wo, wkey):
        ps, xT, mixT = self.ps, self.xT, a["mixT"]
        self.wload(wo[:], self.din["w_out"][l][f * 512:(f + 1) * 512, :].rearrange("(k p) n -> p k n", p=128),
                   [(wkey, i) for i in range(3)], "s%d_0" % wkey[1])
        wkey = (wkey, 0)
        n = 0
        for tt in range(NT):
            tsl = slice(tt * TT, (tt + 1) * TT)
            for dm in range(KC):
                bk = n % 8
                n += 1
                self.mm(ps[:, bk, :], [(wo[:, k, dm * 128:(dm + 1) * 128], mixT[:, k, tsl]) for k in range(4)],
                        reads=[wkey] + [("mixT", k, tt) for k in range(4)], writes=[self.bank(bk)], name="wout")
                xs = xT[:, dm, tsl]
                self.dve((lambda xs, bk: lambda e: e.tensor_tensor(out=xs, in0=xs, in1=ps[:, bk, :], op=ALU.add))(xs, bk),
                         [self.bank(bk), ("xT", tt)], [("xT", tt)])

    def attention(self, l, a):
        P, ar, ps, c = self.P, self.ar, self.ps, self.c
        hT, mixT, vf, qT, kT = self.hT, a["mixT"], a["vfam"], a["qT"], a["kT"]
        psb = [ps[:, b, :].bitcast(BF16) for b in range(8)]
        zb = [ar.alloc("zb", [128, TT], BF16) for _ in range(2)]
        t1 = ar.alloc("t1", [128, TT], F32)
        t2 = ar.alloc("t2", [128, TT], F32)
        Pt = [ar.alloc("Pt", [128, 2, TT], BF16) for _ in range(2)]
        Osb = ar.alloc("Osb", [128, 4, 2, 129], F32)
        o1 = ar.alloc("o1", [128, 4, 128], F32)
        o2 = ar.alloc("o2", [128, 4, 128], F32)
        on4 = ar.alloc("on4", [128, 4, 128], BF16)
        sa = ar.alloc("sa", [128, 6, 8], F32)
        slots = [(a["slotA"], ("slot", 0)), (a["slotB"], ("slot", 1))]
        self.vfam_project(l, a, VA0, *slots[0])
        w_in = self.din["w_in"][l]
        cosT, sinT = c["c_cos"], c["c_sin"]
        nlam = self.lam_s[:, 5:6]
        for h in range(4):
            slot, sk = slots[(h + 1) % 2]
            for i, c0 in enumerate((QA0 + 128 * h, KA0 + 128 * h)):
                self.wload(slot[:, :, i * 128:(i + 1) * 128], w_in[:, c0:c0 + 128].rearrange("(k p) n -> p k n", p=128), (sk, i), "s%d_%d" % (sk[1], i))
            for i, dstT in enumerate((qT, kT)):
                def a_proj(tt, i=i):
                    tsl = slice(tt * TT, (tt + 1) * TT)
                    b0 = (2 * tt) % 4
                    z = zb[tt % 2]
                    zk = ("zb", tt % 2)
                    self.mm(ps[:, b0, :], [(slot[:, k, i * 128:(i + 1) * 128], hT[:, k, tsl]) for k in range(KC)],
                            reads=[(sk, i), ("hT", tt)], writes=[self.bank(b0)], name="aproj")
                    self.dve((lambda z, b0: lambda e: e.tensor_copy(out=z[:], in_=ps[:, b0, :]))(z, b0), [self.bank(b0)], [zk])

                def a_rot(tt, i=i, dstT=dstT):
                    tsl = slice(tt * TT, (tt + 1) * TT)
                    b0 = (2 * tt) % 4
                    b1 = b0 + 1
                    z = zb[tt % 2]
                    zk = ("zb", tt % 2)
                    self.mm(ps[:, b1, :], [(c["c_perm"][:], z[:])], reads=[zk, "c_perm"], writes=[self.bank(b1)], name="rot")
                    self.dve((lambda b0, tsl: lambda e: e.tensor_tensor(out=t1[:], in0=ps[:, b0, :], in1=cosT[:, tsl], op=ALU.mult))(b0, tsl),
                             [self.bank(b0), "c_cos"], ["t1"])
                    self.dve((lambda b1, tsl: lambda e: e.tensor_tensor(out=t2[:], in0=ps[:, b1, :], in1=sinT[:, tsl], op=ALU.mult))(b1, tsl),
                             [self.bank(b1), "c_sin"], ["t2"])
                    self.dve((lambda dstT, tsl: lambda e: e.tensor_tensor(out=dstT[:, tsl], in0=t1[:], in1=t2[:], op=ALU.add))(dstT, tsl),
                             ["t1", "t2"], [("qk", i, tt)])
                a_proj(0)
                for tt in range(NT):
                    if tt + 1 < NT:
                        a_proj(tt + 1)
                    a_rot(tt)
            for qt in range(NT if self.att_level >= 2 else 0):
                nkb = 4 * qt + 4

                def step_qk(kb, qt=qt, h=h):
                    di = kb - 4 * qt
                    q0 = max(di, 0) * 128
                    N = TT - q0
                    pi = kb % 2
                    pt = Pt[pi]
                    sb0, sb1 = 2 * pi, 2 * pi + 1

                    def fqk(e):
                        i0 = e.matmul(ps[:, sb0, 0:N], kT[0:64, kb * 128:(kb + 1) * 128], qT[0:64, qt * TT + q0:(qt + 1) * TT], start=True, stop=True)
                        i1 = e.matmul(ps[:, sb1, 0:N], kT[64:128, kb * 128:(kb + 1) * 128], qT[64:128, qt * TT + q0:(qt + 1) * TT], start=True, stop=True)
                        return i0, i1
                    P.op("pe", fqk, reads=[("qk", 0, qt), ("qk", 1, kb // 4)], writes=[self.bank(sb0), self.bank(sb1)], name="qk")
                    self.act(pt[:, 0, 0:N], ps[:, sb0, 0:N], AF.Exp, [self.bank(sb0)], [("Pt", pi, 0)], scale=SC_A)
                    self.act(pt[:, 1, 0:N], ps[:, sb1, 0:N], AF.Exp, [self.bank(sb1)], [("Pt", pi, 1)], scale=SC_A)
                    if di >= 0:
                        self.dve(lambda e: e.tensor_tensor(out=pt[:, :, 0:128], in0=pt[:, :, 0:128],
                                                           in1=AP(c["c_maskb"], 0, [[128, 128], [0, 2], [1, 128]]), op=ALU.mult),
                                 [("Pt", pi, 0), ("Pt", pi, 1), "c_maskb"], [("Pt", pi, 0), ("Pt", pi, 1)])

                def step_pv(kb, qt=qt, h=h):
                    di = kb - 4 * qt
                    pi = kb % 2
                    pt = Pt[pi]
                    qs0 = max(di, 0)

                    def fpv(e):
                        first = last = None
                        for qs in range(qs0, 4):
                            for cc in range(2):
                                ins = e.matmul(ps[:, 4 + qs, cc * 129:(cc + 1) * 129], pt[:, cc, (qs - qs0) * 128:(qs - qs0 + 1) * 128], vf[:, kb, h, :],
                                               start=(kb == 0 and cc == 0), stop=(kb == 4 * qt + qs), skip_group_check=True)
                                if first is None:
                                    first = ins
                                last = ins
                        return first, last
                    P.op("pe", fpv, reads=[("Pt", pi, 0), ("Pt", pi, 1), ("vfam", kb)], writes=[self.bank(4 + qs) for qs in range(qs0, 4)], name="pv")
                    if di >= 0:
                        qs = di
                        self.dve(lambda e: e.tensor_copy(out=Osb[:, qs, :, :], in_=ps[:, 4 + qs, 0:258].rearrange("p (c e) -> p c e", c=2)),
                                 [self.bank(4 + qs)], ["Osb"])
                step_qk(0)
                for kb in range(nkb):
                    if kb + 1 < nkb:
                        step_qk(kb + 1)
                    if self.att_level >= 3:
                        step_pv(kb)
                if self.att_level < 4:
                    continue
                lcol = AP(Osb, 128, [[4 * 2 * 129, 128], [129, 8]])
                self.dve(lambda e: e.reciprocal(out=sa[:, 0, :], in_=lcol), ["Osb"], ["sa0"])
                self.dve(lambda e: e.tensor_scalar(out=sa[:, 1, :], in0=sa[:, 0, :], scalar1=nlam, scalar2=None, op0=ALU.mult), ["sa0", "nlam"], ["sa1"])
                self.dve(lambda e: e.tensor_tensor(out=o1[:], in0=Osb[:, :, 0, 0:128], in1=AP(sa, 0, [[48, 128], [2, 4], [0, 128]]), op=ALU.mult), ["Osb", "sa0"], ["o1"])
                self.dve(lambda e: e.tensor_tensor(out=o2[:], in0=Osb[:, :, 1, 0:128], in1=AP(sa, 8 + 1, [[48, 128], [2, 4], [0, 128]]), op=ALU.mult), ["Osb", "sa1"], ["o2"])
                self.dve(lambda e: e.tensor_tensor(out=o1[:], in0=o1[:], in1=o2[:], op=ALU.add), ["o1", "o2"], ["o1"])
                self.dve(lambda e: e.tensor_tensor(out=o2[:], in0=o1[:], in1=o1[:], op=ALU.mult), ["o1", "o2"], ["o2"])
                self.dve(lambda e: e.tensor_reduce(out=sa[:, 2, 0:4], in_=o2[:], axis=AX.X, op=ALU.add), ["o2"], ["sa2"])
                self.dve(lambda e: e.tensor_scalar(out=sa[:, 2, 0:4], in0=sa[:, 2, 0:4], scalar1=1.0 / 128, scalar2=None, op0=ALU.mult), ["sa2"], ["sa2"])
                self.rsqrt_small(sa[:, 2, 0:4], "sa2", HEPS)
                self.dve(lambda e: e.tensor_tensor(out=o1[:], in0=o1[:], in1=AP(sa, 16, [[48, 128], [1, 4], [0, 128]]), op=ALU.mult), ["o1", "sa2"], ["o1"])
                self.dve((lambda h: lambda e: e.tensor_tensor(out=on4[:], in0=o1[:], in1=AP(self.gd_bc, h * 128, [[512, 128], [0, 4], [1, 128]]), op=ALU.mult))(h),
                         ["o1", "gd_bc"], ["on4"])
                bk = 2 * (qt % 2)
                self.transposes([(psb[bk][:, cc * 128:(cc + 1) * 128], on4[:, cc, :], c["c_identb"][:]) for cc in range(4)],
                                reads=["on4", "c_identb"], writes=[self.bank(bk)], name="ontr")
                self.act(mixT[:, h, qt * TT:(qt + 1) * TT], psb[bk][:, 0:512], AF.Copy, [self.bank(bk)], [("mixT", h, qt)])

    def ffn(self, l):
        P, ar, ps = self.P, self.ar, self.ps
        hT, xT, din = self.hT, self.xT, self.din
        wg = [ar.alloc("fwg", [128, KC, 6 * 128], BF16) for _ in range(2)]
        wu = [ar.alloc("fwu", [128, KC, 6 * 128], BF16) for _ in range(2)]
        wd = [ar.alloc("fwd", [128, 6, D], BF16) for _ in range(2)]
        actb = [ar.alloc("actb", [128, 6, TT], BF16) for _ in range(2)]
        sg = [ar.alloc("sg", [128, TT], BF16) for _ in range(2)]
        n_e = 0
        n_d = [0]
        step = 0
        pending = None

        def down(i, ncn, tt, ab, abk):
            tsl = slice(tt * TT, (tt + 1) * TT)
            for dm in range(KC):
                bk = 4 + n_d[0] % 4
                n_d[0] += 1
                self.mm(ps[:, bk, :], [(wd[i][:, hc, dm * 128:(dm + 1) * 128], ab[:, hc, :]) for hc in range(ncn)],
                        reads=[("fwd", i)] + [("actb", abk, hc) for hc in range(ncn)], writes=[self.bank(bk)], name="down")
                xs = xT[:, dm, tsl]
                self.dve((lambda xs, bk: lambda e: e.tensor_tensor(out=xs, in0=xs, in1=ps[:, bk, :], op=ALU.add))(xs, bk),
                         [self.bank(bk), ("xT", tt)], [("xT", tt)])

        for gi, (c0, ncn) in enumerate(FFN_GROUPS):
            i = gi % 2
            self.wload(wg[i][:, :, 0:ncn * 128], din["w_gate"][l][:, c0 * 128:(c0 + ncn) * 128].rearrange("(k p) n -> p k n", p=128), ("fwg", i), "fwg%d" % i)
            self.wload(wu[i][:, :, 0:ncn * 128], din["w_up"][l][:, c0 * 128:(c0 + ncn) * 128].rearrange("(k p) n -> p k n", p=128), ("fwu", i), "fwu%d" % i)
            self.wload(wd[i][:, 0:ncn, :], din["w_down"][l][c0 * 128:(c0 + ncn) * 128, :].rearrange("(c p) n -> p c n", p=128), ("fwd", i), "fwd%d" % i)
            for tt in range(NT):
                tsl = slice(tt * TT, (tt + 1) * TT)
                abk = step % 2
                ab = actb[abk]
                step += 1
                for hc in range(ncn):
                    bg_, bu_ = (0, 1) if n_e % 2 == 0 else (2, 3)
                    s_ = sg[n_e % 2]
                    sgk = ("sg", n_e % 2)
                    n_e += 1
                    self.mm(ps[:, bg_, :], [(wg[i][:, k, hc * 128:(hc + 1) * 128], hT[:, k, tsl]) for k in range(KC)],
                            reads=[("fwg", i), ("hT", tt)], writes=[self.bank(bg_)], name="gate")
                    self.mm(ps[:, bu_, :], [(wu[i][:, k, hc * 128:(hc + 1) * 128], hT[:, k, tsl]) for k in range(KC)],
                            reads=[("fwu", i), ("hT", tt)], writes=[self.bank(bu_)], name="up")
                    self.act(s_[:], ps[:, bg_, :], AF.Silu, [self.bank(bg_)], [sgk])
                    self.dve((lambda ab, hc, s_, bu_: lambda e: e.tensor_tensor(out=ab[:, hc, :], in0=s_[:], in1=ps[:, bu_, :], op=ALU.mult))(ab, hc, s_, bu_),
                             [sgk, self.bank(bu_)], [("actb", abk, hc)])
                if pending is not None:
                    down(*pending)
                pending = (i, ncn, tt, ab, abk)
        down(*pending)

    def store(self, sq, dst_dram, normalize):
        P, ar, ps, c = self.P, self.ar, self.ps, self.c
        m = ar.mark()
        yT = [ar.alloc("yT", [128, KC, TT], F32) for _ in range(2)]
        ob = [ar.alloc("ob", [128, D], F32) for _ in range(2)]
        nb_ = 0
        gf = self.gf
        for tt in range(NT):
            if normalize:
                self.norm(lambda cc: gf[:, cc:cc + 1], "gf", out_f32=lambda tt, cc: (yT[tt % 2][:, cc, :], ("yT", tt % 2)), tts=[tt])
            for bb in range(4):
                blk = tt * 4 + bb
                o_ = ob[nb_ % 2]
                ok = ("ob", nb_ % 2)
                nb_ += 1
                for half in range(2):
                    bk = (2 * blk + half) % 8
                    if normalize:
                        srcs = [yT[tt % 2][:, half * 4 + cc, bb * 128:(bb + 1) * 128] for cc in range(4)]
                        rk = [("yT", tt % 2)]
                    else:
                        srcs = [self.xT[:, half * 4 + cc, blk * 128:(blk + 1) * 128] for cc in range(4)]
                        rk = [("xT", tt)]
                    self.transposes([(ps[:, bk, cc * 128:(cc + 1) * 128], srcs[cc], c["c_identf"][:]) for cc in range(4)],
                                    reads=rk + ["c_identf"], writes=[self.bank(bk)], name="otr")
                    self.evac(half, o_[:, half * 512:(half + 1) * 512], ps[:, bk, :], [self.bank(bk)], [ok])
                P.op("sp", (lambda o_, blk: lambda e: e.dma_start(out=dst_dram[sq, blk * 128:(blk + 1) * 128, :], in_=o_[:]))(o_, blk),
                     reads=[ok], name="ostore", dma="st%d" % ((nb_ - 1) % 2))
        P.barrier()
        ar.reset(m)

    def build(self, stage=99):
        P, ar = self.P, self.ar
        self.setup()
        for sq in range(self.nseq):
            self.load_x(sq)
            for l in self.layers:
                if stage < 1:
                    break
                self.layer_params(l)
                g1 = self.g1
                self.norm((lambda l: lambda cc: g1[:, l, cc:cc + 1])(l), ("g1", l))
                P.barrier()
                self.dump("hT", self.hT, [("hT", t) for t in range(NT)])
                m = ar.mark()
                a = self.mixer_alloc()
                m2 = ar.mark()
                if stage >= 3:
                    self.mlstm(l, a)
                    self.dump("mixM", a["mixT"], [("mixT", h, t) for h in range(4) for t in range(NT)])
                if stage >= 4:
                    self.wout_half(l, a, 0, a["woB"], ("slot", 1))
                P.barrier()
                ar.reset(m2)
                if stage >= 5:
                    self.attention(l, a)
                    self.dump("mixA", a["mixT"], [("mixT", h, t) for h in range(4) for t in range(NT)])
                if stage >= 6:
                    self.wout_half(l, a, 1, a["woB"], ("slot", 1))
                    self.dump("xmid", self.xT, [("xT", t) for t in range(NT)])
                P.barrier()
                ar.reset(m)
                if stage >= 7:
                    g2 = self.g2
                    self.norm((lambda l: lambda cc: g2[:, l, cc:cc + 1])(l), ("g2", l))
                    P.barrier()
                    self.ffn(l)
                    P.barrier()
                ar.reset(m)
            self.store(sq, self.out, self.final_norm)
        P.emit()
        return self.nc


_CONSTS = None


def _run(nc_inputs_list, layers, final_norm):
    b = Builder(layers, nseq=2, final_norm=final_norm)
    nc = b.build()
    res = run_bass_kernel_spmd(nc, nc_inputs_list, core_ids=list(range(8)))
    return [r["out"] for r in res.results]


def kernel(**inputs):
    global _CONSTS
    if _CONSTS is None:
        _CONSTS = host_constants()
    x = np.ascontiguousarray(inputs["x"], dtype=np.float32)
    params = {n: np.ascontiguousarray(inputs[n], dtype=np.float32) for n, _ in PARAM_SPECS}
    shards = [x[2 * i:2 * i + 2] for i in range(8)]
    in_maps = []
    for i in range(8):
        mp = {"x": shards[i]}
        mp.update(params)
        mp.update(_CONSTS)
        in_maps.append(mp)
    outs = _run(in_maps, list(range(DEPTH)), True)
    return np.concatenate(outs, axis=0)
```
